# Optimizing a Trainium2 kernel written in Bass

```python
import jax
import jax.numpy as jnp
from jax import lax
import numpy as np

D_MODEL = 1024
BATCH = 8
SEQ = 4096
DEPTH = 1

GRID_W = 64
CTX_LEN = 256
NORM_EPS = 1e-6

RWKV_WIDTH = 1024
RWKV_HEAD = 64
RWKV_HEADS = RWKV_WIDTH // RWKV_HEAD
DECAY_LORA = 64
AAA_LORA = 64
GATE_LORA = 160
LNX_EPS = 64e-5

LRU_WIDTH = 1024
LRU_BLOCKS = 16
LRU_BLOCK = LRU_WIDTH // LRU_BLOCKS
CONV_W = 4
CONV_LEFT = 2
LRU_C = 8.0

N_EXPERTS = 64
TOP_K = 6
N_GROUPS = 8
TOPK_GROUPS = 4
EXPERT_FF = 256
SHARED_FF = 256
ROUTED_SCALE = 2.5
MOE_BLOCK = 128

OFF_R = 0
OFF_K = OFF_R + RWKV_WIDTH
OFF_V = OFF_K + RWKV_WIDTH
OFF_WD = OFF_V + RWKV_WIDTH
OFF_AD = OFF_WD + 2 * DECAY_LORA
OFF_GD = OFF_AD + 2 * AAA_LORA
RWKV_COLS = OFF_GD + GATE_LORA
OFF_LX = RWKV_COLS
OFF_LG = OFF_LX + LRU_WIDTH
OFF_GATE = OFF_LG + LRU_WIDTH
IN_COLS = OFF_GATE + 2 * D_MODEL

kernel_name = 'hybrid_rwkv7_rglru_moe_flow_block'


def _rmsnorm(x, g):
    xf = x.astype(jnp.float32)
    y = xf * lax.rsqrt(jnp.mean(xf * xf, axis=-1, keepdims=True) + NORM_EPS)
    return (y * g.astype(jnp.float32)).astype(x.dtype)


def _modulate(x, shift, scale):
    return x * (1.0 + scale) + shift


def _shift_centred(z, mu):
    pad = [(0, 0)] * (z.ndim - 2) + [(1, 1), (0, 0)]
    zp = jnp.pad(z, pad)
    return z + mu[0] * (zp[..., :-2, :] - z) + mu[1] * (zp[..., 2:, :] - z)


def _dwconv(z, w, b):
    lead, (t, ch) = z.shape[:-2], z.shape[-2:]
    y = lax.conv_general_dilated(z.reshape((-1, t, ch)), w[:, None, :].astype(z.dtype),
                                 window_strides=(1,), padding=[(CONV_LEFT, CONV_W - 1 - CONV_LEFT)],
                                 dimension_numbers=('NWC', 'WIO', 'NWC'), feature_group_count=ch)
    return (y + b).reshape(lead + (t, ch))


def _swiglu(x, w_gu, w_down):
    g, u = jnp.split(x @ w_gu, 2, axis=-1)
    return (jax.nn.silu(g) * u) @ w_down


def _dirs_to_scan(t):
    s = jnp.stack([t[:, :, 0], jnp.flip(t[:, :, 1], axis=1)], axis=0)
    return jnp.moveaxis(s, 2, 0)


def _scan_to_dirs(t):
    t = jnp.moveaxis(t, 0, 2)
    return jnp.stack([t[0], jnp.flip(t[1], axis=1)], axis=2)


def _rwkv_scan(r, decay, k, v, kk, b, s0):
    def step(s, inp):
        r_t, w_t, k_t, v_t, kk_t, b_t = inp
        s_a = jnp.einsum('dbhvk,dbhk->dbhv', s, -kk_t)
        s = s * w_t[..., None, :] + s_a[..., None] * b_t[..., None, :] + v_t[..., None] * k_t[..., None, :]
        return s, jnp.einsum('dbhvk,dbhk->dbhv', s, r_t)
    s_fin, y = lax.scan(step, s0, (r, decay, k, v, kk, b))
    return y, s_fin


def _linear_scan(a, bx, h0):
    def comb(lhs, rhs):
        return lhs[0] * rhs[0], rhs[0] * lhs[1] + rhs[1]
    a_cum, b_cum = lax.associative_scan(comb, (a, bx), axis=0)
    h = a_cum * h0 + b_cum
    return h, h[-1]


def _rwkv_branch(zr, s0, with_output, w0, w_up, a0, a_up, g_up, k_k, k_a, r_k, lnx):
    bsz, t, _ = zr.shape
    f32 = jnp.float32
    r = zr[..., OFF_R:OFF_K]
    k = zr[..., OFF_K:OFF_V]
    v = zr[..., OFF_V:OFF_WD]
    wd = zr[..., OFF_WD:OFF_AD].reshape(bsz, t, 2, DECAY_LORA)
    ad = zr[..., OFF_AD:OFF_GD].reshape(bsz, t, 2, AAA_LORA)
    w_log = -jax.nn.softplus(-(w0 + jnp.einsum('btdr,drc->btdc', jnp.tanh(wd), w_up)).astype(f32)) - 0.5
    decay = jnp.exp(-jnp.exp(w_log))
    a = jax.nn.sigmoid(a0 + jnp.einsum('btdr,drc->btdc', ad, a_up)).astype(f32)
    heads = lambda u: u.reshape(u.shape[:-1] + (RWKV_HEADS, RWKV_HEAD))
    both = lambda u: jnp.broadcast_to(u[:, :, None], (bsz, t, 2) + u.shape[2:])
    kk = heads((k * k_k).astype(f32))
    kk = kk / jnp.maximum(jnp.linalg.norm(kk, axis=-1, keepdims=True), 1e-12)
    k_dir = heads(k[:, :, None].astype(f32) * (1.0 + (a - 1.0) * k_a))
    r_h = heads(r.astype(f32))
    v_h = heads(v.astype(f32))
    if s0 is None:
        s0 = jnp.zeros((2, bsz, RWKV_HEADS, RWKV_HEAD, RWKV_HEAD), f32)
    seq = [both(r_h), heads(decay), k_dir, both(v_h), both(kk), both(kk) * heads(a)]
    y, s_fin = _rwkv_scan(*[_dirs_to_scan(u) for u in seq], s0)
    if not with_output:
        return None, s_fin
    y = _scan_to_dirs(y).sum(axis=2)
    mu = jnp.mean(y, axis=-1, keepdims=True)
    var = jnp.mean(jnp.square(y - mu), axis=-1, keepdims=True)
    y = ((y - mu) * lax.rsqrt(var + LNX_EPS)).reshape(bsz, t, RWKV_WIDTH) * lnx[0] + lnx[1]
    bonus = jnp.sum(r_h[:, :, None] * k_dir * r_k, axis=(2, 4))
    y = y + (bonus[..., None] * v_h).reshape(bsz, t, RWKV_WIDTH)
    g = jax.nn.sigmoid(zr[..., OFF_GD:RWKV_COLS]) @ g_up
    return (y * g).astype(zr.dtype), s_fin


def _rglru_branch(xc, zg, h0, with_output, gate_w, gate_b, lru_l):
    bsz, t, ch = xc.shape
    f32 = jnp.float32
    xb = xc.reshape(bsz, t, LRU_BLOCKS, LRU_BLOCK)
    gates = jax.nn.sigmoid((jnp.einsum('btnj,dgnjk->btdgnk', xb, gate_w) + gate_b).astype(f32))
    gates = gates.reshape(bsz, t, 2, 2, ch)
    log_a = -LRU_C * gates[:, :, :, 0] * jax.nn.softplus(-lru_l.astype(f32))
    bx = jnp.sqrt(-jnp.expm1(2.0 * log_a)) * gates[:, :, :, 1] * xc[:, :, None].astype(f32)
    if h0 is None:
        h0 = jnp.zeros((2, bsz, ch), f32)
    h, h_fin = _linear_scan(_dirs_to_scan(jnp.exp(log_a)), _dirs_to_scan(bx), h0)
    if not with_output:
        return None, h_fin
    h = _scan_to_dirs(h).sum(axis=2)
    return (jax.nn.gelu(zg.astype(f32)) * h).astype(xc.dtype), h_fin


def _token_mixers(z, rows, s_rwkv0, h_lru0, with_output, shift_mu, rw_w0, rw_w_up, rw_a0, rw_a_up,
                  rw_g_up, rw_k_k, rw_k_a, rw_r_k, rw_lnx, lru_conv_w, lru_conv_b, lru_gate_w,
                  lru_gate_b, lru_l, w_branch_a, w_branch_b, w_out):
    bsz, t, _ = z.shape
    zr = z[..., :RWKV_COLS]
    zx = z[..., OFF_LX:OFF_LG]
    zg = None
    if rows is None:
        zr = _shift_centred(zr, shift_mu)
        xc = _dwconv(zx, lru_conv_w, lru_conv_b)
        if with_output:
            zg = z[..., OFF_LG:OFF_GATE]
    else:
        zr = _shift_centred(zr.reshape(bsz, rows, GRID_W, RWKV_COLS), shift_mu).reshape(bsz, t, RWKV_COLS)
        to_cols = lambda u: jnp.swapaxes(u.reshape(bsz, rows, GRID_W, u.shape[-1]), 1, 2)
        xc = _dwconv(to_cols(zx), lru_conv_w, lru_conv_b).reshape(bsz, t, LRU_WIDTH)
        zg = to_cols(z[..., OFF_LG:OFF_GATE]).reshape(bsz, t, LRU_WIDTH)
    y_a, s_rwkv = _rwkv_branch(zr, s_rwkv0, with_output, rw_w0, rw_w_up, rw_a0, rw_a_up, rw_g_up,
                               rw_k_k, rw_k_a, rw_r_k, rw_lnx)
    y_b, h_lru = _rglru_branch(xc, zg, h_lru0, with_output, lru_gate_w, lru_gate_b, lru_l)
    if not with_output:
        return None, s_rwkv, h_lru
    if rows is not None:
        y_b = jnp.swapaxes(y_b.reshape(bsz, GRID_W, rows, LRU_WIDTH), 1, 2).reshape(bsz, t, LRU_WIDTH)
    gates = jax.nn.sigmoid(z[..., OFF_GATE:IN_COLS])
    mixed = gates[..., :D_MODEL] * (y_a @ w_branch_a) + gates[..., D_MODEL:] * (y_b @ w_branch_b)
    return mixed @ w_out, s_rwkv, h_lru


def _grouped_experts(x2, top_idx, gate, w_gu, w_down):
    m, d = x2.shape
    n_assign = m * TOP_K
    flat_e = top_idx.reshape(-1)
    flat_tok = jnp.arange(n_assign, dtype=jnp.int32) // TOP_K
    order = jnp.argsort(flat_e)
    sorted_e = flat_e[order]
    counts = jnp.bincount(flat_e, length=N_EXPERTS)
    padded = (counts + MOE_BLOCK - 1) // MOE_BLOCK * MOE_BLOCK
    pad_end = jnp.cumsum(padded)
    pad_start = pad_end - padded
    start = jnp.cumsum(counts) - counts
    dest = pad_start[sorted_e] + jnp.arange(n_assign, dtype=jnp.int32) - start[sorted_e]
    n_blocks = (n_assign + N_EXPERTS * (MOE_BLOCK - 1) + MOE_BLOCK - 1) // MOE_BLOCK
    p_rows = n_blocks * MOE_BLOCK
    buf_tok = jnp.full((p_rows,), m, jnp.int32).at[dest].set(flat_tok[order])
    buf_gate = jnp.zeros((p_rows,), gate.dtype).at[dest].set(gate.reshape(-1)[order])
    blk_e = jnp.minimum(jnp.searchsorted(pad_end, jnp.arange(n_blocks) * MOE_BLOCK, side='right'),
                        N_EXPERTS - 1)
    x_pad = jnp.concatenate([x2, jnp.zeros((1, d), x2.dtype)], axis=0)

    def block(args):
        tok, g, e = args
        return _swiglu(x_pad[tok], w_gu[e], w_down[e]) * g[:, None]

    y = lax.map(block, (buf_tok.reshape(n_blocks, MOE_BLOCK), buf_gate.reshape(n_blocks, MOE_BLOCK), blk_e))
    out = jnp.zeros((m + 1, d), y.dtype).at[buf_tok].add(y.reshape(p_rows, d))
    return out[:m]


def _moe_ffn(u, router_w, router_b, ex_w_gu, ex_w_down, sh_w_gu, sh_w_down):
    shape = u.shape
    x2 = u.reshape(-1, D_MODEL)
    m = x2.shape[0]
    scores = jax.nn.sigmoid((x2 @ router_w).astype(jnp.float32))
    biased = scores + router_b
    grp_score = lax.top_k(biased.reshape(m, N_GROUPS, N_EXPERTS // N_GROUPS), 2)[0].sum(-1)
    _, grp_idx = lax.top_k(grp_score, TOPK_GROUPS)
    grp_mask = jnp.any(grp_idx[:, :, None] == jnp.arange(N_GROUPS)[None, None, :], axis=1)
    masked = jnp.where(jnp.repeat(grp_mask, N_EXPERTS // N_GROUPS, axis=1), biased, -jnp.inf)
    _, top_idx = lax.top_k(masked, TOP_K)
    top_s = jnp.take_along_axis(scores, top_idx, axis=1)
    gate = (top_s / jnp.sum(top_s, axis=-1, keepdims=True) * ROUTED_SCALE).astype(u.dtype)
    routed = _grouped_experts(x2, top_idx, gate, ex_w_gu, ex_w_down)
    return (routed + _swiglu(x2, sh_w_gu, sh_w_down)).reshape(shape)


def setup_inputs(seed: int = 0) -> dict:
    key = jax.random.key(seed)
    ks = iter(jax.random.split(key, 48))
    f32 = jnp.float32
    nrm = lambda shape, s: jax.random.normal(next(ks), shape, f32) * s
    uni = lambda shape, lo, hi: jax.random.uniform(next(ks), shape, f32, lo, hi)
    d = D_MODEL
    x = nrm((BATCH, SEQ, d), 1.0)
    c = nrm((BATCH, d), 1.0)
    ctx = nrm((BATCH, CTX_LEN, d), 1.0)
    c_ctx = nrm((d,), 1.0)
    w_mod = nrm((DEPTH, d, 6 * d), 0.5 * d ** -0.5)
    b_mod = nrm((DEPTH, 6 * d), 0.02)
    norm_g = 1.0 + nrm((DEPTH, 4, d), 0.05)
    w_in = nrm((DEPTH, d, IN_COLS), d ** -0.5)
    shift_mu = uni((DEPTH, 2, RWKV_COLS), 0.0, 0.5)
    rw_w0 = uni((DEPTH, 2, RWKV_WIDTH), -6.0, -1.0)
    rw_w_up = nrm((DEPTH, 2, DECAY_LORA, RWKV_WIDTH), 0.1 * DECAY_LORA ** -0.5)
    rw_a0 = nrm((DEPTH, 2, RWKV_WIDTH), 0.1)
    rw_a_up = nrm((DEPTH, 2, AAA_LORA, RWKV_WIDTH), 0.5 * AAA_LORA ** -0.5)
    rw_g_up = nrm((DEPTH, GATE_LORA, RWKV_WIDTH), GATE_LORA ** -0.5)
    rw_k_k = 0.85 + nrm((DEPTH, RWKV_WIDTH), 0.05)
    rw_k_a = 1.0 + nrm((DEPTH, RWKV_WIDTH), 0.05)
    rw_r_k = nrm((DEPTH, RWKV_HEADS, RWKV_HEAD), 0.1)
    rw_lnx = jnp.stack([1.0 + nrm((DEPTH, RWKV_WIDTH), 0.05), nrm((DEPTH, RWKV_WIDTH), 0.02)], axis=1)
    lru_conv_w = nrm((DEPTH, CONV_W, LRU_WIDTH), 0.5)
    lru_conv_b = nrm((DEPTH, LRU_WIDTH), 0.02)
    lru_gate_w = nrm((DEPTH, 2, 2, LRU_BLOCKS, LRU_BLOCK, LRU_BLOCK), LRU_BLOCK ** -0.5)
    lru_gate_b = nrm((DEPTH, 2, 2, LRU_BLOCKS, LRU_BLOCK), 0.02)
    sig = uni((DEPTH, 2, LRU_WIDTH), 0.9, 0.999) ** (1.0 / LRU_C)
    lru_l = jnp.log(sig) - jnp.log1p(-sig)
    w_branch_a = nrm((DEPTH, RWKV_WIDTH, d), RWKV_WIDTH ** -0.5)
    w_branch_b = nrm((DEPTH, LRU_WIDTH, d), LRU_WIDTH ** -0.5)
    w_out = nrm((DEPTH, d, d), d ** -0.5)
    router_w = nrm((DEPTH, d, N_EXPERTS), d ** -0.5)
    router_b = nrm((DEPTH, N_EXPERTS), 0.01)
    ex_w_gu = nrm((DEPTH, N_EXPERTS, d, 2 * EXPERT_FF), d ** -0.5)
    ex_w_down = nrm((DEPTH, N_EXPERTS, EXPERT_FF, d), EXPERT_FF ** -0.5)
    sh_w_gu = nrm((DEPTH, d, 2 * SHARED_FF), d ** -0.5)
    sh_w_down = nrm((DEPTH, SHARED_FF, d), SHARED_FF ** -0.5)
    return {'x': x, 'c': c, 'ctx': ctx, 'c_ctx': c_ctx, 'w_mod': w_mod, 'b_mod': b_mod,
            'norm_g': norm_g, 'w_in': w_in, 'shift_mu': shift_mu, 'rw_w0': rw_w0, 'rw_w_up': rw_w_up,
            'rw_a0': rw_a0, 'rw_a_up': rw_a_up, 'rw_g_up': rw_g_up, 'rw_k_k': rw_k_k, 'rw_k_a': rw_k_a,
            'rw_r_k': rw_r_k, 'rw_lnx': rw_lnx, 'lru_conv_w': lru_conv_w, 'lru_conv_b': lru_conv_b,
            'lru_gate_w': lru_gate_w, 'lru_gate_b': lru_gate_b, 'lru_l': lru_l,
            'w_branch_a': w_branch_a, 'w_branch_b': w_branch_b, 'w_out': w_out,
            'router_w': router_w, 'router_b': router_b, 'ex_w_gu': ex_w_gu, 'ex_w_down': ex_w_down,
            'sh_w_gu': sh_w_gu, 'sh_w_down': sh_w_down}


def reference(x, c, ctx, c_ctx, w_mod, b_mod, norm_g, w_in, shift_mu, rw_w0, rw_w_up, rw_a0, rw_a_up,
              rw_g_up, rw_k_k, rw_k_a, rw_r_k, rw_lnx, lru_conv_w, lru_conv_b, lru_gate_w, lru_gate_b,
              lru_l, w_branch_a, w_branch_b, w_out, router_w, router_b, ex_w_gu, ex_w_down,
              sh_w_gu, sh_w_down):
    rows = x.shape[1] // GRID_W
    h = x
    hc = ctx
    for l in range(DEPTH):
        last = l == DEPTH - 1
        mod = (jax.nn.silu(c) @ w_mod[l] + b_mod[l])[:, None, :]
        mod_c = jax.nn.silu(c_ctx) @ w_mod[l] + b_mod[l]
        sh1, sc1, gt1, sh2, sc2, gt2 = jnp.split(mod, 6, axis=-1)
        csh1, csc1, cgt1, csh2, csc2, cgt2 = jnp.split(mod_c, 6, axis=-1)
        g_pre1, g_post1, g_pre2, g_post2 = norm_g[l]
        mix_p = (shift_mu[l], rw_w0[l], rw_w_up[l], rw_a0[l], rw_a_up[l], rw_g_up[l], rw_k_k[l],
                 rw_k_a[l], rw_r_k[l], rw_lnx[l], lru_conv_w[l], lru_conv_b[l], lru_gate_w[l],
                 lru_gate_b[l], lru_l[l], w_branch_a[l], w_branch_b[l], w_out[l])
        ffn_p = (router_w[l], router_b[l], ex_w_gu[l], ex_w_down[l], sh_w_gu[l], sh_w_down[l])
        zc = _modulate(_rmsnorm(hc, g_pre1), csh1, csc1) @ w_in[l]
        yc, s_rwkv, h_lru = _token_mixers(zc, None, None, None, not last, *mix_p)
        z = _modulate(_rmsnorm(h, g_pre1), sh1, sc1) @ w_in[l]
        y, _, _ = _token_mixers(z, rows, s_rwkv, h_lru, True, *mix_p)
        h = h + gt1 * _rmsnorm(y, g_post1)
        u = _modulate(_rmsnorm(h, g_pre2), sh2, sc2)
        h = h + gt2 * _rmsnorm(_moe_ffn(u, *ffn_p), g_post2)
        if not last:
            hc = hc + cgt1 * _rmsnorm(yc, g_post1)
            uc = _modulate(_rmsnorm(hc, g_pre2), csh2, csc2)
            hc = hc + cgt2 * _rmsnorm(_moe_ffn(uc, *ffn_p), g_post2)
    return h
```

```python
import numpy as np
import os
from contextlib import ExitStack
DBGSKIP = os.environ.get('DBGSKIP', '')
MOESKIP = os.environ.get('MOESKIP', '')
LOWP_FROM = int(os.environ.get('LOWP_FROM', '6'))
import concourse.bass as bass
import concourse.mybir as mybir
from concourse.bass_utils import run_bass_kernel_spmd

F32 = mybir.dt.float32
BF16 = mybir.dt.bfloat16
AF = mybir.ActivationFunctionType
ALU = mybir.AluOpType
AX = mybir.AxisListType

D = 1024
T_CTX = 256
T_LAT = 4096
T_ALL = T_CTX + T_LAT
NCH = T_ALL // 128
IN_COLS = 7584
OFF_WD, OFF_AD, OFF_GD, OFF_LX, OFF_LG, OFF_GA, OFF_GB = 3072, 3200, 3328, 3488, 4512, 5536, 6560
NEXP = 64
TT = [(0, 256, True)] + [(256 + i * 512, 512, False) for i in range(8)]


class _Rec:
    def __getattr__(self, name):
        def f(*a, **k):
            return (name, a, k)
        return f


_REC = _Rec()


class Sched:
    ENG = ['pe', 'act', 'dve', 'pool', 'sp']

    def __init__(self, nc, es, ndma=16):
        self.nc = nc
        self.ops = {e: [] for e in self.ENG}
        self.cnt = {e: 0 for e in self.ENG}
        self.sem = {e: es.enter_context(nc.semaphore('s_' + e)) for e in self.ENG}
        self.dsem = {e: [es.enter_context(nc.semaphore('d_%s%d' % (e, i))) for i in range(ndma)]
                     for e in ('sp', 'act')}
        self.dcnt = {e: 0 for e in self.dsem}
        self.dval = {e: [0] * ndma for e in self.dsem}
        self.lastw = {}
        self.readers = {}
        self.waited = {e: {} for e in self.ENG}
        self.ndma = ndma

    def _deps(self, eng, r, w, is_dma):
        deps = []
        for x in r:
            lw = self.lastw.get(x)
            if lw is not None:
                deps.append(lw)
        for x in w:
            lw = self.lastw.get(x)
            if lw is not None and (is_dma or lw[0] != eng or lw[3]):
                deps.append(lw)
            for rd in self.readers.get(x, ()):
                if is_dma or rd[0] != eng or rd[3]:
                    deps.append(rd)
        out = []
        wd = self.waited[eng]
        for (pe_, sem, val, pdma) in deps:
            if pe_ == 'pe' and eng == 'pe' and not pdma and not is_dma:
                continue
            k = id(sem)
            if wd.get(k, 0) >= val:
                continue
            wd[k] = val
            out.append((sem, val))
        return out

    def _commit(self, tok, r, w):
        for x in r:
            self.readers.setdefault(x, []).append(tok)
        for x in w:
            self.lastw[x] = tok
            self.readers[x] = []

    def op(self, eng, fn, r=(), w=()):
        waits = self._deps(eng, r, w, False)
        self.cnt[eng] += 1
        tok = (eng, self.sem[eng], self.cnt[eng], False)
        self.ops[eng].append((fn(_REC), waits, self.sem[eng], 1))
        self._commit(tok, r, w)

    def dma(self, q, out, in_, r=(), w=(), **kw):
        waits = self._deps(q, r, w, True)
        i = self.dcnt[q]
        self.dcnt[q] += 1
        slot = i % self.ndma
        sem = self.dsem[q][slot]
        prev = self.dval[q][slot]
        if prev > 0 and self.waited[q].get(id(sem), 0) < prev:
            self.waited[q][id(sem)] = prev
            waits.append((sem, prev))
        self.dval[q][slot] = prev + 16
        tok = (q, sem, prev + 16, True)
        self.ops[q].append((('dma_start', (), dict(out=out, in_=in_, **kw)), waits, sem, 16))
        self._commit(tok, r, w)
        return tok

    def wait_all(self, eng, toks):
        self.ops[eng].append((None, [(t[1], t[2]) for t in toks], None, 0))

    def barrier(self):
        allw = [(self.sem[e], self.cnt[e]) for e in self.ENG if self.cnt[e] > 0]
        for q in self.dsem:
            for i in range(self.ndma):
                if self.dval[q][i] > 0:
                    allw.append((self.dsem[q][i], self.dval[q][i]))
        for e in self.ENG:
            waits = []
            for (sem, val) in allw:
                if self.waited[e].get(id(sem), 0) < val:
                    self.waited[e][id(sem)] = val
                    waits.append((sem, val))
            self.ops[e].append((None, waits, None, 0))

    def emit(self):
        with self.nc.Block() as block:
            def run(e, name):
                for call, waits, sem, inc in self.ops[name]:
                    for (s, v) in waits:
                        e.wait_ge(s, v)
                    if call is not None:
                        getattr(e, call[0])(*call[1], **call[2]).then_inc(sem, inc)
                self.ops[name] = []

            @block.tensor
            def _(e):
                run(e, 'pe')

            @block.scalar
            def _(e):
                run(e, 'act')

            @block.vector
            def _(e):
                run(e, 'dve')

            @block.gpsimd
            def _(e):
                run(e, 'pool')

            @block.sync
            def _(e):
                run(e, 'sp')


def build(stage='full', nhp=8):
    nc = bass.Bass("TRN2", target_bir_lowering=False)
    di = lambda name, shape, dt=F32: nc.dram_tensor(name, list(shape), dt, kind="ExternalInput").ap()
    x_d = di('x', [T_LAT, D]); ctx_d = di('ctx', [T_CTX, D])
    cvec_d = di('cvec', [128, 8, 2]); wmod_d = di('w_mod', [D, 6 * D]); bmod_d = di('b_modT', [128, 48])
    bmodbc_d = di('b_mod_bc', [128, 2, D])
    ngT_d = di('norm_gT', [128, 4, 8]); gpost_d = di('gpost_bc', [128, 2, D])
    win_d = di('w_in', [D, IN_COLS]); mu_d = di('muT', [128, 28, 2])
    w0_d = di('w0T', [128, 8, 2]); a0_d = di('a0T', [128, 8, 2]); kk_d = di('kkT', [128, 8]); ka_d = di('kaT', [128, 8])
    rk_d = di('rkT', [128, 8]); lnx_d = di('lnx_bc', [128, 2, D])
    wup_d = di('wupT', [128, D]); aup_d = di('aupT', [128, D]); gup_d = di('g_up', [160, D])
    cw_d = di('cwT', [128, 8, 4]); cb_d = di('cbT', [128, 8]); gb_d = di('gbT', [128, 8, 4]); ll_d = di('llT', [128, 8, 2])
    gw_d = di('gwT', [128, 8, 4, 64])
    wa_d = di('w_branch_a', [D, D]); wb_d = di('w_branch_b', [D, D]); wo_d = di('w_out', [D, D])
    rw_d = di('router_w', [D, NEXP]); rb_d = di('router_b_bc', [128, NEXP])
    egu_d = di('ex_w_gu', [NEXP, D, 512]); edn_d = di('ex_w_down', [NEXP, 256, D])
    sgu_d = di('sh_w_gu', [D, 512]); sdn_d = di('sh_w_down', [256, D])
    cst_d = di('consts', [128, 7, 128])
    out_d = nc.dram_tensor('out', [T_LAT, D], F32, kind="ExternalOutput").ap()
    ya_s = nc.dram_tensor('ya_s', [8, 128, T_LAT], BF16, kind="Internal").ap()
    yb_s = nc.dram_tensor('yb_s', [8, 128, T_LAT], BF16, kind="Internal").ap()
    h1_s = nc.dram_tensor('h1_s', [T_LAT, D], F32, kind="Internal").ap()
    uT_s = nc.dram_tensor('uT_s', [128, 8, T_LAT], BF16, kind="Internal").ap()
    xn_s = nc.dram_tensor('xn_s', [9, 128, 8 * 512], BF16, kind="Internal").ap()
    xnv = lambda ti: xn_s[ti].rearrange("p (k t) -> p k t", t=512)
    sgd_s = nc.dram_tensor('sgd_s', [2, 128, T_ALL], BF16, kind="Internal").ap()
    gt_s = nc.dram_tensor('gt_s', [128, 2, D], F32, kind="Internal").ap()
    dbg_d = None
    if stage != 'full':
        dbg_d = nc.dram_tensor('dbg', [T_LAT, D], F32, kind="ExternalOutput").ap()

    with ExitStack() as es:
        S = Sched(nc, es)

        def T(st, name, shape, dt):
            return st.enter_context(nc.sbuf_tensor('t_' + name, list(shape), dt))

        open_stacks = []

        def finish_early(src_ap, rows, cols, rname):
            ps = ExitStack(); open_stacks.append(ps)
            if True:
                df_ = T(ps, 'dbg_f', [128, 8], F32)
                S.op('pool', lambda e: e.memset(df_[:], 0.0), w=['dbg_f'])
                toks = [S.dma('sp', dbg_d[0:rows, 0:cols], src_ap, r=[rname], w=['dbg']),
                        S.dma('sp', out_d[0:128, 0:8], df_[:], r=['dbg_f'], w=['out'])]
                S.wait_all('sp', toks)
                S.emit()
            for st_ in reversed(open_stacks):
                st_.close()
            return nc

        def finish_early4(ys):
            ps = ExitStack(); open_stacks.append(ps)
            df_ = T(ps, 'dbg_f', [128, 8], F32)
            S.op('pool', lambda e: e.memset(df_[:], 0.0), w=['dbg_f'])
            toks = [S.dma('sp', dbg_d[0:128, :], ys[:, 0:8, :].rearrange("p a b -> p (a b)"), r=['ysum'], w=['dbg']),
                    S.dma('sp', dbg_d[128:256, :], ys[:, 8:16, :].rearrange("p a b -> p (a b)"), r=['ysum'], w=['dbg']),
                    S.dma('sp', dbg_d[256:384, :], ys[:, 16:24, :].rearrange("p a b -> p (a b)"), r=['ysum'], w=['dbg']),
                    S.dma('sp', dbg_d[384:512, :], ys[:, 24:32, :].rearrange("p a b -> p (a b)"), r=['ysum'], w=['dbg']),
                    S.dma('sp', out_d[0:128, 0:8], df_[:], r=['dbg_f'], w=['out'])]
            S.wait_all('sp', toks)
            S.emit()
            for st_ in reversed(open_stacks):
                st_.close()
            return nc

        def finish_rows(tile_):
            ps = ExitStack(); open_stacks.append(ps)
            df_ = T(ps, 'dbg_f', [128, 8], F32)
            S.op('pool', lambda e: e.memset(df_[:], 0.0), w=['dbg_f'])
            toks = [S.dma('sp', dbg_d.rearrange("(a b) d -> a (b d)", b=4)[0:128, :], tile_[:, 0:T_LAT], r=['hh0'], w=['dbg']),
                    S.dma('sp', out_d[0:128, 0:8], df_[:], r=['dbg_f'], w=['out'])]
            S.wait_all('sp', toks); S.emit()
            for st_ in reversed(open_stacks):
                st_.close()
            return nc

        def finish_rows2(h1s, gts):
            ps = ExitStack(); open_stacks.append(ps)
            df_ = T(ps, 'dbg_f', [128, D], F32)
            S.dma('sp', df_[:], h1s[0:128, :], r=['h1_s'], w=['dbg_f'])
            toks = [S.dma('sp', dbg_d[0:128, :], df_[:], r=['dbg_f'], w=['dbg']),
                    S.dma('sp', dbg_d[128:256, 0:256], gts[:, 0:4, :].rearrange("p a b -> p (a b)"), r=['gates'], w=['dbg']),
                    S.dma('sp', out_d[0:128, 0:8], df_[:, 0:8], r=['dbg_f'], w=['out'])]
            S.wait_all('sp', toks); S.emit()
            for st_ in reversed(open_stacks):
                st_.close()
            return nc

        PB = [es.enter_context(nc.psum_tensor('pb%d' % i, [128, 512], F32)) for i in range(7)]
        PT = es.enter_context(nc.psum_tensor('pbT', [128, 1024], BF16))

        cst = T(es, 'cst', [128, 7, 128], F32)
        cstb = T(es, 'cstb', [128, 7, 128], BF16)
        S.dma('sp', cst[:], cst_d[:, :, :], w=['cst'])
        S.op('dve', lambda e: e.tensor_copy(out=cstb[:], in_=cst[:]), r=['cst'], w=['cstb'])
        identb = cstb[:, 0, :]; identf = cst[:, 0, :]
        MASK = {'SU': cst[:, 1, :], 'IU': cst[:, 2, :], 'SL': cst[:, 3, :], 'IL': cst[:, 4, :]}
        bonesb = cstb[:, 5, :]; onesf = cst[:, 6, :]
        mG = T(es, 'mG', [128, 2, 512], BF16)
        for d_, (s_, i_) in enumerate([('SU', 'IU'), ('SL', 'IL')]):
            for q_, nm in enumerate([s_, i_, s_, i_]):
                S.op('pool', lambda e, d_=d_, q_=q_, nm=nm: e.tensor_copy(out=mG[:, d_, q_ * 128:(q_ + 1) * 128], in_=MASK[nm]),
                     r=['cst'], w=['mG'])
        mN = [MASK['SL'], MASK['SU']]

        def small(name, src, shape):
            t = T(es, name, shape, F32)
            S.dma('sp', t[:], src, w=[name])
            return t
        cvec = small('cvec', cvec_d[:, :, :], [128, 8, 2])
        bmodT = small('bmodT', bmod_d[:, :], [128, 48])
        ngT = small('ngT', ngT_d[:, :, :], [128, 4, 8])
        muT = small('muT', mu_d[:, :, :], [128, 28, 2])
        w0T = small('w0T', w0_d[:, :, :], [128, 8, 2]); a0T = small('a0T', a0_d[:, :, :], [128, 8, 2])
        kkT = small('kkT', kk_d[:, :], [128, 8]); kaT = small('kaT', ka_d[:, :], [128, 8]); rkT = small('rkT', rk_d[:, :], [128, 8])
        cwT = small('cwT', cw_d[:, :, :], [128, 8, 4]); cbT = small('cbT', cb_d[:, :], [128, 8])
        gbT = small('gbT', gb_d[:, :, :], [128, 8, 4]); llT = small('llT', ll_d[:, :, :], [128, 8, 2])
        SM = ['cvec', 'bmodT', 'ngT', 'muT', 'w0T', 'a0T', 'kkT', 'kaT', 'rkT', 'cwT', 'cbT', 'gbT', 'llT']

        cmu = T(es, 'cmu', [128, 28], F32)
        S.op('dve', lambda e: e.tensor_tensor(out=cmu[:], in0=muT[:, :, 0], in1=muT[:, :, 1], op=ALU.add), r=['muT'], w=['cmu'])
        S.op('dve', lambda e: e.tensor_scalar(out=cmu[:], in0=cmu[:], scalar1=-1.0, scalar2=1.0, op0=ALU.mult, op1=ALU.add), r=['cmu'], w=['cmu'])
        nw0 = T(es, 'nw0', [128, 8, 2], F32)
        S.op('dve', lambda e: e.tensor_scalar(out=nw0[:], in0=w0T[:], scalar1=-1.0, scalar2=None, op0=ALU.mult), r=['w0T'], w=['nw0'])
        oka = T(es, 'oka', [128, 8], F32)
        S.op('dve', lambda e: e.tensor_scalar(out=oka[:], in0=kaT[:], scalar1=-1.0, scalar2=1.0, op0=ALU.mult, op1=ALU.add), r=['kaT'], w=['oka'])
        nsp = T(es, 'nsp', [128, 8, 2], F32)
        S.op('act', lambda e: e.activation(out=nsp[:], in_=llT[:], func=AF.Softplus, scale=-1.0), r=['llT'], w=['nsp'])
        S.op('dve', lambda e: e.tensor_scalar(out=nsp[:], in0=nsp[:], scalar1=-8.0, scalar2=None, op0=ALU.mult), r=['nsp'], w=['nsp'])
        nsp2 = T(es, 'nsp2', [128, 8, 2], F32)
        S.op('dve', lambda e: e.tensor_scalar(out=nsp2[:], in0=nsp[:], scalar1=2.0, scalar2=None, op0=ALU.mult), r=['nsp'], w=['nsp2'])

        scT = T(es, 'scT', [128, 8, 2], F32)
        S.op('act', lambda e: e.activation(out=scT[:], in_=cvec[:], func=AF.Silu), r=['cvec'], w=['scT'])
        modT = T(es, 'modT', [128, 48, 2], F32)
        with ExitStack() as ps:
            gt_bc = T(ps, 'gt_bc', [128, 2, D], F32)
            scbc = T(ps, 'scbc', [128, 8, 128], F32)
            for k in range(8):
                S.op('act', lambda e, k=k: e.activation(out=scbc[:, k, :], in_=onesf, func=AF.Identity, scale=scT[:, k, 0:1]),
                     r=['cst', 'scT'], w=['scbc'])
            wms = [T(ps, 'wms%d' % i, [128, 8, 512], F32) for i in range(2)]
            wmv = wmod_d.rearrange("(k p) c -> p k c", p=128)
            for g in range(12):
                wm = wms[g % 2]; wn = 'wms%d' % (g % 2)
                S.dma('sp', wm[:], wmv[:, :, g * 512:(g + 1) * 512], w=[wn])
                bank = PB[g % 2]; bn = 'pb%d' % (g % 2)
                for sub in range(4):
                    j = g * 4 + sub
                    for k in range(8):
                        S.op('pe', lambda e, wm=wm, sub=sub, k=k, bank=bank: e.matmul(
                            bank[:, sub * 2:sub * 2 + 2], lhsT=wm[:, k, sub * 128:(sub + 1) * 128], rhs=scT[:, k, :],
                            start=(k == 0), stop=(k == 7)), r=[wn, 'scT'], w=[bn])
                    S.op('dve', lambda e, j=j, sub=sub, bank=bank: e.tensor_scalar(
                        out=modT[:, j, :], in0=bank[:, sub * 2:sub * 2 + 2], scalar1=bmodT[:, j:j + 1], scalar2=None, op0=ALU.add),
                        r=[bn, 'bmodT'], w=['modT'])
                if g // 2 in (2, 5):
                    which = 0 if g < 6 else 1
                    half = g % 2
                    bk = PB[2 + half]; bkn = 'pb%d' % (2 + half)
                    for k in range(8):
                        S.op('pe', lambda e, wm=wm, k=k, bk=bk: e.matmul(bk[:, :], lhsT=scbc[:, k, :], rhs=wm[:, k, :],
                                                                         start=(k == 0), stop=(k == 7)), r=[wn, 'scbc'], w=[bkn])
                    S.op('act', lambda e, which=which, half=half, bk=bk: e.activation(
                        out=gt_bc[:, which, half * 512:(half + 1) * 512], in_=bk[:, :], func=AF.Copy), r=[bkn], w=['gt_bc'])
            bmbc = T(ps, 'bmbc', [128, 2, D], F32); gpbc = T(ps, 'gpbc', [128, 2, D], F32)
            S.dma('sp', bmbc[:], bmodbc_d[:, :, :], w=['bmbc']); S.dma('sp', gpbc[:], gpost_d[:, :, :], w=['gpbc'])
            S.op('dve', lambda e: e.tensor_tensor(out=gt_bc[:], in0=gt_bc[:], in1=bmbc[:], op=ALU.add), r=['gt_bc', 'bmbc'], w=['gt_bc'])
            S.op('dve', lambda e: e.tensor_tensor(out=gt_bc[:], in0=gt_bc[:], in1=gpbc[:], op=ALU.mult), r=['gt_bc', 'gpbc'], w=['gt_bc'])
            S.dma('sp', gt_s[:, :, :], gt_bc[:], r=['gt_bc'], w=['gt_s'])
            S.barrier(); S.emit()
        gs1 = T(es, 'gs1', [128, 8, 2], F32); sh1 = T(es, 'sh1', [128, 8, 2], F32)
        gs2 = T(es, 'gs2', [128, 8], F32); sh2 = T(es, 'sh2', [128, 8], F32)
        for ci in range(2):
            S.op('dve', lambda e, ci=ci: e.scalar_tensor_tensor(out=gs1[:, :, ci], in0=modT[:, 8:16, ci], scalar=1.0, in1=ngT[:, 0, :],
                                                                 op0=ALU.add, op1=ALU.mult), r=['modT', 'ngT'], w=['gs1'])
            S.op('dve', lambda e, ci=ci: e.tensor_copy(out=sh1[:, :, ci], in_=modT[:, 0:8, ci]), r=['modT'], w=['sh1'])
        S.op('dve', lambda e: e.scalar_tensor_tensor(out=gs2[:], in0=modT[:, 32:40, 0], scalar=1.0, in1=ngT[:, 2, :],
                                                     op0=ALU.add, op1=ALU.mult), r=['modT', 'ngT'], w=['gs2'])
        S.op('dve', lambda e: e.tensor_copy(out=sh2[:], in_=modT[:, 24:32, 0]), r=['modT'], w=['sh2'])

        if stage == 'p1':
            return finish_early(modT[:].rearrange("p a b -> p (a b)"), 128, 96, 'modT')
        def norm_to_featT(st, xt, xtn, dstT, dstn, col0, gs_ap, sh_ap, tag):
            ss = st['ss']; xs = st['xs']
            S.op('act', lambda e: e.activation(out=st['junk'][:], in_=xt, func=AF.Square, accum_out=ss[:, 0:1]),
                 r=[xtn], w=[tag + 'junk', tag + 'ss'])
            S.op('dve', lambda e: e.tensor_scalar(out=ss[:, 1:2], in0=ss[:, 0:1], scalar1=1.0 / D, scalar2=1e-6, op0=ALU.mult, op1=ALU.add),
                 r=[tag + 'ss'], w=[tag + 'ss1'])
            S.op('act', lambda e: e.activation(out=ss[:, 2:3], in_=ss[:, 1:2], func=AF.Sqrt), r=[tag + 'ss1'], w=[tag + 'ss2'])
            S.op('dve', lambda e: e.reciprocal(out=ss[:, 3:4], in_=ss[:, 2:3]), r=[tag + 'ss2'], w=[tag + 'ss3'])
            S.op('dve', lambda e: e.tensor_scalar(out=xs[:], in0=xt, scalar1=ss[:, 3:4], scalar2=None, op0=ALU.mult),
                 r=[xtn, tag + 'ss3'], w=[tag + 'xs'])
            for k in range(8):
                S.op('pe', lambda e, k=k: e.transpose(out=PT[:, k * 128:(k + 1) * 128], in_=xs[:, k * 128:(k + 1) * 128], identity=identb),
                     r=[tag + 'xs', 'cstb'], w=['pbT'])
            for k in range(8):
                S.op('act', lambda e, k=k: e.activation(out=dstT[:, k, col0:col0 + 128], in_=PT[:, k * 128:(k + 1) * 128], func=AF.Identity,
                                                        scale=gs_ap(k), bias=sh_ap(k)), r=['pbT', 'gs1', 'sh1', 'gs2', 'sh2'], w=[dstn])

        xnt = [T(es, 'xnt%d' % i, [128, 8, 512], BF16) for i in range(2)]
        with ExitStack() as ps:
            xin = [T(ps, 'xin%d' % i, [128, D], F32) for i in range(2)]
            st = {'ss': T(ps, 'n_ss', [128, 4], F32), 'xs': T(ps, 'n_xs', [128, D], BF16), 'junk': T(ps, 'n_junk', [128, D], BF16)}
            for tti, (g0, n, is_ctx) in enumerate(TT):
                stg = xnt[tti % 2]; sn = 'xnt%d' % (tti % 2) + 'a'
                for j in range(n // 128):
                    ti = g0 // 128 + j
                    xt = xin[ti % 2]; xn_ = 'xin%d' % (ti % 2)
                    src = ctx_d[ti * 128:(ti + 1) * 128, :] if ti < 2 else x_d[(ti - 2) * 128:(ti - 1) * 128, :]
                    S.dma('sp', xt[:], src, w=[xn_])
                    ci = 1 if ti < 2 else 0
                    norm_to_featT(st, xt[:], xn_, stg, sn, j * 128,
                                  lambda k, ci=ci: gs1[:, k, ci:ci + 1], lambda k, ci=ci: sh1[:, k, ci:ci + 1], 'n_')
                S.dma('sp', xnv(tti)[:, :, 0:n], stg[:, :, 0:n], r=[sn], w=['xn_s'])
            S.barrier(); S.emit()

        if stage == 'p2':
            xf_ = T(es, 'xf_dbg', [128, 512], F32)
            S.dma('sp', xnt[0][:, :, 0:512], xnv(1)[:, :, 0:512], r=['xn_s'], w=['xnt0a', 'xnt0b'])
            S.op('dve', lambda e: e.tensor_copy(out=xf_[:], in_=xnt[0][:, 3, :]), r=['xnt0'], w=['xf_dbg'])
            return finish_early(xf_[:], 128, 512, 'xf_dbg')
        winv = win_d.rearrange("(k p) c -> p k c", p=128)
        wst = [T(es, 'wst%d' % i, [128, 8, 128], F32) for i in range(2)]
        wbf = T(es, 'wbf', [128, 8, 512], BF16)
        pj = {'i': 0, 'bank': 0, 'x': 0}

        def project_group(chunks, banks=(4, 5, 6)):
            offs = []
            o = 0
            for (col0, m, _) in chunks:
                i = pj['i']; pj['i'] += 1
                ws = wst[i % 2]
                S.dma('sp', ws[:, :, 0:m], winv[:, :, col0:col0 + m], w=['wst%d' % (i % 2)])
                S.op('pool', lambda e, ws=ws, o=o, m=m: e.tensor_copy(out=wbf[:, :, o:o + m], in_=ws[:, :, 0:m]), r=['wst%d' % (i % 2)], w=['wbf'])
                offs.append(o); o += m
            for ti, (g0, n, is_ctx) in enumerate(TT):
                xi = pj['x'] % 2; pj['x'] += 1
                xt = xnt[xi]; xtn = 'xnt%d' % xi
                S.dma('sp', xt[:, 0:4, 0:n], xnv(ti)[:, 0:4, 0:n], r=['xn_s'], w=[xtn + 'a'])
                S.dma('sp', xt[:, 4:8, 0:n], xnv(ti)[:, 4:8, 0:n], r=['xn_s'], w=[xtn + 'b'])
                for (col0, m, consumer), o in zip(chunks, offs):
                    bi = banks[pj['bank'] % len(banks)]; pj['bank'] += 1
                    bank = PB[bi]; bn = 'pb%d' % bi
                    for k in range(8):
                        S.op('pe', lambda e, k=k, bank=bank, n=n, m=m, o=o, xt=xt: e.matmul(bank[0:m, 0:n], lhsT=wbf[:, k, o:o + m], rhs=xt[:, k, 0:n],
                                                                                          start=(k == 0), stop=(k == 7)),
                             r=['wbf', xtn + ('a' if k < 4 else 'b')], w=[bn])
                    consumer(bank[0:m, 0:n], bn, g0, n, is_ctx)

        sh_t1 = [T(es, 'sh_t1_%d' % i, [128, 512], F32) for i in range(2)]
        shc = {'i': 0}

        def shift_consume(m, ci, final):
            def cons(ps_ap, bn, g0, n, is_ctx):
                i = shc['i']; shc['i'] += 1
                t1 = sh_t1[i % 2]; tn = 'sh_t1_%d' % (i % 2)
                rw = n if is_ctx else 64
                S.op('act', lambda e: e.activation(out=t1[0:m, 0:n], in_=ps_ap, func=AF.Identity, scale=cmu[0:m, ci:ci + 1]),
                     r=[bn, 'cmu'], w=[tn])
                zv = ps_ap.rearrange("p (r c) -> p r c", c=rw)
                tv = t1[0:m, 0:n].rearrange("p (r c) -> p r c", c=rw)
                S.op('dve', lambda e: e.scalar_tensor_tensor(out=tv[:, :, 1:rw], in0=zv[:, :, 0:rw - 1], scalar=muT[0:m, ci, 0:1], in1=tv[:, :, 1:rw],
                                                             op0=ALU.mult, op1=ALU.add), r=[bn, tn, 'muT'], w=[tn])
                S.op('dve', lambda e: e.scalar_tensor_tensor(out=tv[:, :, 0:rw - 1], in0=zv[:, :, 1:rw], scalar=muT[0:m, ci, 1:2], in1=tv[:, :, 0:rw - 1],
                                                             op0=ALU.mult, op1=ALU.add), r=[bn, tn, 'muT'], w=[tn])
                final(t1[0:m, 0:n], tn, g0, n)
            return cons

        rw = ExitStack(); open_stacks.append(rw)
        twd = T(rw, 'twd', [128, T_ALL], BF16); adT = T(rw, 'adT', [128, T_ALL], BF16)
        sgst = [T(rw, 'sgst%d' % i, [128, 512], BF16) for i in range(2)]
        sgt = T(rw, 'sgt', [128, 2, 512], BF16)
        sgc = {'i': 0}

        def sg_final(ch, m):
            def f(t1, tn, g0, n):
                i = sgc['i']; sgc['i'] += 1
                st_ = sgst[i % 2]; sn_ = 'sgst%d' % (i % 2)
                S.op('act', lambda e: e.activation(out=st_[0:m, 0:n], in_=t1, func=AF.Sigmoid), r=[tn], w=[sn_])
                S.dma('sp', sgd_s[ch, 0:m, g0:g0 + n], st_[0:m, 0:n], r=[sn_], w=['sgd_s'])
            return f
        project_group([
            (OFF_WD, 128, shift_consume(128, 24, lambda t1, tn, g0, n: S.op(
                'act', lambda e: e.activation(out=twd[:, g0:g0 + n], in_=t1, func=AF.Tanh), r=[tn], w=['twd']))),
            (OFF_AD, 128, shift_consume(128, 25, lambda t1, tn, g0, n: S.op(
                'pool', lambda e: e.tensor_copy(out=adT[:, g0:g0 + n], in_=t1), r=[tn], w=['adT']))),
            (OFF_GD, 128, shift_consume(128, 26, sg_final(0, 128))),
            (OFF_GD + 128, 32, shift_consume(32, 27, sg_final(1, 32)))])
        if stage == 'p3':
            xf_ = T(rw, 'xf_dbg', [128, 512], F32)
            S.op('dve', lambda e: e.tensor_copy(out=xf_[:], in_=twd[:, 256:768]), r=['twd'], w=['xf_dbg'])
            return finish_early(xf_[:], 128, 512, 'xf_dbg')
        ysum = T(rw, 'ysum', [128, 32, 128], F32)
        lw_st = ysum[:, 0:8, :].rearrange("p a b -> p (a b)"); wupb = T(rw, 'wupb', [128, D], BF16); aupb = T(rw, 'aupb', [128, D], BF16)
        gupb = T(rw, 'gupb', [128, 2, D], BF16)
        for src_, dst_, np_ in [(wup_d[:, :], wupb[:], 128), (aup_d[:, :], aupb[:], 128), (gup_d[0:128, :], gupb[:, 0, :], 128), (gup_d[128:160, :], gupb[0:32, 1, :], 32)]:
            S.dma('sp', lw_st[0:np_, :], src_, w=['ysum'])
            S.op('pool', lambda e, dst_=dst_, np_=np_: e.tensor_copy(out=dst_, in_=lw_st[0:np_, :]), r=['ysum'], w=['lwdst'])
        lnxbc = T(rw, 'lnxbc', [128, 2, 128], F32)

        rb = T(rw, 'rb', [128, T_ALL], BF16); kb = T(rw, 'kb', [128, T_ALL], BF16); vb = T(rw, 'vb', [128, T_ALL], BF16)
        kkb = T(rw, 'kkb', [128, T_ALL], BF16); ksum = T(rw, 'ksum', [128, T_LAT], BF16)
        Vm = T(rw, 'Vm', [128, NCH, 128], BF16)
        yaT = [T(rw, 'yaT%d' % i, [128, 512], BF16) for i in range(2)]
        PL = T(rw, 'PL', [128, 2, NCH], F32)
        seg_sh = {}
        for nm in ['nlw', 'cn', 'en']:
            seg_sh[nm] = T(rw, 'sgs_%s' % nm, [128, 512], F32)
        for nm in ['Ee', 'Ec', 'Ei', 'Et', 'af', 'beta', 'kd', 'tt']:
            seg_sh[nm] = T(rw, 'sgs_%s' % nm, [128, 512], F32 if nm == 'tt' else BF16)

        def seg_bufs(i):
            b = dict(seg_sh)
            for nm in ['aT', 'rT', 'bT', 'kT', 'BhT', 'KhT']:
                b[nm] = T(rw, 'sg%d_%s' % (i, nm), [128, 512], BF16)
            b['Bhm'] = T(rw, 'sg%d_Bhm' % i, [128, 4, 128], BF16)
            b['Khm'] = T(rw, 'sg%d_Khm' % i, [128, 4, 128], BF16)
            b['n'] = 'sg%d' % i
            return b
        SG = [seg_bufs(0), seg_bufs(1)]
        Gt = [T(rw, 'G%d' % i, [128, 512], BF16) for i in range(4)]
        N0 = [T(rw, 'N0_%d' % i, [128, 128], F32) for i in range(4)]
        NT0 = [T(rw, 'NT0_%d' % i, [128, 128], F32) for i in range(4)]
        NP = [[T(rw, 'NP%d_%d' % (i, j), [128, 256], F32) for j in range(2)] for i in range(4)]
        Xb = [[T(rw, 'Xb%d_%d' % (i, j), [128, 128], F32) for j in range(2)] for i in range(4)]
        Xf = [T(rw, 'Xf%d' % i, [128, 128], BF16) for i in range(4)]
        MTs = [T(rw, 'MTs%d' % i, [128, 64], F32) for i in range(3)]; Nns = [T(rw, 'Nns%d' % i, [128, 64], F32) for i in range(3)]; RpT = [T(rw, 'RpT%d' % i, [128, 128], BF16) for i in range(3)]
        Sst = [[T(rw, 'Sst%d_%d' % (d_, i), [128, 64], F32) for i in range(2)] for d_ in range(2)]
        Sb = [T(rw, 'Sb%d' % i, [128, 64], BF16) for i in range(2)]
        kk_t = [seg_sh['tt'], seg_sh['cn']]
        kk_q = [seg_sh['Ee'], seg_sh['Ec']]
        kk_r = [seg_sh['nlw'], seg_sh['en']]
        KKN = [('sgs_tt', 'sgs_Ee', 'sgs_nlw'), ('sgs_cn', 'sgs_Ec', 'sgs_en')]
        finA = [{nm: T(rw, 'fin%d_' % r_ + nm, shp, dt) for nm, shp, dt in [
            ('st', [128, 24], F32), ('yc', [128, 128], F32), ('sq', [128, 128], BF16), ('bon', [128, 2], F32),
('yo', [128, 128], BF16), ('pr', [128, 512], BF16)]} for r_ in range(2)]

        for hp in range(nhp):
            project_group([
                (hp * 128, 128, shift_consume(128, hp, lambda t1, tn, g0, n: S.op(
                    'pool', lambda e: e.tensor_copy(out=rb[:, g0:g0 + n], in_=t1), r=[tn], w=['rb']))),
                (D + hp * 128, 128, shift_consume(128, 8 + hp, lambda t1, tn, g0, n: S.op(
                    'pool', lambda e: e.tensor_copy(out=kb[:, g0:g0 + n], in_=t1), r=[tn], w=['kb']))),
                (2 * D + hp * 128, 128, shift_consume(128, 16 + hp, lambda t1, tn, g0, n: S.op(
                    'pool', lambda e: e.tensor_copy(out=vb[:, g0:g0 + n], in_=t1), r=[tn], w=['vb'])))])
            S.dma('sp', lnxbc[:, 0, :], lnx_d[:, 0, hp * 128:(hp + 1) * 128], w=['lnxbc'])
            S.dma('sp', lnxbc[:, 1, :], lnx_d[:, 1, hp * 128:(hp + 1) * 128], w=['lnxbc'])
            for ti, (g0, n, is_ctx) in enumerate(TT):
                q = ti % 2
                kt, kq, kr = kk_t[q], kk_q[q], kk_r[q]
                ktn, kqn, krn = KKN[q]
                kbank = PB[6] if q == 0 else PB[3]; kbn = 'pb6' if q == 0 else 'pb3'
                S.op('act', lambda e, kq=kq, g0=g0, n=n: e.activation(out=kq[:, 0:n], in_=kb[:, g0:g0 + n], func=AF.Square, scale=kkT[:, hp:hp + 1]), r=['kb', 'kkT'], w=[kqn])
                S.op('pe', lambda e, kq=kq, n=n, kbank=kbank: e.matmul(kbank[:, 0:n], lhsT=bonesb, rhs=kq[:, 0:n], start=True, stop=True), r=[kqn, 'cstb'], w=[kbn])
                S.op('act', lambda e, kr=kr, n=n, kbank=kbank: e.activation(out=kr[:, 0:n], in_=kbank[:, 0:n], func=AF.Sqrt), r=[kbn], w=[krn])
                S.op('dve', lambda e, kr=kr, n=n: e.tensor_scalar(out=kr[:, 0:n], in0=kr[:, 0:n], scalar1=1e-12, scalar2=None, op0=ALU.max), r=[krn], w=[krn])
                S.op('dve', lambda e, kr=kr, n=n: e.reciprocal(out=kr[:, 0:n], in_=kr[:, 0:n]), r=[krn], w=[krn])
                S.op('dve', lambda e, kr=kr, g0=g0, n=n: e.scalar_tensor_tensor(out=kkb[:, g0:g0 + n], in0=kb[:, g0:g0 + n], scalar=kkT[:, hp:hp + 1], in1=kr[:, 0:n], op0=ALU.mult, op1=ALU.mult),
                     r=['kb', 'kkT', krn], w=['kkb'])
            for c0 in range(0, NCH, 8):
                nchk = min(8, NCH - c0)
                for j in range(nchk):
                    c = c0 + j
                    S.op('pe', lambda e, c=c, j=j: e.transpose(out=PT[:, j * 128:(j + 1) * 128], in_=vb[:, c * 128:(c + 1) * 128], identity=identb),
                         r=['vb', 'cstb'], w=['pbT'])
                S.op('act', lambda e, c0=c0, nchk=nchk: e.activation(out=Vm[:, c0:c0 + nchk, :], in_=PT[:, 0:nchk * 128].rearrange("p (a b) -> p a b", b=128), func=AF.Copy),
                     r=['pbT'], w=['Vm'])

            if stage == 'p4':
                xf_ = ysum[:, 0:8, :].rearrange("p a b -> p (a b)")
                S.op('dve', lambda e: e.tensor_copy(out=xf_[:, 0:512], in_=kkb[:, 256:768]), r=['kkb'], w=['xf_dbg'])
                S.op('dve', lambda e: e.tensor_copy(out=xf_[:, 512:1024], in_=Vm[:, 2:6, :].rearrange("p a b -> p (a b)")), r=['Vm'], w=['xf_dbg'])
                return finish_early(xf_[:], 128, 1024, 'xf_dbg')
            segsD = [[[0, 1]] + [[2 + 4 * s_ + j for j in range(4)] for s_ in range(8)],
                     [[1, 0]] + [[2 + 4 * s_ + j for j in range(3, -1, -1)] for s_ in range(7, -1, -1)]]
            for d in range(2):
                S.op('pool', lambda e, d=d: e.memset(Sst[d][0][:], 0.0), w=['Sst%d_0' % d])
                S.op('pool', lambda e, d=d: e.memset(Sb[d][:], 0.0), w=['Sb%d' % d])
            par = [0, 0]
            seen_y = set(); seen_k = set()
            HS = [slice(0, 64), slice(64, 128)]

            def emit_prep(si, d):
                seg = segsD[d][si]
                B = SG[d]; bn_ = B['n']
                lo = min(seg) * 128; n = len(seg) * 128
                sl = slice(lo, lo + n)
                dsl = slice(d * 64, d * 64 + 64)
                S.op('pe', lambda e, sl=sl, n=n, dsl=dsl: e.matmul(PB[6][:, 0:n], lhsT=wupb[dsl, hp * 128:(hp + 1) * 128], rhs=twd[dsl, sl], start=True, stop=True),
                     r=['lwdst', 'twd'], w=['pb6'])
                S.op('act', lambda e, B=B, n=n, d=d: e.activation(out=B['tt'][:, 0:n], in_=PB[6][:, 0:n], func=AF.Softplus, scale=-1.0, bias=nw0[:, hp, d:d + 1]),
                     r=['pb6', 'nw0'], w=['sgs_tt'])
                S.op('pe', lambda e, sl=sl, n=n, dsl=dsl: e.matmul(PB[6][:, 0:n], lhsT=aupb[dsl, hp * 128:(hp + 1) * 128], rhs=adT[dsl, sl], start=True, stop=True),
                     r=['lwdst', 'adT'], w=['pb6'])
                S.op('act', lambda e, B=B, n=n: e.activation(out=B['nlw'][:, 0:n], in_=B['tt'][:, 0:n], func=AF.Exp, scale=-1.0, bias=-0.5),
                     r=['sgs_tt'], w=['sgs_nlw'])
                S.op('act', lambda e, B=B, n=n, d=d: e.activation(out=B['af'][:, 0:n], in_=PB[6][:, 0:n], func=AF.Sigmoid, bias=a0T[:, hp, d:d + 1]),
                     r=['pb6', 'a0T'], w=['sgs_af'])
                for c in seg:
                    o = c * 128 - lo
                    if d == 0:
                        S.op('dve', lambda e, B=B, o=o: e.tensor_tensor_scan(out=B['cn'][:, o:o + 128], data0=onesf, data1=B['nlw'][:, o:o + 128],
                                                                             initial=0.0, op0=ALU.mult, op1=ALU.add), r=['sgs_nlw', 'cst'], w=['sgs_cn'])
                        tot = B['cn'][:, o + 127:o + 128]
                    else:
                        S.op('dve', lambda e, B=B, o=o: e.tensor_tensor_scan(out=B['cn'][:, o + 127:(o - 1 if o > 0 else None):-1], data0=onesf,
                                                                             data1=B['nlw'][:, o + 127:(o - 1 if o > 0 else None):-1],
                                                                             initial=0.0, op0=ALU.mult, op1=ALU.add), r=['sgs_nlw', 'cst'], w=['sgs_cn'])
                        tot = B['cn'][:, o:o + 1]
                    S.op('dve', lambda e, tot=tot, c=c, d=d: e.tensor_scalar(out=PL[:, d, c:c + 1], in0=tot, scalar1=-1.0, scalar2=None, op0=ALU.mult),
                         r=['sgs_cn'], w=['PLn'])
                    S.op('act', lambda e, B=B, o=o, c=c, d=d: e.activation(out=B['Et'][:, o:o + 128], in_=B['cn'][:, o:o + 128], func=AF.Exp, bias=PL[:, d, c:c + 1]),
                         r=['sgs_cn', 'PLn'], w=['sgs_Et'])
                    S.op('act', lambda e, c=c, d=d: e.activation(out=PL[:, d, c:c + 1], in_=PL[:, d, c:c + 1], func=AF.Exp), r=['PLn'], w=['PLn', 'PL'])
                S.op('pool', lambda e, B=B, n=n: e.tensor_tensor(out=B['en'][:, 0:n], in0=B['cn'][:, 0:n], in1=B['nlw'][:, 0:n], op=ALU.subtract),
                     r=['sgs_cn', 'sgs_nlw'], w=['sgs_en'])
                S.op('act', lambda e, B=B, n=n: e.activation(out=B['Ee'][:, 0:n], in_=B['en'][:, 0:n], func=AF.Exp, scale=-1.0), r=['sgs_en'], w=['sgs_Ee'])
                S.op('act', lambda e, B=B, n=n: e.activation(out=B['Ec'][:, 0:n], in_=B['cn'][:, 0:n], func=AF.Exp, scale=-1.0), r=['sgs_cn'], w=['sgs_Ec'])
                S.op('act', lambda e, B=B, n=n: e.activation(out=B['Ei'][:, 0:n], in_=B['cn'][:, 0:n], func=AF.Exp), r=['sgs_cn'], w=['sgs_Ei'])
                S.op('dve', lambda e, B=B, sl=sl, n=n: e.scalar_tensor_tensor(out=B['aT'][:, 0:n], in0=kkb[:, sl], scalar=-1.0, in1=B['Ee'][:, 0:n], op0=ALU.mult, op1=ALU.mult),
                     r=['kkb', 'sgs_Ee'], w=[bn_ + 'aT'])
                S.op('pool', lambda e, B=B, sl=sl, n=n: e.tensor_tensor(out=B['rT'][:, 0:n], in0=rb[:, sl], in1=B['Ec'][:, 0:n], op=ALU.mult),
                     r=['rb', 'sgs_Ec'], w=[bn_ + 'rT'])
                S.op('pool', lambda e, B=B, sl=sl, n=n: e.tensor_tensor(out=B['beta'][:, 0:n], in0=kkb[:, sl], in1=B['af'][:, 0:n], op=ALU.mult),
                     r=['kkb', 'sgs_af'], w=['sgs_beta'])
                S.op('dve', lambda e, B=B, n=n: e.tensor_tensor(out=B['bT'][:, 0:n], in0=B['beta'][:, 0:n], in1=B['Ei'][:, 0:n], op=ALU.mult),
                     r=['sgs_beta', 'sgs_Ei'], w=[bn_ + 'bT'])
                S.op('pool', lambda e, B=B, n=n: e.tensor_tensor(out=B['BhT'][:, 0:n], in0=B['beta'][:, 0:n], in1=B['Et'][:, 0:n], op=ALU.mult),
                     r=['sgs_beta', 'sgs_Et'], w=[bn_ + 'BhT'])
                S.op('dve', lambda e, B=B, n=n: e.tensor_scalar(out=B['tt'][:, 0:n], in0=B['af'][:, 0:n], scalar1=kaT[:, hp:hp + 1], scalar2=oka[:, hp:hp + 1], op0=ALU.mult, op1=ALU.add),
                     r=['sgs_af', 'kaT', 'oka'], w=['sgs_tt'])
                S.op('pool', lambda e, B=B, sl=sl, n=n: e.tensor_tensor(out=B['kd'][:, 0:n], in0=kb[:, sl], in1=B['tt'][:, 0:n], op=ALU.mult),
                     r=['kb', 'sgs_tt'], w=['sgs_kd'])
                S.op('dve', lambda e, B=B, n=n: e.tensor_tensor(out=B['kT'][:, 0:n], in0=B['kd'][:, 0:n], in1=B['Ei'][:, 0:n], op=ALU.mult),
                     r=['sgs_kd', 'sgs_Ei'], w=[bn_ + 'kT'])
                S.op('pool', lambda e, B=B, n=n: e.tensor_tensor(out=B['KhT'][:, 0:n], in0=B['kd'][:, 0:n], in1=B['Et'][:, 0:n], op=ALU.mult),
                     r=['sgs_kd', 'sgs_Et'], w=[bn_ + 'KhT'])
                if lo >= T_CTX:
                    ls = slice(lo - T_CTX, lo - T_CTX + n)
                    if lo not in seen_k:
                        seen_k.add(lo)
                        S.op('pool', lambda e, B=B, ls=ls, n=n: e.tensor_copy(out=ksum[:, ls], in_=B['kd'][:, 0:n]), r=['sgs_kd'], w=['ksum'])
                    else:
                        S.op('pool', lambda e, B=B, ls=ls, n=n: e.tensor_tensor(out=ksum[:, ls], in0=ksum[:, ls], in1=B['kd'][:, 0:n], op=ALU.add), r=['sgs_kd', 'ksum'], w=['ksum'])
                nck = len(seg)
                for j in range(nck):
                    S.op('pe', lambda e, B=B, j=j: e.transpose(out=PT[:, j * 128:(j + 1) * 128], in_=B['BhT'][:, j * 128:(j + 1) * 128], identity=identb),
                         r=[bn_ + 'BhT', 'cstb'], w=['pbT'])
                    S.op('pe', lambda e, B=B, j=j: e.transpose(out=PT[:, 512 + j * 128:512 + (j + 1) * 128], in_=B['KhT'][:, j * 128:(j + 1) * 128], identity=identb),
                         r=[bn_ + 'KhT', 'cstb'], w=['pbT'])
                S.op('act', lambda e, B=B, nck=nck: e.activation(out=B['Bhm'][:, 0:nck, :], in_=PT[:, 0:nck * 128].rearrange("p (a b) -> p a b", b=128), func=AF.Copy),
                     r=['pbT'], w=[bn_ + 'Bhm'])
                S.op('act', lambda e, B=B, nck=nck: e.activation(out=B['Khm'][:, 0:nck, :], in_=PT[:, 512:512 + nck * 128].rearrange("p (a b) -> p a b", b=128), func=AF.Copy),
                     r=['pbT'], w=[bn_ + 'Khm'])


            stream = []
            for si in range(9):
                for jj in range(len(segsD[0][si])):
                    for d in range(2):
                        seg = segsD[d][si]; c = seg[jj]; lo = min(seg) * 128
                        o = c * 128 - lo
                        for hl in range(2):
                            stream.append(dict(d=d, hl=hl, c=c, cs=slice(o, o + 128), jc=o // 128, hs=HS[hl], pb=hl * 64, B=SG[d], bn=SG[d]['n'],
                                               prep=(si, d) if (jj == 0 and hl == 0) else None))
            NG = 3
            for g0_ in range(0, len(stream), NG):
                INS = stream[g0_:g0_ + NG]
                for sl_, I in enumerate(INS):
                    I['i'] = sl_; I['XB'] = PB[2 * sl_]; I['xn'] = 'pb%d' % (2 * sl_); I['QB'] = PB[2 * sl_ + 1]; I['qn'] = 'pb%d' % (2 * sl_ + 1)
                    if I['prep'] is not None:
                        emit_prep(*I['prep'])
                for I in INS:
                    i, B, hs, cs, bn_, d = I['i'], I['B'], I['hs'], I['cs'], I['bn'], I['d']
                    XB_, xn, QB_, qn = I['XB'], I['xn'], I['QB'], I['qn']
                    for q_, (l_, r_) in enumerate([('bT', 'aT'), ('bT', 'rT'), ('kT', 'aT'), ('kT', 'rT')]):
                        S.op('pe', lambda e, XB_=XB_, B=B, hs=hs, cs=cs, q_=q_, l_=l_, r_=r_: e.matmul(XB_[:, q_ * 128:(q_ + 1) * 128], lhsT=B[l_][hs, cs], rhs=B[r_][hs, cs], start=True, stop=True),
                             r=[bn_ + l_, bn_ + r_], w=[xn])
                    S.op('pe', lambda e, QB_=QB_, B=B, hs=hs, cs=cs: e.matmul(QB_[:, 0:128], lhsT=B['aT'][hs, cs], rhs=B['bT'][hs, cs], start=True, stop=True),
                         r=[bn_ + 'aT', bn_ + 'bT'], w=[qn])
                for I in INS:
                    i, d, XB_, xn, QB_, qn = I['i'], I['d'], I['XB'], I['xn'], I['QB'], I['qn']
                    S.op('dve', lambda e, i=i, d=d, XB_=XB_: e.tensor_tensor(out=Gt[i][:], in0=XB_[:, :], in1=mG[:, d, :], op=ALU.mult), r=[xn, 'mG'], w=['G%d' % i])
                    S.op('dve', lambda e, i=i, d=d, XB_=XB_: e.tensor_tensor(out=NT0[i][:], in0=XB_[:, 0:128], in1=mN[1 - d], op=ALU.mult), r=[xn, 'cst'], w=['N0_%d' % i])
                    S.op('dve', lambda e, i=i, d=d, QB_=QB_: e.tensor_tensor(out=N0[i][:], in0=QB_[:, 0:128], in1=mN[d], op=ALU.mult), r=[qn, 'cst'], w=['N0_%d' % i])
                for I in INS:
                    i, B, hs, cs, bn_, pb_, c, XB_, xn = I['i'], I['B'], I['hs'], I['cs'], I['bn'], I['pb'], I['c'], I['XB'], I['xn']
                    S.op('pe', lambda e, XB_=XB_, B=B, hs=hs, cs=cs, pb_=pb_: e.matmul(XB_[:, 0:64], lhsT=B['aT'][hs, cs], rhs=identb[hs, pb_:pb_ + 64], start=True, stop=False),
                         r=[bn_ + 'aT', 'cstb', 'G%d' % i, 'N0_%d' % i], w=[xn])
                    S.op('pe', lambda e, XB_=XB_, i=i, pb_=pb_, c=c: e.matmul(XB_[:, 64:128], lhsT=Gt[i][:, 256:384], rhs=Vm[:, c, pb_:pb_ + 64], start=False, stop=False, skip_group_check=True),
                         r=['G%d' % i, 'Vm'], w=[xn])
                Ncur = [N0[i][:] for i in range(NG)]; NTcur = [NT0[i][:] for i in range(NG)]
                Nres = [['N0_%d' % i] for i in range(NG)]
                for p in range(7):
                    for I in INS:
                        i, XB_, xn, QB_, qn = I['i'], I['XB'], I['xn'], I['QB'], I['qn']
                        xbn = 'Xb%d_%d' % (i, p % 2)
                        lowp = (p >= LOWP_FROM)
                        xb_ap = Xb[i][p % 2][:].bitcast(BF16)[:, 0:128] if lowp else Xb[i][p % 2][:]
                        S.op('act', lambda e, XB_=XB_, xb_ap=xb_ap: e.activation(out=xb_ap, in_=XB_[:, 0:128], func=AF.Copy), r=[xn], w=[xbn])
                        S.op('pe', lambda e, XB_=XB_, xb_ap=xb_ap, nt=NTcur[i], p=p: e.matmul(XB_[:, 0:128], lhsT=nt, rhs=xb_ap, start=False, stop=(p == 6), skip_group_check=True),
                             r=[xbn] + Nres[i], w=[xn])
                        if p < 6:
                            S.op('pe', lambda e, QB_=QB_, nt=NTcur[i], nn=Ncur[i]: e.matmul(QB_[:, 128:256], lhsT=nt, rhs=nn, start=True, stop=True), r=Nres[i], w=[qn])
                            S.op('pe', lambda e, QB_=QB_, nt=NTcur[i], nn=Ncur[i]: e.matmul(QB_[:, 256:384], lhsT=nn, rhs=nt, start=True, stop=True), r=Nres[i], w=[qn])
                            for _dm in range(int(os.environ.get('DUMMY', '0'))):
                                S.op('pe', lambda e, QB_=QB_: e.matmul(QB_[:, 384:512], lhsT=identb, rhs=identb, start=True, stop=True), r=['cstb'], w=[qn])
                            npn = 'NP%d_%d' % (i, p % 2)
                            npt_ap = NP[i][p % 2][:].bitcast(BF16)[:, 0:256] if p >= LOWP_FROM - 1 else NP[i][p % 2][:]
                            S.op('dve', lambda e, QB_=QB_, npt_ap=npt_ap: e.tensor_copy(out=npt_ap, in_=QB_[:, 128:384]), r=[qn], w=[npn])
                            Ncur[i] = npt_ap[:, 0:128]; NTcur[i] = npt_ap[:, 128:256]; Nres[i] = [npn]
                for I in INS:
                    i, XB_, xn = I['i'], I['XB'], I['xn']
                    S.op('act', lambda e, i=i, XB_=XB_: e.activation(out=Xf[i][:], in_=XB_[:, 0:128], func=AF.Copy), r=[xn], w=['Xf%d' % i])
                for I in INS:
                    i, B, hs, cs, bn_, pb_, c, d, jc, hl, xbk, xn = I['i'], I['B'], I['hs'], I['cs'], I['bn'], I['pb'], I['c'], I['d'], I['jc'], I['hl'], I['XB'], I['xn']
                    S.op('pe', lambda e, B=B, hs=hs, pb_=pb_, jc=jc, i=i, xbk=xbk: e.matmul(xbk[hs, 128:192], lhsT=B['Bhm'][:, jc, pb_:pb_ + 64], rhs=Xf[i][:, 64:128], start=True, stop=False),
                         r=[bn_ + 'Bhm', 'Xf%d' % i], w=[xn])
                    S.op('pe', lambda e, B=B, hs=hs, pb_=pb_, jc=jc, c=c, xbk=xbk: e.matmul(xbk[hs, 128:192], lhsT=B['Khm'][:, jc, pb_:pb_ + 64], rhs=Vm[:, c, pb_:pb_ + 64], start=False, stop=True),
                         r=[bn_ + 'Khm', 'Vm'], w=[xn])
                    S.op('pe', lambda e, B=B, hs=hs, pb_=pb_, jc=jc, i=i, xbk=xbk: e.matmul(xbk[hs, 192:256], lhsT=Xf[i][:, 0:64], rhs=B['Bhm'][:, jc, pb_:pb_ + 64], start=True, stop=True),
                         r=[bn_ + 'Bhm', 'Xf%d' % i], w=[xn])
                    S.op('pe', lambda e, hs=hs, i=i, xbk=xbk: e.matmul(xbk[hs, 256:384], lhsT=Xf[i][:, 0:64], rhs=Gt[i][:, 128:256], start=True, stop=True),
                         r=['G%d' % i, 'Xf%d' % i], w=[xn])
                    S.op('dve', lambda e, hs=hs, pb_=pb_, d=d, c=c, i=i, xbk=xbk: e.scalar_tensor_tensor(out=MTs[i][hs, :], in0=identf[hs, pb_:pb_ + 64], scalar=PL[hs, d, c:c + 1], in1=xbk[hs, 192:256],
                                                                                                   op0=ALU.mult, op1=ALU.add), r=[xn, 'PL', 'cst'], w=['MTs%d' % i])
                    S.op('dve', lambda e, hs=hs, i=i, xbk=xbk: e.tensor_copy(out=Nns[i][hs, :], in_=xbk[hs, 128:192]), r=[xn], w=['Nns%d' % i])
                    S.op('dve', lambda e, B=B, hs=hs, cs=cs, i=i, xbk=xbk: e.tensor_tensor(out=RpT[i][hs, :], in0=xbk[hs, 256:384], in1=B['rT'][hs, cs], op=ALU.add), r=[xn, bn_ + 'rT'], w=['RpT%d' % i])
                for I in INS:
                    i, hs, pb_, c, d, hl, xbk, xn = I['i'], I['hs'], I['pb'], I['c'], I['d'], I['hl'], I['XB'], I['xn']
                    sp_ = par[d]
                    st0, st1 = Sst[d][sp_], Sst[d][1 - sp_]; sn0, sn1 = 'Sst%d_%d' % (d, sp_), 'Sst%d_%d' % (d, 1 - sp_)
                    S.op('pe', lambda e, hs=hs, i=i, st0=st0, xbk=xbk: e.matmul(xbk[hs, 384:448], lhsT=MTs[i][hs, :], rhs=st0[hs, :], start=True, stop=True),
                         r=['MTs%d' % i, sn0, 'Nns%d' % i, 'RpT%d' % i], w=[xn])
                    S.op('dve', lambda e, hs=hs, i=i, st1=st1, xbk=xbk: e.tensor_tensor(out=st1[hs, :], in0=xbk[hs, 384:448], in1=Nns[i][hs, :], op=ALU.add),
                         r=[xn, 'Nns%d' % i], w=[sn1])
                    if c >= 2:
                        lc = c - 2
                        S.op('pe', lambda e, hs=hs, d=d, i=i, xbk=xbk: e.matmul(xbk[:, 448:512], lhsT=RpT[i][hs, :], rhs=Sb[d][hs, :], start=True, stop=False),
                             r=['RpT%d' % i, 'Sb%d' % d], w=[xn])
                        S.op('pe', lambda e, i=i, xbk=xbk: e.matmul(xbk[:, 448:512], lhsT=Gt[i][:, 128:256], rhs=Xf[i][:, 64:128], start=False, stop=False),
                             r=['G%d' % i, 'Xf%d' % i], w=[xn])
                        S.op('pe', lambda e, i=i, pb_=pb_, c=c, xbk=xbk: e.matmul(xbk[:, 448:512], lhsT=Gt[i][:, 384:512], rhs=Vm[:, c, pb_:pb_ + 64], start=False, stop=True),
                             r=['G%d' % i, 'Vm'], w=[xn])
                        key = (lc, hl)
                        if key not in seen_y:
                            seen_y.add(key)
                            S.op('dve', lambda e, pb_=pb_, lc=lc, xbk=xbk: e.tensor_copy(out=ysum[:, lc, pb_:pb_ + 64], in_=xbk[:, 448:512]), r=[xn], w=['ysum'])
                        else:
                            S.op('dve', lambda e, pb_=pb_, lc=lc, xbk=xbk: e.tensor_tensor(out=ysum[:, lc, pb_:pb_ + 64], in0=xbk[:, 448:512], in1=ysum[:, lc, pb_:pb_ + 64], op=ALU.add),
                                 r=[xn, 'ysum'], w=['ysum'])
                    S.op('act', lambda e, d=d, hs=hs, st1=st1: e.activation(out=Sb[d][hs, :], in_=st1[hs, :], func=AF.Copy), r=[sn1], w=['Sb%d' % d])
                    if hl == 1:
                        par[d] = 1 - par[d]
            if stage == 'rwkv_y':
                return finish_early(ysum[:].rearrange("p a b -> p (a b)"), 128, 1024, 'ysum') if False else finish_early4(ysum)
            for s8 in range(8):
                finp = finA[s8 % 2]; FP_ = 'fin%d_' % (s8 % 2)
                ls = slice(s8 * 512, (s8 + 1) * 512); gsl = slice(T_CTX + s8 * 512, T_CTX + (s8 + 1) * 512)
                S.dma('sp', sgt[:, 0, :], sgd_s[0, :, gsl], r=['sgd_s'], w=['sgt'])
                S.dma('sp', sgt[0:32, 1, :], sgd_s[1, 0:32, gsl], r=['sgd_s'], w=['sgt'])
                S.op('dve', lambda e, ls=ls, gsl=gsl: e.tensor_tensor(out=finp['pr'][:], in0=rb[:, gsl], in1=ksum[:, ls], op=ALU.mult), r=['rb', 'ksum'], w=[FP_ + 'pr'])
                S.op('dve', lambda e: e.tensor_scalar(out=finp['pr'][:], in0=finp['pr'][:], scalar1=rkT[:, hp:hp + 1], scalar2=None, op0=ALU.mult), r=[FP_ + 'pr', 'rkT'], w=[FP_ + 'pr'])
                for j in range(4):
                    lc = s8 * 4 + j; c = lc + 2
                    fin = finA[j % 2]; FN_ = 'fin%d_' % (j % 2)
                    gbk = PB[5] if j % 2 == 0 else PB[4]; gbn = 'pb5' if j % 2 == 0 else 'pb4'
                    bbk = PB[6] if j % 2 == 0 else PB[3]; bbn = 'pb6' if j % 2 == 0 else 'pb3'
                    for hl in range(2):
                        S.op('pe', lambda e, hl=hl, j=j: e.matmul(bbk[:, hl:hl + 1], lhsT=finp['pr'][hl * 64:(hl + 1) * 64, j * 128:(j + 1) * 128],
                                                                  rhs=bonesb[hl * 64:(hl + 1) * 64, hl * 64:hl * 64 + 1], start=True, stop=True), r=[FP_ + 'pr', 'cstb'], w=[bbn])
                    S.op('act', lambda e: e.activation(out=fin['bon'][:], in_=bbk[:, 0:2], func=AF.Copy), r=[bbn], w=[FN_ + 'bon'])
                    S.op('pe', lambda e, c=c: e.matmul(gbk[:, 0:128], lhsT=sgt[:, 0, j * 128:(j + 1) * 128], rhs=gupb[:, 0, hp * 128:(hp + 1) * 128], start=True, stop=False),
                         r=['sgt', 'lwdst'], w=[gbn])
                    S.op('pe', lambda e, c=c: e.matmul(gbk[:, 0:128], lhsT=sgt[0:32, 1, j * 128:(j + 1) * 128], rhs=gupb[0:32, 1, hp * 128:(hp + 1) * 128], start=False, stop=True),
                         r=['sgt', 'lwdst'], w=[gbn])
                    for hl in range(2):
                        hc = slice(hl * 64, hl * 64 + 64)
                        ysl = ysum[:, lc, hc]
                        so = hl * 12
                        S.op('dve', lambda e, ysl=ysl, so=so: e.bn_stats(out=fin['st'][:, so:so + 6], in_=ysl), r=['ysum'], w=[FN_ + 'st%d' % hl])
                        S.op('dve', lambda e, so=so: e.bn_aggr(out=fin['st'][:, so + 6:so + 8], in_=fin['st'][:, so:so + 6]), r=[FN_ + 'st%d' % hl], w=[FN_ + 'nm%d' % hl])
                        S.op('dve', lambda e, so=so: e.tensor_scalar(out=fin['st'][:, so + 8:so + 9], in0=fin['st'][:, so + 7:so + 8], scalar1=64e-5, scalar2=None, op0=ALU.add),
                             r=[FN_ + 'nm%d' % hl], w=[FN_ + 'v%d' % hl])
                        S.op('act', lambda e, so=so: e.activation(out=fin['st'][:, so + 8:so + 9], in_=fin['st'][:, so + 8:so + 9], func=AF.Sqrt), r=[FN_ + 'v%d' % hl], w=[FN_ + 'v%d' % hl])
                        S.op('dve', lambda e, so=so: e.reciprocal(out=fin['st'][:, so + 9:so + 10], in_=fin['st'][:, so + 8:so + 9]), r=[FN_ + 'v%d' % hl], w=[FN_ + 'rs%d' % hl])
                        S.op('dve', lambda e, ysl=ysl, hc=hc, so=so: e.tensor_scalar(out=fin['yc'][:, hc], in0=ysl, scalar1=fin['st'][:, so + 6:so + 7], scalar2=fin['st'][:, so + 9:so + 10],
                                                                                  op0=ALU.subtract, op1=ALU.mult), r=['ysum', FN_ + 'nm%d' % hl, FN_ + 'rs%d' % hl], w=[FN_ + 'yc%d' % hl])
                        gch = slice(hl * 64, hl * 64 + 64)
                        S.op('pool', lambda e, hc=hc, gch=gch: e.tensor_tensor(out=fin['yc'][:, hc], in0=fin['yc'][:, hc], in1=lnxbc[:, 0, gch], op=ALU.mult), r=[FN_ + 'yc%d' % hl, 'lnxbc'], w=[FN_ + 'yc%d' % hl])
                        S.op('pool', lambda e, hc=hc, gch=gch: e.tensor_tensor(out=fin['yc'][:, hc], in0=fin['yc'][:, hc], in1=lnxbc[:, 1, gch], op=ALU.add),
                             r=[FN_ + 'yc%d' % hl, 'lnxbc'], w=[FN_ + 'yc%d' % hl])
                        S.op('dve', lambda e, hl=hl, hc=hc, c=c: e.scalar_tensor_tensor(out=fin['yc'][:, hc], in0=Vm[:, c, hc], scalar=fin['bon'][:, hl:hl + 1], in1=fin['yc'][:, hc],
                                                                                      op0=ALU.mult, op1=ALU.add), r=['Vm', FN_ + 'bon', FN_ + 'yc%d' % hl], w=[FN_ + 'yc%d' % hl])
                    S.op('dve', lambda e: e.tensor_tensor(out=fin['yo'][:], in0=gbk[:, 0:128], in1=fin['yc'][:], op=ALU.mult), r=[gbn, FN_ + 'yc0', FN_ + 'yc1'], w=[FN_ + 'yo'])
                    S.op('pe', lambda e, j=j: e.transpose(out=PT[:, j * 128:(j + 1) * 128], in_=fin['yo'][:], identity=identb), r=[FN_ + 'yo', 'cstb'], w=['pbT'])
                S.op('act', lambda e, s8=s8: e.activation(out=yaT[s8 % 2][:], in_=PT[:, 0:512], func=AF.Copy), r=['pbT'], w=['yaT%d' % (s8 % 2)])
                S.dma('sp', ya_s[hp, :, ls], yaT[s8 % 2][:], r=['yaT%d' % (s8 % 2)], w=['ya_s'])
        S.barrier(); S.emit()
        rw.close(); open_stacks.pop()

        if stage == 'rwkv':
            with ExitStack() as ps:
                dt_ = T(ps, 'dbg_t', [128, T_LAT], BF16); df_ = T(ps, 'dbg_f', [128, T_LAT], F32)
                toks = []
                dbgv = dbg_d.rearrange("(a b) d -> a (b d)", b=4)
                for hp in range(nhp):
                    S.dma('sp', dt_[:], ya_s[hp, :, :], r=['ya_s'], w=['dbg_t'])
                    S.op('dve', lambda e: e.tensor_copy(out=df_[:], in_=dt_[:]), r=['dbg_t'], w=['dbg_f'])
                    toks.append(S.dma('sp', dbgv[hp * 128:(hp + 1) * 128, :], df_[:], r=['dbg_f'], w=['dbg']))
                S.op('pool', lambda e: e.memset(df_[:, 0:8], 0.0), r=['dbg'], w=['dbg_f'])
                toks.append(S.dma('sp', out_d[0:128, 0:8], df_[:, 0:8], r=['dbg_f'], w=['out']))
                S.wait_all('sp', toks)
                S.emit()
            return nc

        lr = ExitStack(); open_stacks.append(lr)
        zx = T(lr, 'zx', [128, T_ALL], F32); xc = T(lr, 'xc', [128, T_ALL], F32); xcb = T(lr, 'xcb', [128, T_ALL], BF16)
        glu = T(lr, 'glu', [128, T_LAT], BF16)
        aa = [T(lr, 'aa0', [128, T_ALL], F32)] * 2
        bx = [T(lr, 'bx0', [128, T_ALL], F32)] * 2
        hh = [T(lr, 'hh%d' % i, [128, T_ALL], F32) for i in range(2)]
        ybt = T(lr, 'ybt', [128, T_LAT], BF16)
        gws = T(lr, 'gws', [128, 4, 64], F32); gwb = T(lr, 'gwb', [128, 4, 64], BF16)
        ltA = [[T(lr, 'lt%d_%d' % (i, r_), [128, 512], F32) for i in range(4)] for r_ in range(3)]
        ltc = {'i': 0}
        for cb in range(8 if nhp == 8 else 1):
            def cons_lx(ps_ap, bn, g0, n, is_ctx):
                S.op('act', lambda e: e.activation(out=zx[:, g0:g0 + n], in_=ps_ap, func=AF.Copy), r=[bn], w=['zx'])

            def cons_lg(ps_ap, bn, g0, n, is_ctx):
                if not is_ctx:
                    S.op('act', lambda e: e.activation(out=glu[:, g0 - T_CTX:g0 - T_CTX + n], in_=ps_ap, func=AF.Gelu_apprx_tanh), r=[bn], w=['glu'])
            project_group([(OFF_LX + cb * 128, 128, cons_lx), (OFF_LG + cb * 128, 128, cons_lg)])
            S.dma('sp', gws[:], gw_d[:, cb, :, :], w=['gws'])
            S.op('pool', lambda e: e.tensor_copy(out=gwb[:], in_=gws[:]), r=['gws'], w=['gwb'])
            S.op('act', lambda e: e.activation(out=xc[:], in_=zx[:], func=AF.Identity, scale=cwT[:, cb, 2:3], bias=cbT[:, cb:cb + 1]), r=['zx', 'cwT', 'cbT'], w=['xc'])
            L0 = T_CTX
            for (o_sl, i_sl, wj) in [(slice(L0 + 128, T_ALL), slice(L0, T_ALL - 128), 0), (slice(L0 + 64, T_ALL), slice(L0, T_ALL - 64), 1),
                                     (slice(L0, T_ALL - 64), slice(L0 + 64, T_ALL), 3),
                                     (slice(2, 256), slice(0, 254), 0), (slice(1, 256), slice(0, 255), 1), (slice(0, 255), slice(1, 256), 3)]:
                S.op('dve', lambda e, o_sl=o_sl, i_sl=i_sl, wj=wj: e.scalar_tensor_tensor(out=xc[:, o_sl], in0=zx[:, i_sl], scalar=cwT[:, cb, wj:wj + 1], in1=xc[:, o_sl],
                                                                                      op0=ALU.mult, op1=ALU.add), r=['zx', 'xc', 'cwT'], w=['xc'])
            S.op('pool', lambda e: e.tensor_copy(out=xcb[:], in_=xc[:]), r=['xc'], w=['xcb'])
            for d in range(2):
                for ti, (g0, n, is_ctx) in enumerate(TT):
                    sl = slice(g0, g0 + n)
                    rot = ltc['i'] % 3; bset = ltc['i'] % 2; ltc['i'] += 1
                    lt = ltA[rot]; L_ = lambda k_: 'lt%d_%d' % (k_, rot)
                    for g_ in range(2):
                        dg = d * 2 + g_
                        bank = PB[g_ + 2 * bset]; bnn = 'pb%d' % (g_ + 2 * bset)
                        for nl in range(2):
                            hs = slice(nl * 64, nl * 64 + 64)
                            S.op('pe', lambda e, hs=hs, dg=dg, bank=bank, sl=sl, n=n: e.matmul(bank[hs, 0:n], lhsT=gwb[hs, dg, :], rhs=xcb[hs, sl], start=True, stop=True),
                                 r=['gwb', 'xcb'], w=[bnn])
                        S.op('act', lambda e, bank=bank, g_=g_, dg=dg, n=n: e.activation(out=lt[g_][:, 0:n], in_=bank[:, 0:n], func=AF.Sigmoid, bias=gbT[:, cb, dg:dg + 1]),
                             r=[bnn, 'gbT'], w=[L_(g_)])
                    if is_ctx:
                        a_out = aa[d][:, sl]; b_out = bx[d][:, sl]; v3 = lambda ap_: ap_
                    else:
                        r0 = (g0 - T_CTX) // 64
                        a_out = aa[d][:, T_CTX:].rearrange("p (c r) -> p r c", r=64)[:, r0:r0 + 8, :]
                        b_out = bx[d][:, T_CTX:].rearrange("p (c r) -> p r c", r=64)[:, r0:r0 + 8, :]
                        v3 = lambda ap_: ap_.rearrange("p (r c) -> p r c", c=64)
                    S.op('act', lambda e, n=n, a_out=a_out, v3=v3: e.activation(out=a_out, in_=v3(lt[0][:, 0:n]), func=AF.Exp, scale=nsp[:, cb, d:d + 1]), r=[L_(0), 'nsp'], w=['aa0'])
                    S.op('act', lambda e, n=n: e.activation(out=lt[2][:, 0:n], in_=lt[0][:, 0:n], func=AF.Exp, scale=nsp2[:, cb, d:d + 1]), r=[L_(0), 'nsp2'], w=[L_(2)])
                    S.op('act', lambda e, n=n: e.activation(out=lt[3][:, 0:n], in_=lt[2][:, 0:n], func=AF.Sqrt, scale=-1.0, bias=1.0), r=[L_(2)], w=[L_(3)])
                    S.op('pool', lambda e, n=n: e.tensor_tensor(out=lt[3][:, 0:n], in0=lt[3][:, 0:n], in1=lt[1][:, 0:n], op=ALU.mult), r=[L_(3), L_(1)], w=[L_(3)])
                    S.op('dve', lambda e, sl=sl, n=n, b_out=b_out, v3=v3: e.tensor_tensor(out=b_out, in0=v3(lt[3][:, 0:n]), in1=v3(xc[:, sl]), op=ALU.mult), r=[L_(3), 'xc'], w=['bx0'])
                A, Bx, Hh = aa[d], bx[d], hh[d]; an, bn2, hn = 'aa0', 'bx0', 'hh%d' % d
                if d == 0:
                    S.op('dve', lambda e: e.tensor_tensor_scan(out=Hh[:, 0:256], data0=A[:, 0:256], data1=Bx[:, 0:256], initial=0.0, op0=ALU.mult, op1=ALU.add), r=[an, bn2], w=[hn])
                    S.op('dve', lambda e: e.tensor_tensor_scan(out=Hh[:, L0:], data0=A[:, L0:], data1=Bx[:, L0:], initial=Hh[:, 255:256], op0=ALU.mult, op1=ALU.add), r=[an, bn2, hn], w=[hn])
                else:
                    S.op('dve', lambda e: e.tensor_tensor_scan(out=Hh[:, 255::-1], data0=A[:, 255::-1], data1=Bx[:, 255::-1], initial=0.0, op0=ALU.mult, op1=ALU.add), r=[an, bn2], w=[hn])
                    S.op('dve', lambda e: e.tensor_tensor_scan(out=Hh[:, T_ALL - 1:L0 - 1:-1], data0=A[:, T_ALL - 1:L0 - 1:-1], data1=Bx[:, T_ALL - 1:L0 - 1:-1], initial=Hh[:, 0:1], op0=ALU.mult, op1=ALU.add),
                         r=[an, bn2, hn], w=[hn])
            S.op('pool', lambda e: e.tensor_tensor(out=hh[0][:, L0:], in0=hh[0][:, L0:], in1=hh[1][:, L0:], op=ALU.add), r=['hh0', 'hh1'], w=['hh0'])
            S.op('dve', lambda e: e.tensor_tensor(out=ybt[:].rearrange("p (r c) -> p r c", c=64), in0=hh[0][:, L0:].rearrange("p (c r) -> p r c", r=64), in1=glu[:].rearrange("p (r c) -> p r c", c=64), op=ALU.mult), r=['hh0', 'glu'], w=['ybt'])
            S.dma('sp', yb_s[cb, :, :], ybt[:], r=['ybt'], w=['yb_s'])
        if stage == 'lru':
            S.dma('sp', ybt[:], yb_s[0, :, :], r=['yb_s'], w=['ybt'])
            S.op('dve', lambda e: e.tensor_copy(out=hh[0][:, 0:T_LAT], in_=ybt[:]), r=['ybt'], w=['hh0'])
            return finish_early(hh[0][:, 0:T_LAT], 128, 1024, 'hh0') if False else finish_rows(hh[0])
        S.barrier(); S.emit()
        lr.close(); open_stacks.pop()

        gates = T(es, 'gates', [128, 32, NEXP], F32)
        mg = ExitStack(); open_stacks.append(mg)
        Wm = {nm: T(mg, 'W_' + nm, [128, 8, D], BF16) for nm in ['ga', 'gb', 'a', 'b', 'o']}
        wstg = T(mg, 'wstg', [128, 8, 256], F32)
        srcs = {'ga': winv[:, :, OFF_GA:OFF_GA + D], 'gb': winv[:, :, OFF_GB:OFF_GB + D],
                'a': wa_d.rearrange("(k p) c -> p k c", p=128), 'b': wb_d.rearrange("(k p) c -> p k c", p=128), 'o': wo_d.rearrange("(k p) c -> p k c", p=128)}
        for nm in ['ga', 'gb', 'a', 'b', 'o']:
            for hf in range(4):
                S.dma('sp', wstg[:], srcs[nm][:, :, hf * 256:(hf + 1) * 256], w=['wstg'])
                S.op('pool', lambda e, nm=nm, hf=hf: e.tensor_copy(out=Wm[nm][:, :, hf * 256:(hf + 1) * 256], in_=wstg[:]), r=['wstg'], w=['W_' + nm])
        rws = T(mg, 'rws', [128, 8, NEXP], F32); rwb = T(mg, 'rwb', [128, 8, NEXP], BF16)
        S.dma('sp', rws[:], rw_d.rearrange("(k p) c -> p k c", p=128), w=['rws'])
        S.op('pool', lambda e: e.tensor_copy(out=rwb[:], in_=rws[:]), r=['rws'], w=['rwb'])
        rbb = T(mg, 'rbb', [128, NEXP], F32); S.dma('sp', rbb[:], rb_d[:, :], w=['rbb'])
        gg = T(mg, 'gg', [128, 2, D], F32); S.dma('sp', gg[:], gt_s[:, :, :], r=['gt_s'], w=['gg'])
        yat = T(mg, 'yat', [128, 8, 512], BF16); ybt2 = T(mg, 'ybt2', [128, 8, 512], BF16)
        mixT = T(mg, 'mixT', [128, 8, 512], BF16); ust = T(mg, 'ust', [128, 8, 512], BF16)
        sgt = [T(mg, 'sgt%d' % i, [128, 512], F32) for i in range(2)]
        mt_ = [T(mg, 'mt%d' % i, [128, 512], F32) for i in range(2)]
        xres = T(mg, 'xres', [128, D], F32); h1t = T(mg, 'h1t', [128, D], F32); tmpy = h1t
        nst = {'ss': T(mg, 'm_ss', [128, 4], F32), 'xs': T(mg, 'm_xs', [128, D], BF16), 'junk': T(mg, 'm_junk', [128, D], BF16)}
        rt = {nm: T(mg, 'rt_' + nm, shp, F32) for nm, shp in [('sc', [128, 64]), ('bi', [128, 64]), ('m8', [128, 8, 8]), ('gs', [128, 8]), ('g8', [128, 8]),
                                                            ('gm', [128, 8]), ('mk', [128, 64]), ('t8', [128, 8]), ('gu', [128, 64]), ('dn', [128, 2]), ('ss', [128, 4])]}
        yav = ya_s.rearrange("h p t -> p h t"); ybv = yb_s.rearrange("h p t -> p h t")
        NT6 = 8 if nhp == 8 else 1
        for tt in range(NT6):
            g0 = T_CTX + tt * 512; l0 = tt * 512
            xt = xnt[tt % 2]; xtn = 'xnt%d' % (tt % 2)
            S.dma('sp', xt[:, 0:4, :], xnv(tt + 1)[:, 0:4, :], r=['xn_s'], w=[xtn + 'a'])
            S.dma('sp', xt[:, 4:8, :], xnv(tt + 1)[:, 4:8, :], r=['xn_s'], w=[xtn + 'b'])
            S.dma('sp', yat[:], yav[:, :, l0:l0 + 512], r=['ya_s'], w=['yat'])
            S.dma('sp', ybt2[:], ybv[:, :, l0:l0 + 512], r=['yb_s'], w=['ybt2'])
            for dc in range(8):
                dsl = slice(dc * 128, (dc + 1) * 128)
                for bi_, (wn, rhs_t, rn) in enumerate([('ga', xt, xtn + 'a'), ('a', yat, 'yat'), ('gb', xt, xtn + 'a'), ('b', ybt2, 'ybt2')]):
                    for k in range(8):
                        S.op('pe', lambda e, bi_=bi_, wn=wn, rhs_t=rhs_t, k=k: e.matmul(PB[bi_][:, :], lhsT=Wm[wn][:, k, dsl], rhs=rhs_t[:, k, :], start=(k == 0), stop=(k == 7)),
                             r=['W_' + wn, rn] + ([xtn + 'b'] if rhs_t is xt else []), w=['pb%d' % bi_])
                S.op('act', lambda e: e.activation(out=sgt[0][:], in_=PB[0][:, :], func=AF.Sigmoid), r=['pb0'], w=['sgt0'])
                S.op('act', lambda e: e.activation(out=sgt[1][:], in_=PB[2][:, :], func=AF.Sigmoid), r=['pb2'], w=['sgt1'])
                S.op('dve', lambda e: e.tensor_tensor(out=mt_[0][:], in0=PB[1][:, :], in1=sgt[0][:], op=ALU.mult), r=['pb1', 'sgt0'], w=['mt0'])
                S.op('dve', lambda e: e.tensor_tensor(out=mt_[1][:], in0=PB[3][:, :], in1=sgt[1][:], op=ALU.mult), r=['pb3', 'sgt1'], w=['mt1'])
                S.op('pool', lambda e, dc=dc: e.tensor_tensor(out=mixT[:, dc, :], in0=mt_[0][:], in1=mt_[1][:], op=ALU.add), r=['mt0', 'mt1'], w=['mixT'])
            for j in range(4):
                tsl = slice(j * 128, (j + 1) * 128); st_i = tt * 4 + j
                row0 = l0 + j * 128
                S.dma('sp', xres[:], x_d[row0:row0 + 128, :], w=['xres'])
                for hf in range(2):
                    for k in range(8):
                        S.op('pe', lambda e, hf=hf, k=k: e.matmul(PB[4 + hf][:, :], lhsT=mixT[:, k, tsl], rhs=Wm['o'][:, k, hf * 512:(hf + 1) * 512], start=(k == 0), stop=(k == 7)),
                             r=['mixT', 'W_o'], w=['pb%d' % (4 + hf)])
                ss = rt['ss']
                for hf in range(2):
                    S.op('act', lambda e, hf=hf: e.activation(out=nst['junk'][:, hf * 512:(hf + 1) * 512], in_=PB[4 + hf][:, :], func=AF.Square, accum_out=ss[:, hf:hf + 1]),
                         r=['pb%d' % (4 + hf)], w=['m_junk', 'rt_ss%d' % hf])
                S.op('dve', lambda e: e.tensor_tensor(out=ss[:, 2:3], in0=ss[:, 0:1], in1=ss[:, 1:2], op=ALU.add), r=['rt_ss0', 'rt_ss1'], w=['rt_ss2'])
                S.op('dve', lambda e: e.tensor_scalar(out=ss[:, 2:3], in0=ss[:, 2:3], scalar1=1.0 / D, scalar2=1e-6, op0=ALU.mult, op1=ALU.add), r=['rt_ss2'], w=['rt_ss2'])
                S.op('act', lambda e: e.activation(out=ss[:, 2:3], in_=ss[:, 2:3], func=AF.Sqrt), r=['rt_ss2'], w=['rt_ss2'])
                S.op('dve', lambda e: e.reciprocal(out=ss[:, 3:4], in_=ss[:, 2:3]), r=['rt_ss2'], w=['rt_ss3'])
                for hf in range(2):
                    hsl = slice(hf * 512, (hf + 1) * 512)
                    S.op('dve', lambda e, hf=hf, hsl=hsl: e.scalar_tensor_tensor(out=tmpy[:, hsl], in0=PB[4 + hf][:, :], scalar=ss[:, 3:4], in1=gg[:, 0, hsl], op0=ALU.mult, op1=ALU.mult),
                         r=['pb%d' % (4 + hf), 'rt_ss3', 'gg'], w=['h1t'])
                S.op('pool', lambda e: e.tensor_tensor(out=h1t[:], in0=tmpy[:], in1=xres[:], op=ALU.add), r=['h1t', 'xres'], w=['h1t'])
                S.dma('sp', h1_s[row0:row0 + 128, :], h1t[:], r=['h1t'], w=['h1_s'])
                norm_to_featT(nst, h1t[:], 'h1t', ust, 'ust', j * 128, lambda k: gs2[:, k:k + 1], lambda k: sh2[:, k:k + 1], 'm_')
                for k in range(8):
                    S.op('pe', lambda e, k=k: e.matmul(PB[6][:, 0:NEXP], lhsT=ust[:, k, tsl], rhs=rwb[:, k, :], start=(k == 0), stop=(k == 7)), r=['ust', 'rwb'], w=['pb6'])
                S.op('act', lambda e: e.activation(out=rt['sc'][:], in_=PB[6][:, 0:NEXP], func=AF.Sigmoid), r=['pb6'], w=['rt_sc'])
                S.op('dve', lambda e: e.tensor_tensor(out=rt['bi'][:], in0=rt['sc'][:], in1=rbb[:], op=ALU.add), r=['rt_sc', 'rbb'], w=['rt_bi'])
                for gI in range(8):
                    S.op('dve', lambda e, gI=gI: e.max(out=rt['m8'][:, gI, :], in_=rt['bi'][:, gI * 8:(gI + 1) * 8]), r=['rt_bi'], w=['rt_m8'])
                S.op('dve', lambda e: e.tensor_tensor(out=rt['gs'][:], in0=rt['m8'][:, :, 0], in1=rt['m8'][:, :, 1], op=ALU.add), r=['rt_m8'], w=['rt_gs'])
                S.op('dve', lambda e: e.max(out=rt['g8'][:], in_=rt['gs'][:]), r=['rt_gs'], w=['rt_g8'])
                S.op('dve', lambda e: e.tensor_scalar(out=rt['gm'][:], in0=rt['gs'][:], scalar1=rt['g8'][:, 3:4], scalar2=None, op0=ALU.is_ge), r=['rt_gs', 'rt_g8'], w=['rt_gm'])
                for gI in range(8):
                    S.op('dve', lambda e, gI=gI: e.tensor_scalar(out=rt['mk'][:, gI * 8:(gI + 1) * 8], in0=rt['bi'][:, gI * 8:(gI + 1) * 8], scalar1=10.0, scalar2=rt['gm'][:, gI:gI + 1],
                                                                op0=ALU.add, op1=ALU.mult), r=['rt_bi', 'rt_gm'], w=['rt_mk'])
                S.op('dve', lambda e: e.max(out=rt['t8'][:], in_=rt['mk'][:]), r=['rt_mk'], w=['rt_t8'])
                S.op('dve', lambda e: e.scalar_tensor_tensor(out=rt['gu'][:], in0=rt['mk'][:], scalar=rt['t8'][:, 5:6], in1=rt['sc'][:], op0=ALU.is_ge, op1=ALU.mult),
                     r=['rt_mk', 'rt_t8', 'rt_sc'], w=['rt_gu'])
                S.op('dve', lambda e: e.tensor_reduce(out=rt['dn'][:, 0:1], in_=rt['gu'][:], axis=AX.X, op=ALU.add), r=['rt_gu'], w=['rt_dn'])
                S.op('dve', lambda e: e.reciprocal(out=rt['dn'][:, 1:2], in_=rt['dn'][:, 0:1]), r=['rt_dn'], w=['rt_dn1'])
                S.op('dve', lambda e, st_i=st_i: e.tensor_scalar(out=gates[:, st_i, :], in0=rt['gu'][:], scalar1=rt['dn'][:, 1:2], scalar2=2.5, op0=ALU.mult, op1=ALU.mult),
                     r=['rt_gu', 'rt_dn1'], w=['gates'])
            S.dma('sp', uT_s[:, :, l0:l0 + 512], ust[:], r=['ust'], w=['uT_s'])
        S.barrier(); S.emit()
        mg.close(); open_stacks.pop()
        if stage == 'merge':
            return finish_rows2(h1_s, gates)

        me = ExitStack(); open_stacks.append(me)
        HT = T_LAT // 2
        uTh = T(me, 'uTh', [128, 8, HT], BF16)
        acc = T(me, 'acc', [128, 16, D], F32)
        gus = T(me, 'gus', [128, 4, 512], F32); dns = T(me, 'dns', [128, 2, 512], F32)
        gub = [T(me, 'gub%d' % i, [128, 8, 512], BF16) for i in range(2)]
        dnb = [T(me, 'dnb%d' % i, [128, 2, D], BF16) for i in range(2)]
        sgm = [T(me, 'sgm%d' % i, [128, 512], F32) for i in range(2)]
        hT = [T(me, 'hT%d' % i, [128, 2, 512], BF16) for i in range(2)]
        gg2 = T(me, 'gg2', [128, D], F32); S.dma('sp', gg2[:], gt_s[:, 1, :], r=['gt_s'], w=['gg2'])
        h1r = T(me, 'h1r', [128, D], F32); ot = T(me, 'ot', [128, D], F32)
        fss = T(me, 'fss', [128, 4], F32); fjk = gub[0][:, 0:2, :].rearrange("p a b -> p (a b)")
        out_toks = []
        NEX = NEXP if nhp == 8 else 2
        ei = 0
        for half in range(2 if nhp == 8 else 1):
            S.dma('sp', uTh[:], uT_s[:, :, half * HT:(half + 1) * HT], r=['uT_s'], w=['uTh'])
            for e_ in [-1] + list(range(NEX)):
                q = ei % 2; ei += 1
                gsrc = (sgu_d if e_ < 0 else egu_d[e_]).rearrange("(k p) c -> p k c", p=128)
                dsrc = (sdn_d if e_ < 0 else edn_d[e_]).rearrange("(k p) c -> p k c", p=128)
                for gh in range(2):
                    if 'w' in MOESKIP and e_ >= 1: break
                    S.dma('sp', gus[:], gsrc[:, gh * 4:(gh + 1) * 4, :], w=['gus'])
                    S.op('pool', lambda e, q=q, gh=gh: e.tensor_copy(out=gub[q][:, gh * 4:(gh + 1) * 4, :], in_=gus[:]), r=['gus'], w=['gub%d' % q])
                for dh in range(2):
                    if 'w' in MOESKIP and e_ >= 1: break
                    S.dma('sp', dns[:], dsrc[:, :, dh * 512:(dh + 1) * 512], w=['dns'])
                    S.op('pool', lambda e, q=q, dh=dh: e.tensor_copy(out=dnb[q][:, :, dh * 512:(dh + 1) * 512], in_=dns[:]), r=['dns'], w=['dnb%d' % q])
                for t4 in range(4):
                    tk = slice(t4 * 512, (t4 + 1) * 512)
                    for fc in range(4):
                        for k in range(8):
                            S.op('pe', lambda e, fc=fc, k=k, q=q: e.matmul(PB[fc][:, :], lhsT=gub[q][:, k, fc * 128:(fc + 1) * 128], rhs=uTh[:, k, tk], start=(k == 0), stop=(k == 7)),
                                 r=['gub%d' % q, 'uTh'], w=['pb%d' % fc])
                    hq = hT[t4 % 2]; hqn = 'hT%d' % (t4 % 2)
                    for fc in range(2):
                        S.op('act', lambda e, fc=fc: e.activation(out=sgm[fc][:], in_=PB[fc][:, :], func=AF.Silu), r=['pb%d' % fc], w=['sgm%d' % fc])
                        S.op('dve', lambda e, fc=fc, hq=hq: e.tensor_tensor(out=hq[:, fc, :], in0=PB[2 + fc][:, :], in1=sgm[fc][:], op=ALU.mult), r=['pb%d' % (2 + fc), 'sgm%d' % fc], w=[hqn])
                    for j in range(4):
                        st_l = t4 * 4 + j; st_g = half * 16 + st_l
                        for hf in range(2):
                            bk = 4 + (j * 2 + hf) % 3; bkn = 'pb%d' % bk
                            for fc in range(2):
                                S.op('pe', lambda e, bk=bk, fc=fc, hf=hf, j=j, hq=hq, q=q: e.matmul(PB[bk][:, :], lhsT=hq[:, fc, j * 128:(j + 1) * 128], rhs=dnb[q][:, fc, hf * 512:(hf + 1) * 512],
                                                                                                 start=(fc == 0), stop=(fc == 1)), r=[hqn, 'dnb%d' % q], w=[bkn])
                            asl = acc[:, st_l, hf * 512:(hf + 1) * 512]
                            if e_ < 0:
                                S.op('act', lambda e, bk=bk, asl=asl: e.activation(out=asl, in_=PB[bk][:, :], func=AF.Copy), r=[bkn], w=['acc%d' % st_l])
                            elif 'a' not in MOESKIP:
                                S.op('dve', lambda e, bk=bk, asl=asl, st_g=st_g, e_=e_: e.scalar_tensor_tensor(out=asl, in0=PB[bk][:, :], scalar=gates[:, st_g, e_:e_ + 1], in1=asl, op0=ALU.mult, op1=ALU.add),
                                     r=[bkn, 'gates', 'acc%d' % st_l], w=['acc%d' % st_l])
            for st_l in range(16):
                row0 = half * HT + st_l * 128
                S.dma('sp', h1r[:], h1_s[row0:row0 + 128, :], r=['h1_s'], w=['h1r'])
                S.op('act', lambda e, st_l=st_l: e.activation(out=fjk, in_=acc[:, st_l, :], func=AF.Square, accum_out=fss[:, 0:1]), r=['acc%d' % st_l], w=['gub0', 'fss0'])
                S.op('dve', lambda e: e.tensor_scalar(out=fss[:, 1:2], in0=fss[:, 0:1], scalar1=1.0 / D, scalar2=1e-6, op0=ALU.mult, op1=ALU.add), r=['fss0'], w=['fss1'])
                S.op('act', lambda e: e.activation(out=fss[:, 2:3], in_=fss[:, 1:2], func=AF.Sqrt), r=['fss1'], w=['fss2'])
                S.op('dve', lambda e: e.reciprocal(out=fss[:, 3:4], in_=fss[:, 2:3]), r=['fss2'], w=['fss3'])
                S.op('dve', lambda e, st_l=st_l: e.scalar_tensor_tensor(out=ot[:], in0=acc[:, st_l, :], scalar=fss[:, 3:4], in1=gg2[:], op0=ALU.mult, op1=ALU.mult),
                     r=['acc%d' % st_l, 'fss3', 'gg2'], w=['ot'])
                S.op('pool', lambda e: e.tensor_tensor(out=ot[:], in0=ot[:], in1=h1r[:], op=ALU.add), r=['ot', 'h1r'], w=['ot'])
                out_toks.append(S.dma('sp', out_d[row0:row0 + 128, :], ot[:], r=['ot'], w=['out']))
        S.wait_all('sp', out_toks)
        S.emit()
        me.close(); open_stacks.pop()
    return nc


def host_layout(inp, b):
    f = lambda a: np.ascontiguousarray(a, dtype=np.float32)
    pk = lambda v: f(np.asarray(v).reshape(-1, 128).T)
    bc = lambda v: f(np.broadcast_to(np.asarray(v)[None], (128,) + np.asarray(v).shape))
    m = {}
    m['x'] = f(inp['x'][b]); m['ctx'] = f(inp['ctx'][b])
    m['cvec'] = f(np.stack([pk(inp['c'][b]), pk(inp['c_ctx'])], axis=-1))
    m['w_mod'] = f(inp['w_mod'][0]); m['b_modT'] = pk(inp['b_mod'][0])
    bm = inp['b_mod'][0]
    m['b_mod_bc'] = bc(np.stack([bm[2048:3072], bm[5120:6144]]))
    ng = inp['norm_g'][0]
    m['norm_gT'] = f(np.stack([pk(ng[i]) for i in range(4)], axis=1))
    m['gpost_bc'] = bc(np.stack([ng[1], ng[3]]))
    m['w_in'] = f(inp['w_in'][0])
    mu = np.zeros((2, 28 * 128), np.float32); mu[:, :3488] = inp['shift_mu'][0]
    m['muT'] = f(np.stack([pk(mu[0]), pk(mu[1])], axis=-1))
    m['w0T'] = f(np.stack([pk(inp['rw_w0'][0][d]) for d in range(2)], axis=-1))
    m['a0T'] = f(np.stack([pk(inp['rw_a0'][0][d]) for d in range(2)], axis=-1))
    m['kkT'] = pk(inp['rw_k_k'][0]); m['kaT'] = pk(inp['rw_k_a'][0]); m['rkT'] = pk(inp['rw_r_k'][0].reshape(-1))
    m['lnx_bc'] = bc(inp['rw_lnx'][0])
    m['wupT'] = f(inp['rw_w_up'][0].reshape(128, D)); m['aupT'] = f(inp['rw_a_up'][0].reshape(128, D)); m['g_up'] = f(inp['rw_g_up'][0])
    m['cwT'] = f(np.stack([pk(inp['lru_conv_w'][0][j]) for j in range(4)], axis=-1)); m['cbT'] = pk(inp['lru_conv_b'][0])
    gb = inp['lru_gate_b'][0].reshape(4, D)
    m['gbT'] = f(np.stack([pk(gb[i]) for i in range(4)], axis=-1))
    m['llT'] = f(np.stack([pk(inp['lru_l'][0][d]) for d in range(2)], axis=-1))
    gw = inp['lru_gate_w'][0].reshape(4, 8, 2, 64, 64)
    m['gwT'] = f(np.transpose(gw, (2, 3, 1, 0, 4)).reshape(128, 8, 4, 64))
    m['w_branch_a'] = f(inp['w_branch_a'][0]); m['w_branch_b'] = f(inp['w_branch_b'][0]); m['w_out'] = f(inp['w_out'][0])
    m['router_w'] = f(inp['router_w'][0]); m['router_b_bc'] = bc(inp['router_b'][0])
    m['ex_w_gu'] = f(inp['ex_w_gu'][0]); m['ex_w_down'] = f(inp['ex_w_down'][0])
    m['sh_w_gu'] = f(inp['sh_w_gu'][0]); m['sh_w_down'] = f(inp['sh_w_down'][0])
    p = np.arange(128)[:, None]; c = np.arange(128)[None, :]
    cs = np.zeros((128, 7, 128), np.float32)
    cs[:, 0] = (p == c); cs[:, 1] = (p < c); cs[:, 2] = (p <= c); cs[:, 3] = (p > c); cs[:, 4] = (p >= c)
    cs[:, 5] = ((p // 64) == (c // 64)); cs[:, 6] = 1.0
    m['consts'] = cs
    return m


_NC = {}


def kernel(**inputs):
    inp = {k: np.asarray(v) for k, v in inputs.items()}
    if 'full' not in _NC:
        _NC['full'] = build('full')
    nc = _NC['full']
    in_maps = [host_layout(inp, b) for b in range(8)]
    res = run_bass_kernel_spmd(nc, in_maps, core_ids=list(range(8)))
    return np.stack([np.asarray(r['out'], dtype=np.float32) for r in res.results], axis=0)
```

```python
import numpy as np
import os
from contextlib import ExitStack
DBGSKIP = os.environ.get('DBGSKIP', '')
MOESKIP = os.environ.get('MOESKIP', '')
LOWP_FROM = int(os.environ.get('LOWP_FROM', '6'))
import concourse.bass as bass
import concourse.mybir as mybir
from concourse.bass_utils import run_bass_kernel_spmd

F32 = mybir.dt.float32
BF16 = mybir.dt.bfloat16
AF = mybir.ActivationFunctionType
ALU = mybir.AluOpType
AX = mybir.AxisListType

D = 1024
T_CTX = 256
T_LAT = 4096
T_ALL = T_CTX + T_LAT
NCH = T_ALL // 128
IN_COLS = 7584
OFF_WD, OFF_AD, OFF_GD, OFF_LX, OFF_LG, OFF_GA, OFF_GB = 3072, 3200, 3328, 3488, 4512, 5536, 6560
NEXP = 64
TT = [(0, 256, True)] + [(256 + i * 512, 512, False) for i in range(8)]


class _Rec:
    def __getattr__(self, name):
        def f(*a, **k):
            return (name, a, k)
        return f


_REC = _Rec()


class Sched:
    ENG = ['pe', 'act', 'dve', 'pool', 'sp']

    def __init__(self, nc, es, ndma=16):
        self.nc = nc
        self.ops = {e: [] for e in self.ENG}
        self.cnt = {e: 0 for e in self.ENG}
        self.sem = {e: es.enter_context(nc.semaphore('s_' + e)) for e in self.ENG}
        self.dsem = {e: [es.enter_context(nc.semaphore('d_%s%d' % (e, i))) for i in range(ndma)]
                     for e in ('sp', 'act')}
        self.dcnt = {e: 0 for e in self.dsem}
        self.dval = {e: [0] * ndma for e in self.dsem}
        self.lastw = {}
        self.readers = {}
        self.waited = {e: {} for e in self.ENG}
        self.ndma = ndma

    def _deps(self, eng, r, w, is_dma):
        deps = []
        for x in r:
            lw = self.lastw.get(x)
            if lw is not None:
                deps.append(lw)
        for x in w:
            lw = self.lastw.get(x)
            if lw is not None and (is_dma or lw[0] != eng or lw[3]):
                deps.append(lw)
            for rd in self.readers.get(x, ()):
                if is_dma or rd[0] != eng or rd[3]:
                    deps.append(rd)
        out = []
        wd = self.waited[eng]
        for (pe_, sem, val, pdma) in deps:
            if pe_ == 'pe' and eng == 'pe' and not pdma and not is_dma:
                continue
            k = id(sem)
            if wd.get(k, 0) >= val:
                continue
            wd[k] = val
            out.append((sem, val))
        return out

    def _commit(self, tok, r, w):
        for x in r:
            self.readers.setdefault(x, []).append(tok)
        for x in w:
            self.lastw[x] = tok
            self.readers[x] = []

    def op(self, eng, fn, r=(), w=()):
        waits = self._deps(eng, r, w, False)
        self.cnt[eng] += 1
        tok = (eng, self.sem[eng], self.cnt[eng], False)
        self.ops[eng].append((fn(_REC), waits, self.sem[eng], 1))
        self._commit(tok, r, w)

    def dma(self, q, out, in_, r=(), w=(), **kw):
        waits = self._deps(q, r, w, True)
        i = self.dcnt[q]
        self.dcnt[q] += 1
        slot = i % self.ndma
        sem = self.dsem[q][slot]
        prev = self.dval[q][slot]
        if prev > 0 and self.waited[q].get(id(sem), 0) < prev:
            self.waited[q][id(sem)] = prev
            waits.append((sem, prev))
        self.dval[q][slot] = prev + 16
        tok = (q, sem, prev + 16, True)
        self.ops[q].append((('dma_start', (), dict(out=out, in_=in_, **kw)), waits, sem, 16))
        self._commit(tok, r, w)
        return tok

    def wait_all(self, eng, toks):
        self.ops[eng].append((None, [(t[1], t[2]) for t in toks], None, 0))

    def barrier(self):
        allw = [(self.sem[e], self.cnt[e]) for e in self.ENG if self.cnt[e] > 0]
        for q in self.dsem:
            for i in range(self.ndma):
                if self.dval[q][i] > 0:
                    allw.append((self.dsem[q][i], self.dval[q][i]))
        for e in self.ENG:
            waits = []
            for (sem, val) in allw:
                if self.waited[e].get(id(sem), 0) < val:
                    self.waited[e][id(sem)] = val
                    waits.append((sem, val))
            self.ops[e].append((None, waits, None, 0))

    def emit(self):
        with self.nc.Block() as block:
            def run(e, name):
                for call, waits, sem, inc in self.ops[name]:
                    for (s, v) in waits:
                        e.wait_ge(s, v)
                    if call is not None:
                        getattr(e, call[0])(*call[1], **call[2]).then_inc(sem, inc)
                self.ops[name] = []

            @block.tensor
            def _(e):
                run(e, 'pe')

            @block.scalar
            def _(e):
                run(e, 'act')

            @block.vector
            def _(e):
                run(e, 'dve')

            @block.gpsimd
            def _(e):
                run(e, 'pool')

            @block.sync
            def _(e):
                run(e, 'sp')


def build(stage='full', nhp=8):
    nc = bass.Bass("TRN2", target_bir_lowering=False)
    di = lambda name, shape, dt=F32: nc.dram_tensor(name, list(shape), dt, kind="ExternalInput").ap()
    x_d = di('x', [T_LAT, D]); ctx_d = di('ctx', [T_CTX, D])
    cvec_d = di('cvec', [128, 8, 2]); wmod_d = di('w_mod', [D, 6 * D]); bmod_d = di('b_modT', [128, 48])
    bmodbc_d = di('b_mod_bc', [128, 2, D])
    ngT_d = di('norm_gT', [128, 4, 8]); gpost_d = di('gpost_bc', [128, 2, D])
    win_d = di('w_in', [D, IN_COLS]); mu_d = di('muT', [128, 28, 2])
    w0_d = di('w0T', [128, 8, 2]); a0_d = di('a0T', [128, 8, 2]); kk_d = di('kkT', [128, 8]); ka_d = di('kaT', [128, 8])
    rk_d = di('rkT', [128, 8]); lnx_d = di('lnx_bc', [128, 2, D])
    wup_d = di('wupT', [128, D]); aup_d = di('aupT', [128, D]); gup_d = di('g_up', [160, D])
    cw_d = di('cwT', [128, 8, 4]); cb_d = di('cbT', [128, 8]); gb_d = di('gbT', [128, 8, 4]); ll_d = di('llT', [128, 8, 2])
    gw_d = di('gwT', [128, 8, 4, 64])
    wa_d = di('w_branch_a', [D, D]); wb_d = di('w_branch_b', [D, D]); wo_d = di('w_out', [D, D])
    rw_d = di('router_w', [D, NEXP]); rb_d = di('router_b_bc', [128, NEXP])
    egu_d = di('ex_w_gu', [NEXP, D, 512]); edn_d = di('ex_w_down', [NEXP, 256, D])
    sgu_d = di('sh_w_gu', [D, 512]); sdn_d = di('sh_w_down', [256, D])
    cst_d = di('consts', [128, 7, 128])
    out_d = nc.dram_tensor('out', [T_LAT, D], F32, kind="ExternalOutput").ap()
    ya_s = nc.dram_tensor('ya_s', [8, 128, T_LAT], BF16, kind="Internal").ap()
    yb_s = nc.dram_tensor('yb_s', [8, 128, T_LAT], BF16, kind="Internal").ap()
    h1_s = nc.dram_tensor('h1_s', [T_LAT, D], F32, kind="Internal").ap()
    uT_s = nc.dram_tensor('uT_s', [128, 8, T_LAT], BF16, kind="Internal").ap()
    xn_s = nc.dram_tensor('xn_s', [9, 128, 8 * 512], BF16, kind="Internal").ap()
    xnv = lambda ti: xn_s[ti].rearrange("p (k t) -> p k t", t=512)
    sgd_s = nc.dram_tensor('sgd_s', [2, 128, T_ALL], BF16, kind="Internal").ap()
    gt_s = nc.dram_tensor('gt_s', [128, 2, D], F32, kind="Internal").ap()
    dbg_d = None
    if stage != 'full':
        dbg_d = nc.dram_tensor('dbg', [T_LAT, D], F32, kind="ExternalOutput").ap()

    with ExitStack() as es:
        S = Sched(nc, es)

        def T(st, name, shape, dt):
            return st.enter_context(nc.sbuf_tensor('t_' + name, list(shape), dt))

        open_stacks = []

        def finish_early(src_ap, rows, cols, rname):
            ps = ExitStack(); open_stacks.append(ps)
            if True:
                df_ = T(ps, 'dbg_f', [128, 8], F32)
                S.op('pool', lambda e: e.memset(df_[:], 0.0), w=['dbg_f'])
                toks = [S.dma('sp', dbg_d[0:rows, 0:cols], src_ap, r=[rname], w=['dbg']),
                        S.dma('sp', out_d[0:128, 0:8], df_[:], r=['dbg_f'], w=['out'])]
                S.wait_all('sp', toks)
                S.emit()
            for st_ in reversed(open_stacks):
                st_.close()
            return nc

        def finish_early4(ys):
            ps = ExitStack(); open_stacks.append(ps)
            df_ = T(ps, 'dbg_f', [128, 8], F32)
            S.op('pool', lambda e: e.memset(df_[:], 0.0), w=['dbg_f'])
            toks = [S.dma('sp', dbg_d[0:128, :], ys[:, 0:8, :].rearrange("p a b -> p (a b)"), r=['ysum'], w=['dbg']),
                    S.dma('sp', dbg_d[128:256, :], ys[:, 8:16, :].rearrange("p a b -> p (a b)"), r=['ysum'], w=['dbg']),
                    S.dma('sp', dbg_d[256:384, :], ys[:, 16:24, :].rearrange("p a b -> p (a b)"), r=['ysum'], w=['dbg']),
                    S.dma('sp', dbg_d[384:512, :], ys[:, 24:32, :].rearrange("p a b -> p (a b)"), r=['ysum'], w=['dbg']),
                    S.dma('sp', out_d[0:128, 0:8], df_[:], r=['dbg_f'], w=['out'])]
            S.wait_all('sp', toks)
            S.emit()
            for st_ in reversed(open_stacks):
                st_.close()
            return nc

        def finish_rows(tile_):
            ps = ExitStack(); open_stacks.append(ps)
            df_ = T(ps, 'dbg_f', [128, 8], F32)
            S.op('pool', lambda e: e.memset(df_[:], 0.0), w=['dbg_f'])
            toks = [S.dma('sp', dbg_d.rearrange("(a b) d -> a (b d)", b=4)[0:128, :], tile_[:, 0:T_LAT], r=['hh0'], w=['dbg']),
                    S.dma('sp', out_d[0:128, 0:8], df_[:], r=['dbg_f'], w=['out'])]
            S.wait_all('sp', toks); S.emit()
            for st_ in reversed(open_stacks):
                st_.close()
            return nc

        def finish_rows2(h1s, gts):
            ps = ExitStack(); open_stacks.append(ps)
            df_ = T(ps, 'dbg_f', [128, D], F32)
            S.dma('sp', df_[:], h1s[0:128, :], r=['h1_s'], w=['dbg_f'])
            toks = [S.dma('sp', dbg_d[0:128, :], df_[:], r=['dbg_f'], w=['dbg']),
                    S.dma('sp', dbg_d[128:256, 0:256], gts[:, 0:4, :].rearrange("p a b -> p (a b)"), r=['gates'], w=['dbg']),
                    S.dma('sp', out_d[0:128, 0:8], df_[:, 0:8], r=['dbg_f'], w=['out'])]
            S.wait_all('sp', toks); S.emit()
            for st_ in reversed(open_stacks):
                st_.close()
            return nc

        PB = [es.enter_context(nc.psum_tensor('pb%d' % i, [128, 512], F32)) for i in range(7)]
        PT = es.enter_context(nc.psum_tensor('pbT', [128, 1024], BF16))

        cst = T(es, 'cst', [128, 7, 128], F32)
        cstb = T(es, 'cstb', [128, 7, 128], BF16)
        S.dma('sp', cst[:], cst_d[:, :, :], w=['cst'])
        S.op('dve', lambda e: e.tensor_copy(out=cstb[:], in_=cst[:]), r=['cst'], w=['cstb'])
        identb = cstb[:, 0, :]; identf = cst[:, 0, :]
        MASK = {'SU': cst[:, 1, :], 'IU': cst[:, 2, :], 'SL': cst[:, 3, :], 'IL': cst[:, 4, :]}
        bonesb = cstb[:, 5, :]; onesf = cst[:, 6, :]
        mG = T(es, 'mG', [128, 2, 512], BF16)
        for d_, (s_, i_) in enumerate([('SU', 'IU'), ('SL', 'IL')]):
            for q_, nm in enumerate([s_, i_, s_, i_]):
                S.op('pool', lambda e, d_=d_, q_=q_, nm=nm: e.tensor_copy(out=mG[:, d_, q_ * 128:(q_ + 1) * 128], in_=MASK[nm]),
                     r=['cst'], w=['mG'])
        mN = [MASK['SL'], MASK['SU']]

        def small(name, src, shape):
            t = T(es, name, shape, F32)
            S.dma('sp', t[:], src, w=[name])
            return t
        cvec = small('cvec', cvec_d[:, :, :], [128, 8, 2])
        bmodT = small('bmodT', bmod_d[:, :], [128, 48])
        ngT = small('ngT', ngT_d[:, :, :], [128, 4, 8])
        muT = small('muT', mu_d[:, :, :], [128, 28, 2])
        w0T = small('w0T', w0_d[:, :, :], [128, 8, 2]); a0T = small('a0T', a0_d[:, :, :], [128, 8, 2])
        kkT = small('kkT', kk_d[:, :], [128, 8]); kaT = small('kaT', ka_d[:, :], [128, 8]); rkT = small('rkT', rk_d[:, :], [128, 8])
        cwT = small('cwT', cw_d[:, :, :], [128, 8, 4]); cbT = small('cbT', cb_d[:, :], [128, 8])
        gbT = small('gbT', gb_d[:, :, :], [128, 8, 4]); llT = small('llT', ll_d[:, :, :], [128, 8, 2])
        SM = ['cvec', 'bmodT', 'ngT', 'muT', 'w0T', 'a0T', 'kkT', 'kaT', 'rkT', 'cwT', 'cbT', 'gbT', 'llT']

        cmu = T(es, 'cmu', [128, 28], F32)
        S.op('dve', lambda e: e.tensor_tensor(out=cmu[:], in0=muT[:, :, 0], in1=muT[:, :, 1], op=ALU.add), r=['muT'], w=['cmu'])
        S.op('dve', lambda e: e.tensor_scalar(out=cmu[:], in0=cmu[:], scalar1=-1.0, scalar2=1.0, op0=ALU.mult, op1=ALU.add), r=['cmu'], w=['cmu'])
        nw0 = T(es, 'nw0', [128, 8, 2], F32)
        S.op('dve', lambda e: e.tensor_scalar(out=nw0[:], in0=w0T[:], scalar1=-1.0, scalar2=None, op0=ALU.mult), r=['w0T'], w=['nw0'])
        oka = T(es, 'oka', [128, 8], F32)
        S.op('dve', lambda e: e.tensor_scalar(out=oka[:], in0=kaT[:], scalar1=-1.0, scalar2=1.0, op0=ALU.mult, op1=ALU.add), r=['kaT'], w=['oka'])
        nsp = T(es, 'nsp', [128, 8, 2], F32)
        S.op('act', lambda e: e.activation(out=nsp[:], in_=llT[:], func=AF.Softplus, scale=-1.0), r=['llT'], w=['nsp'])
        S.op('dve', lambda e: e.tensor_scalar(out=nsp[:], in0=nsp[:], scalar1=-8.0, scalar2=None, op0=ALU.mult), r=['nsp'], w=['nsp'])
        nsp2 = T(es, 'nsp2', [128, 8, 2], F32)
        S.op('dve', lambda e: e.tensor_scalar(out=nsp2[:], in0=nsp[:], scalar1=2.0, scalar2=None, op0=ALU.mult), r=['nsp'], w=['nsp2'])

        scT = T(es, 'scT', [128, 8, 2], F32)
        S.op('act', lambda e: e.activation(out=scT[:], in_=cvec[:], func=AF.Silu), r=['cvec'], w=['scT'])
        modT = T(es, 'modT', [128, 48, 2], F32)
        with ExitStack() as ps:
            gt_bc = T(ps, 'gt_bc', [128, 2, D], F32)
            scbc = T(ps, 'scbc', [128, 8, 128], F32)
            for k in range(8):
                S.op('act', lambda e, k=k: e.activation(out=scbc[:, k, :], in_=onesf, func=AF.Identity, scale=scT[:, k, 0:1]),
                     r=['cst', 'scT'], w=['scbc'])
            wms = [T(ps, 'wms%d' % i, [128, 8, 512], F32) for i in range(2)]
            wmv = wmod_d.rearrange("(k p) c -> p k c", p=128)
            for g in range(12):
                wm = wms[g % 2]; wn = 'wms%d' % (g % 2)
                S.dma('sp', wm[:], wmv[:, :, g * 512:(g + 1) * 512], w=[wn])
                bank = PB[g % 2]; bn = 'pb%d' % (g % 2)
                for sub in range(4):
                    j = g * 4 + sub
                    for k in range(8):
                        S.op('pe', lambda e, wm=wm, sub=sub, k=k, bank=bank: e.matmul(
                            bank[:, sub * 2:sub * 2 + 2], lhsT=wm[:, k, sub * 128:(sub + 1) * 128], rhs=scT[:, k, :],
                            start=(k == 0), stop=(k == 7)), r=[wn, 'scT'], w=[bn])
                    S.op('dve', lambda e, j=j, sub=sub, bank=bank: e.tensor_scalar(
                        out=modT[:, j, :], in0=bank[:, sub * 2:sub * 2 + 2], scalar1=bmodT[:, j:j + 1], scalar2=None, op0=ALU.add),
                        r=[bn, 'bmodT'], w=['modT'])
                if g // 2 in (2, 5):
                    which = 0 if g < 6 else 1
                    half = g % 2
                    bk = PB[2 + half]; bkn = 'pb%d' % (2 + half)
                    for k in range(8):
                        S.op('pe', lambda e, wm=wm, k=k, bk=bk: e.matmul(bk[:, :], lhsT=scbc[:, k, :], rhs=wm[:, k, :],
                                                                         start=(k == 0), stop=(k == 7)), r=[wn, 'scbc'], w=[bkn])
                    S.op('act', lambda e, which=which, half=half, bk=bk: e.activation(
                        out=gt_bc[:, which, half * 512:(half + 1) * 512], in_=bk[:, :], func=AF.Copy), r=[bkn], w=['gt_bc'])
            bmbc = T(ps, 'bmbc', [128, 2, D], F32); gpbc = T(ps, 'gpbc', [128, 2, D], F32)
            S.dma('sp', bmbc[:], bmodbc_d[:, :, :], w=['bmbc']); S.dma('sp', gpbc[:], gpost_d[:, :, :], w=['gpbc'])
            S.op('dve', lambda e: e.tensor_tensor(out=gt_bc[:], in0=gt_bc[:], in1=bmbc[:], op=ALU.add), r=['gt_bc', 'bmbc'], w=['gt_bc'])
            S.op('dve', lambda e: e.tensor_tensor(out=gt_bc[:], in0=gt_bc[:], in1=gpbc[:], op=ALU.mult), r=['gt_bc', 'gpbc'], w=['gt_bc'])
            S.dma('sp', gt_s[:, :, :], gt_bc[:], r=['gt_bc'], w=['gt_s'])
            S.barrier(); S.emit()
        gs1 = T(es, 'gs1', [128, 8, 2], F32); sh1 = T(es, 'sh1', [128, 8, 2], F32)
        gs2 = T(es, 'gs2', [128, 8], F32); sh2 = T(es, 'sh2', [128, 8], F32)
        for ci in range(2):
            S.op('dve', lambda e, ci=ci: e.scalar_tensor_tensor(out=gs1[:, :, ci], in0=modT[:, 8:16, ci], scalar=1.0, in1=ngT[:, 0, :],
                                                                 op0=ALU.add, op1=ALU.mult), r=['modT', 'ngT'], w=['gs1'])
            S.op('dve', lambda e, ci=ci: e.tensor_copy(out=sh1[:, :, ci], in_=modT[:, 0:8, ci]), r=['modT'], w=['sh1'])
        S.op('dve', lambda e: e.scalar_tensor_tensor(out=gs2[:], in0=modT[:, 32:40, 0], scalar=1.0, in1=ngT[:, 2, :],
                                                     op0=ALU.add, op1=ALU.mult), r=['modT', 'ngT'], w=['gs2'])
        S.op('dve', lambda e: e.tensor_copy(out=sh2[:], in_=modT[:, 24:32, 0]), r=['modT'], w=['sh2'])

        if stage == 'p1':
            return finish_early(modT[:].rearrange("p a b -> p (a b)"), 128, 96, 'modT')
        def norm_to_featT(st, xt, xtn, dstT, dstn, col0, gs_ap, sh_ap, tag):
            ss = st['ss']; xs = st['xs']
            S.op('act', lambda e: e.activation(out=st['junk'][:], in_=xt, func=AF.Square, accum_out=ss[:, 0:1]),
                 r=[xtn], w=[tag + 'junk', tag + 'ss'])
            S.op('dve', lambda e: e.tensor_scalar(out=ss[:, 1:2], in0=ss[:, 0:1], scalar1=1.0 / D, scalar2=1e-6, op0=ALU.mult, op1=ALU.add),
                 r=[tag + 'ss'], w=[tag + 'ss1'])
            S.op('act', lambda e: e.activation(out=ss[:, 2:3], in_=ss[:, 1:2], func=AF.Sqrt), r=[tag + 'ss1'], w=[tag + 'ss2'])
            S.op('dve', lambda e: e.reciprocal(out=ss[:, 3:4], in_=ss[:, 2:3]), r=[tag + 'ss2'], w=[tag + 'ss3'])
            S.op('dve', lambda e: e.tensor_scalar(out=xs[:], in0=xt, scalar1=ss[:, 3:4], scalar2=None, op0=ALU.mult),
                 r=[xtn, tag + 'ss3'], w=[tag + 'xs'])
            for k in range(8):
                S.op('pe', lambda e, k=k: e.transpose(out=PT[:, k * 128:(k + 1) * 128], in_=xs[:, k * 128:(k + 1) * 128], identity=identb),
                     r=[tag + 'xs', 'cstb'], w=['pbT'])
            for k in range(8):
                S.op('act', lambda e, k=k: e.activation(out=dstT[:, k, col0:col0 + 128], in_=PT[:, k * 128:(k + 1) * 128], func=AF.Identity,
                                                        scale=gs_ap(k), bias=sh_ap(k)), r=['pbT', 'gs1', 'sh1', 'gs2', 'sh2'], w=[dstn])

        xnt = [T(es, 'xnt%d' % i, [128, 8, 512], BF16) for i in range(2)]
        with ExitStack() as ps:
            xin = [T(ps, 'xin%d' % i, [128, D], F32) for i in range(2)]
            st = {'ss': T(ps, 'n_ss', [128, 4], F32), 'xs': T(ps, 'n_xs', [128, D], BF16), 'junk': T(ps, 'n_junk', [128, D], BF16)}
            for tti, (g0, n, is_ctx) in enumerate(TT):
                stg = xnt[tti % 2]; sn = 'xnt%d' % (tti % 2) + 'a'
                for j in range(n // 128):
                    ti = g0 // 128 + j
                    xt = xin[ti % 2]; xn_ = 'xin%d' % (ti % 2)
                    src = ctx_d[ti * 128:(ti + 1) * 128, :] if ti < 2 else x_d[(ti - 2) * 128:(ti - 1) * 128, :]
                    S.dma('sp', xt[:], src, w=[xn_])
                    ci = 1 if ti < 2 else 0
                    norm_to_featT(st, xt[:], xn_, stg, sn, j * 128,
                                  lambda k, ci=ci: gs1[:, k, ci:ci + 1], lambda k, ci=ci: sh1[:, k, ci:ci + 1], 'n_')
                S.dma('sp', xnv(tti)[:, :, 0:n], stg[:, :, 0:n], r=[sn], w=['xn_s'])
            S.barrier(); S.emit()

        if stage == 'p2':
            xf_ = T(es, 'xf_dbg', [128, 512], F32)
            S.dma('sp', xnt[0][:, :, 0:512], xnv(1)[:, :, 0:512], r=['xn_s'], w=['xnt0a', 'xnt0b'])
            S.op('dve', lambda e: e.tensor_copy(out=xf_[:], in_=xnt[0][:, 3, :]), r=['xnt0'], w=['xf_dbg'])
            return finish_early(xf_[:], 128, 512, 'xf_dbg')
        winv = win_d.rearrange("(k p) c -> p k c", p=128)
        wst = [T(es, 'wst%d' % i, [128, 8, 128], F32) for i in range(2)]
        wbf = T(es, 'wbf', [128, 8, 512], BF16)
        pj = {'i': 0, 'bank': 0, 'x': 0}

        def project_group(chunks, banks=(4, 5, 6)):
            offs = []
            o = 0
            for (col0, m, _) in chunks:
                i = pj['i']; pj['i'] += 1
                ws = wst[i % 2]
                S.dma('sp', ws[:, :, 0:m], winv[:, :, col0:col0 + m], w=['wst%d' % (i % 2)])
                S.op('pool', lambda e, ws=ws, o=o, m=m: e.tensor_copy(out=wbf[:, :, o:o + m], in_=ws[:, :, 0:m]), r=['wst%d' % (i % 2)], w=['wbf'])
                offs.append(o); o += m
            for ti, (g0, n, is_ctx) in enumerate(TT):
                xi = pj['x'] % 2; pj['x'] += 1
                xt = xnt[xi]; xtn = 'xnt%d' % xi
                S.dma('sp', xt[:, 0:4, 0:n], xnv(ti)[:, 0:4, 0:n], r=['xn_s'], w=[xtn + 'a'])
                S.dma('sp', xt[:, 4:8, 0:n], xnv(ti)[:, 4:8, 0:n], r=['xn_s'], w=[xtn + 'b'])
                for (col0, m, consumer), o in zip(chunks, offs):
                    bi = banks[pj['bank'] % len(banks)]; pj['bank'] += 1
                    bank = PB[bi]; bn = 'pb%d' % bi
                    for k in range(8):
                        S.op('pe', lambda e, k=k, bank=bank, n=n, m=m, o=o, xt=xt: e.matmul(bank[0:m, 0:n], lhsT=wbf[:, k, o:o + m], rhs=xt[:, k, 0:n],
                                                                                          start=(k == 0), stop=(k == 7)),
                             r=['wbf', xtn + ('a' if k < 4 else 'b')], w=[bn])
                    consumer(bank[0:m, 0:n], bn, g0, n, is_ctx)

        sh_t1 = [T(es, 'sh_t1_%d' % i, [128, 512], F32) for i in range(2)]
        shc = {'i': 0}

        def shift_consume(m, ci, final):
            def cons(ps_ap, bn, g0, n, is_ctx):
                i = shc['i']; shc['i'] += 1
                t1 = sh_t1[i % 2]; tn = 'sh_t1_%d' % (i % 2)
                rw = n if is_ctx else 64
                S.op('act', lambda e: e.activation(out=t1[0:m, 0:n], in_=ps_ap, func=AF.Identity, scale=cmu[0:m, ci:ci + 1]),
                     r=[bn, 'cmu'], w=[tn])
                zv = ps_ap.rearrange("p (r c) -> p r c", c=rw)
                tv = t1[0:m, 0:n].rearrange("p (r c) -> p r c", c=rw)
                S.op('dve', lambda e: e.scalar_tensor_tensor(out=tv[:, :, 1:rw], in0=zv[:, :, 0:rw - 1], scalar=muT[0:m, ci, 0:1], in1=tv[:, :, 1:rw],
                                                             op0=ALU.mult, op1=ALU.add), r=[bn, tn, 'muT'], w=[tn])
                S.op('dve', lambda e: e.scalar_tensor_tensor(out=tv[:, :, 0:rw - 1], in0=zv[:, :, 1:rw], scalar=muT[0:m, ci, 1:2], in1=tv[:, :, 0:rw - 1],
                                                             op0=ALU.mult, op1=ALU.add), r=[bn, tn, 'muT'], w=[tn])
                final(t1[0:m, 0:n], tn, g0, n)
            return cons

        rw = ExitStack(); open_stacks.append(rw)
        twd = T(rw, 'twd', [128, T_ALL], BF16); adT = T(rw, 'adT', [128, T_ALL], BF16)
        sgst = [T(rw, 'sgst%d' % i, [128, 512], BF16) for i in range(2)]
        sgt = T(rw, 'sgt', [128, 2, 512], BF16)
        sgc = {'i': 0}

        def sg_final(ch, m):
            def f(t1, tn, g0, n):
                i = sgc['i']; sgc['i'] += 1
                st_ = sgst[i % 2]; sn_ = 'sgst%d' % (i % 2)
                S.op('act', lambda e: e.activation(out=st_[0:m, 0:n], in_=t1, func=AF.Sigmoid), r=[tn], w=[sn_])
                S.dma('sp', sgd_s[ch, 0:m, g0:g0 + n], st_[0:m, 0:n], r=[sn_], w=['sgd_s'])
            return f
        project_group([
            (OFF_WD, 128, shift_consume(128, 24, lambda t1, tn, g0, n: S.op(
                'act', lambda e: e.activation(out=twd[:, g0:g0 + n], in_=t1, func=AF.Tanh), r=[tn], w=['twd']))),
            (OFF_AD, 128, shift_consume(128, 25, lambda t1, tn, g0, n: S.op(
                'pool', lambda e: e.tensor_copy(out=adT[:, g0:g0 + n], in_=t1), r=[tn], w=['adT']))),
            (OFF_GD, 128, shift_consume(128, 26, sg_final(0, 128))),
            (OFF_GD + 128, 32, shift_consume(32, 27, sg_final(1, 32)))])
        if stage == 'p3':
            xf_ = T(rw, 'xf_dbg', [128, 512], F32)
            S.op('dve', lambda e: e.tensor_copy(out=xf_[:], in_=twd[:, 256:768]), r=['twd'], w=['xf_dbg'])
            return finish_early(xf_[:], 128, 512, 'xf_dbg')
        ysum = T(rw, 'ysum', [128, 32, 128], F32)
        lw_st = ysum[:, 0:8, :].rearrange("p a b -> p (a b)"); wupb = T(rw, 'wupb', [128, D], BF16); aupb = T(rw, 'aupb', [128, D], BF16)
        gupb = T(rw, 'gupb', [128, 2, D], BF16)
        for src_, dst_, np_ in [(wup_d[:, :], wupb[:], 128), (aup_d[:, :], aupb[:], 128), (gup_d[0:128, :], gupb[:, 0, :], 128), (gup_d[128:160, :], gupb[0:32, 1, :], 32)]:
            S.dma('sp', lw_st[0:np_, :], src_, w=['ysum'])
            S.op('pool', lambda e, dst_=dst_, np_=np_: e.tensor_copy(out=dst_, in_=lw_st[0:np_, :]), r=['ysum'], w=['lwdst'])
        lnxbc = T(rw, 'lnxbc', [128, 2, 128], F32)

        rb = T(rw, 'rb', [128, T_ALL], BF16); kb = T(rw, 'kb', [128, T_ALL], BF16); vb = T(rw, 'vb', [128, T_ALL], BF16)
        kkb = T(rw, 'kkb', [128, T_ALL], BF16); ksum = T(rw, 'ksum', [128, T_LAT], BF16)
        Vm = T(rw, 'Vm', [128, NCH, 128], BF16)
        yaT = [T(rw, 'yaT%d' % i, [128, 512], BF16) for i in range(2)]
        PL = T(rw, 'PL', [128, 2, NCH], F32)
        seg_sh = {}
        for nm in ['nlw', 'cn', 'en']:
            seg_sh[nm] = T(rw, 'sgs_%s' % nm, [128, 512], F32)
        for nm in ['Ee', 'Ec', 'Ei', 'Et', 'af', 'beta', 'kd', 'tt']:
            seg_sh[nm] = T(rw, 'sgs_%s' % nm, [128, 512], F32 if nm == 'tt' else BF16)

        def seg_bufs(i):
            b = dict(seg_sh)
            for nm in ['aT', 'rT', 'bT', 'kT', 'BhT', 'KhT']:
                b[nm] = T(rw, 'sg%d_%s' % (i, nm), [128, 512], BF16)
            b['Bhm'] = T(rw, 'sg%d_Bhm' % i, [128, 4, 128], BF16)
            b['Khm'] = T(rw, 'sg%d_Khm' % i, [128, 4, 128], BF16)
            b['n'] = 'sg%d' % i
            return b
        SG = [seg_bufs(0), seg_bufs(1)]
        Gt = [T(rw, 'G%d' % i, [128, 512], BF16) for i in range(4)]
        N0 = [T(rw, 'N0_%d' % i, [128, 128], F32) for i in range(4)]
        NT0 = [T(rw, 'NT0_%d' % i, [128, 128], F32) for i in range(4)]
        NP = [[T(rw, 'NP%d_%d' % (i, j), [128, 256], F32) for j in range(2)] for i in range(4)]
        Xb = [[T(rw, 'Xb%d_%d' % (i, j), [128, 128], F32) for j in range(2)] for i in range(4)]
        Xf = [T(rw, 'Xf%d' % i, [128, 128], BF16) for i in range(4)]
        MTs = [T(rw, 'MTs%d' % i, [128, 64], F32) for i in range(3)]; Nns = [T(rw, 'Nns%d' % i, [128, 64], F32) for i in range(3)]; RpT = [T(rw, 'RpT%d' % i, [128, 128], BF16) for i in range(3)]
        Sst = [[T(rw, 'Sst%d_%d' % (d_, i), [128, 64], F32) for i in range(2)] for d_ in range(2)]
        Sb = [T(rw, 'Sb%d' % i, [128, 64], BF16) for i in range(2)]
        kk_t = [seg_sh['tt'], seg_sh['cn']]
        kk_q = [seg_sh['Ee'], seg_sh['Ec']]
        kk_r = [seg_sh['nlw'], seg_sh['en']]
        KKN = [('sgs_tt', 'sgs_Ee', 'sgs_nlw'), ('sgs_cn', 'sgs_Ec', 'sgs_en')]
        finA = [{nm: T(rw, 'fin%d_' % r_ + nm, shp, dt) for nm, shp, dt in [
            ('st', [128, 24], F32), ('yc', [128, 128], F32), ('sq', [128, 128], BF16), ('bon', [128, 2], F32),
('yo', [128, 128], BF16), ('pr', [128, 512], BF16)]} for r_ in range(2)]

        for hp in range(nhp):
            project_group([
                (hp * 128, 128, shift_consume(128, hp, lambda t1, tn, g0, n: S.op(
                    'pool', lambda e: e.tensor_copy(out=rb[:, g0:g0 + n], in_=t1), r=[tn], w=['rb']))),
                (D + hp * 128, 128, shift_consume(128, 8 + hp, lambda t1, tn, g0, n: S.op(
                    'pool', lambda e: e.tensor_copy(out=kb[:, g0:g0 + n], in_=t1), r=[tn], w=['kb']))),
                (2 * D + hp * 128, 128, shift_consume(128, 16 + hp, lambda t1, tn, g0, n: S.op(
                    'pool', lambda e: e.tensor_copy(out=vb[:, g0:g0 + n], in_=t1), r=[tn], w=['vb'])))])
            S.dma('sp', lnxbc[:, 0, :], lnx_d[:, 0, hp * 128:(hp + 1) * 128], w=['lnxbc'])
            S.dma('sp', lnxbc[:, 1, :], lnx_d[:, 1, hp * 128:(hp + 1) * 128], w=['lnxbc'])
            for ti, (g0, n, is_ctx) in enumerate(TT):
                q = ti % 2
                kt, kq, kr = kk_t[q], kk_q[q], kk_r[q]
                ktn, kqn, krn = KKN[q]
                kbank = PB[6] if q == 0 else PB[3]; kbn = 'pb6' if q == 0 else 'pb3'
                S.op('act', lambda e, kq=kq, g0=g0, n=n: e.activation(out=kq[:, 0:n], in_=kb[:, g0:g0 + n], func=AF.Square, scale=kkT[:, hp:hp + 1]), r=['kb', 'kkT'], w=[kqn])
                S.op('pe', lambda e, kq=kq, n=n, kbank=kbank: e.matmul(kbank[:, 0:n], lhsT=bonesb, rhs=kq[:, 0:n], start=True, stop=True), r=[kqn, 'cstb'], w=[kbn])
                S.op('act', lambda e, kr=kr, n=n, kbank=kbank: e.activation(out=kr[:, 0:n], in_=kbank[:, 0:n], func=AF.Sqrt), r=[kbn], w=[krn])
                S.op('dve', lambda e, kr=kr, n=n: e.tensor_scalar(out=kr[:, 0:n], in0=kr[:, 0:n], scalar1=1e-12, scalar2=None, op0=ALU.max), r=[krn], w=[krn])
                S.op('dve', lambda e, kr=kr, n=n: e.reciprocal(out=kr[:, 0:n], in_=kr[:, 0:n]), r=[krn], w=[krn])
                S.op('dve', lambda e, kr=kr, g0=g0, n=n: e.scalar_tensor_tensor(out=kkb[:, g0:g0 + n], in0=kb[:, g0:g0 + n], scalar=kkT[:, hp:hp + 1], in1=kr[:, 0:n], op0=ALU.mult, op1=ALU.mult),
                     r=['kb', 'kkT', krn], w=['kkb'])
            for c0 in range(0, NCH, 8):
                nchk = min(8, NCH - c0)
                for j in range(nchk):
                    c = c0 + j
                    S.op('pe', lambda e, c=c, j=j: e.transpose(out=PT[:, j * 128:(j + 1) * 128], in_=vb[:, c * 128:(c + 1) * 128], identity=identb),
                         r=['vb', 'cstb'], w=['pbT'])
                S.op('act', lambda e, c0=c0, nchk=nchk: e.activation(out=Vm[:, c0:c0 + nchk, :], in_=PT[:, 0:nchk * 128].rearrange("p (a b) -> p a b", b=128), func=AF.Copy),
                     r=['pbT'], w=['Vm'])

            if stage == 'p4':
                xf_ = ysum[:, 0:8, :].rearrange("p a b -> p (a b)")
                S.op('dve', lambda e: e.tensor_copy(out=xf_[:, 0:512], in_=kkb[:, 256:768]), r=['kkb'], w=['xf_dbg'])
                S.op('dve', lambda e: e.tensor_copy(out=xf_[:, 512:1024], in_=Vm[:, 2:6, :].rearrange("p a b -> p (a b)")), r=['Vm'], w=['xf_dbg'])
                return finish_early(xf_[:], 128, 1024, 'xf_dbg')
            segsD = [[[0, 1]] + [[2 + 4 * s_ + j for j in range(4)] for s_ in range(8)],
                     [[1, 0]] + [[2 + 4 * s_ + j for j in range(3, -1, -1)] for s_ in range(7, -1, -1)]]
            for d in range(2):
                S.op('pool', lambda e, d=d: e.memset(Sst[d][0][:], 0.0), w=['Sst%d_0' % d])
                S.op('pool', lambda e, d=d: e.memset(Sb[d][:], 0.0), w=['Sb%d' % d])
            par = [0, 0]
            seen_y = set(); seen_k = set()
            HS = [slice(0, 64), slice(64, 128)]

            def emit_prep(si, d):
                seg = segsD[d][si]
                B = SG[d]; bn_ = B['n']
                lo = min(seg) * 128; n = len(seg) * 128
                sl = slice(lo, lo + n)
                dsl = slice(d * 64, d * 64 + 64)
                S.op('pe', lambda e, sl=sl, n=n, dsl=dsl: e.matmul(PB[6][:, 0:n], lhsT=wupb[dsl, hp * 128:(hp + 1) * 128], rhs=twd[dsl, sl], start=True, stop=True),
                     r=['lwdst', 'twd'], w=['pb6'])
                S.op('act', lambda e, B=B, n=n, d=d: e.activation(out=B['tt'][:, 0:n], in_=PB[6][:, 0:n], func=AF.Softplus, scale=-1.0, bias=nw0[:, hp, d:d + 1]),
                     r=['pb6', 'nw0'], w=['sgs_tt'])
                S.op('pe', lambda e, sl=sl, n=n, dsl=dsl: e.matmul(PB[6][:, 0:n], lhsT=aupb[dsl, hp * 128:(hp + 1) * 128], rhs=adT[dsl, sl], start=True, stop=True),
                     r=['lwdst', 'adT'], w=['pb6'])
                S.op('act', lambda e, B=B, n=n: e.activation(out=B['nlw'][:, 0:n], in_=B['tt'][:, 0:n], func=AF.Exp, scale=-1.0, bias=-0.5),
                     r=['sgs_tt'], w=['sgs_nlw'])
                S.op('act', lambda e, B=B, n=n, d=d: e.activation(out=B['af'][:, 0:n], in_=PB[6][:, 0:n], func=AF.Sigmoid, bias=a0T[:, hp, d:d + 1]),
                     r=['pb6', 'a0T'], w=['sgs_af'])
                for c in seg:
                    o = c * 128 - lo
                    if d == 0:
                        S.op('dve', lambda e, B=B, o=o: e.tensor_tensor_scan(out=B['cn'][:, o:o + 128], data0=onesf, data1=B['nlw'][:, o:o + 128],
                                                                             initial=0.0, op0=ALU.mult, op1=ALU.add), r=['sgs_nlw', 'cst'], w=['sgs_cn'])
                        tot = B['cn'][:, o + 127:o + 128]
                    else:
                        S.op('dve', lambda e, B=B, o=o: e.tensor_tensor_scan(out=B['cn'][:, o + 127:(o - 1 if o > 0 else None):-1], data0=onesf,
                                                                             data1=B['nlw'][:, o + 127:(o - 1 if o > 0 else None):-1],
                                                                             initial=0.0, op0=ALU.mult, op1=ALU.add), r=['sgs_nlw', 'cst'], w=['sgs_cn'])
                        tot = B['cn'][:, o:o + 1]
                    S.op('dve', lambda e, tot=tot, c=c, d=d: e.tensor_scalar(out=PL[:, d, c:c + 1], in0=tot, scalar1=-1.0, scalar2=None, op0=ALU.mult),
                         r=['sgs_cn'], w=['PLn'])
                    S.op('act', lambda e, B=B, o=o, c=c, d=d: e.activation(out=B['Et'][:, o:o + 128], in_=B['cn'][:, o:o + 128], func=AF.Exp, bias=PL[:, d, c:c + 1]),
                         r=['sgs_cn', 'PLn'], w=['sgs_Et'])
                    S.op('act', lambda e, c=c, d=d: e.activation(out=PL[:, d, c:c + 1], in_=PL[:, d, c:c + 1], func=AF.Exp), r=['PLn'], w=['PLn', 'PL'])
                S.op('pool', lambda e, B=B, n=n: e.tensor_tensor(out=B['en'][:, 0:n], in0=B['cn'][:, 0:n], in1=B['nlw'][:, 0:n], op=ALU.subtract),
                     r=['sgs_cn', 'sgs_nlw'], w=['sgs_en'])
                S.op('act', lambda e, B=B, n=n: e.activation(out=B['Ee'][:, 0:n], in_=B['en'][:, 0:n], func=AF.Exp, scale=-1.0), r=['sgs_en'], w=['sgs_Ee'])
                S.op('act', lambda e, B=B, n=n: e.activation(out=B['Ec'][:, 0:n], in_=B['cn'][:, 0:n], func=AF.Exp, scale=-1.0), r=['sgs_cn'], w=['sgs_Ec'])
                S.op('act', lambda e, B=B, n=n: e.activation(out=B['Ei'][:, 0:n], in_=B['cn'][:, 0:n], func=AF.Exp), r=['sgs_cn'], w=['sgs_Ei'])
                S.op('dve', lambda e, B=B, sl=sl, n=n: e.scalar_tensor_tensor(out=B['aT'][:, 0:n], in0=kkb[:, sl], scalar=-1.0, in1=B['Ee'][:, 0:n], op0=ALU.mult, op1=ALU.mult),
                     r=['kkb', 'sgs_Ee'], w=[bn_ + 'aT'])
                S.op('pool', lambda e, B=B, sl=sl, n=n: e.tensor_tensor(out=B['rT'][:, 0:n], in0=rb[:, sl], in1=B['Ec'][:, 0:n], op=ALU.mult),
                     r=['rb', 'sgs_Ec'], w=[bn_ + 'rT'])
                S.op('pool', lambda e, B=B, sl=sl, n=n: e.tensor_tensor(out=B['beta'][:, 0:n], in0=kkb[:, sl], in1=B['af'][:, 0:n], op=ALU.mult),
                     r=['kkb', 'sgs_af'], w=['sgs_beta'])
                S.op('dve', lambda e, B=B, n=n: e.tensor_tensor(out=B['bT'][:, 0:n], in0=B['beta'][:, 0:n], in1=B['Ei'][:, 0:n], op=ALU.mult),
                     r=['sgs_beta', 'sgs_Ei'], w=[bn_ + 'bT'])
                S.op('pool', lambda e, B=B, n=n: e.tensor_tensor(out=B['BhT'][:, 0:n], in0=B['beta'][:, 0:n], in1=B['Et'][:, 0:n], op=ALU.mult),
                     r=['sgs_beta', 'sgs_Et'], w=[bn_ + 'BhT'])
                S.op('dve', lambda e, B=B, n=n: e.tensor_scalar(out=B['tt'][:, 0:n], in0=B['af'][:, 0:n], scalar1=kaT[:, hp:hp + 1], scalar2=oka[:, hp:hp + 1], op0=ALU.mult, op1=ALU.add),
                     r=['sgs_af', 'kaT', 'oka'], w=['sgs_tt'])
                S.op('pool', lambda e, B=B, sl=sl, n=n: e.tensor_tensor(out=B['kd'][:, 0:n], in0=kb[:, sl], in1=B['tt'][:, 0:n], op=ALU.mult),
                     r=['kb', 'sgs_tt'], w=['sgs_kd'])
                S.op('dve', lambda e, B=B, n=n: e.tensor_tensor(out=B['kT'][:, 0:n], in0=B['kd'][:, 0:n], in1=B['Ei'][:, 0:n], op=ALU.mult),
                     r=['sgs_kd', 'sgs_Ei'], w=[bn_ + 'kT'])
                S.op('pool', lambda e, B=B, n=n: e.tensor_tensor(out=B['KhT'][:, 0:n], in0=B['kd'][:, 0:n], in1=B['Et'][:, 0:n], op=ALU.mult),
                     r=['sgs_kd', 'sgs_Et'], w=[bn_ + 'KhT'])
                if lo >= T_CTX:
                    ls = slice(lo - T_CTX, lo - T_CTX + n)
                    if lo not in seen_k:
                        seen_k.add(lo)
                        S.op('pool', lambda e, B=B, ls=ls, n=n: e.tensor_copy(out=ksum[:, ls], in_=B['kd'][:, 0:n]), r=['sgs_kd'], w=['ksum'])
                    else:
                        S.op('pool', lambda e, B=B, ls=ls, n=n: e.tensor_tensor(out=ksum[:, ls], in0=ksum[:, ls], in1=B['kd'][:, 0:n], op=ALU.add), r=['sgs_kd', 'ksum'], w=['ksum'])
                nck = len(seg)
                for j in range(nck):
                    S.op('pe', lambda e, B=B, j=j: e.transpose(out=PT[:, j * 128:(j + 1) * 128], in_=B['BhT'][:, j * 128:(j + 1) * 128], identity=identb),
                         r=[bn_ + 'BhT', 'cstb'], w=['pbT'])
                    S.op('pe', lambda e, B=B, j=j: e.transpose(out=PT[:, 512 + j * 128:512 + (j + 1) * 128], in_=B['KhT'][:, j * 128:(j + 1) * 128], identity=identb),
                         r=[bn_ + 'KhT', 'cstb'], w=['pbT'])
                S.op('act', lambda e, B=B, nck=nck: e.activation(out=B['Bhm'][:, 0:nck, :], in_=PT[:, 0:nck * 128].rearrange("p (a b) -> p a b", b=128), func=AF.Copy),
                     r=['pbT'], w=[bn_ + 'Bhm'])
                S.op('act', lambda e, B=B, nck=nck: e.activation(out=B['Khm'][:, 0:nck, :], in_=PT[:, 512:512 + nck * 128].rearrange("p (a b) -> p a b", b=128), func=AF.Copy),
                     r=['pbT'], w=[bn_ + 'Khm'])


            stream = []
            for si in range(9):
                for jj in range(len(segsD[0][si])):
                    for d in range(2):
                        seg = segsD[d][si]; c = seg[jj]; lo = min(seg) * 128
                        o = c * 128 - lo
                        for hl in range(2):
                            stream.append(dict(d=d, hl=hl, c=c, cs=slice(o, o + 128), jc=o // 128, hs=HS[hl], pb=hl * 64, B=SG[d], bn=SG[d]['n'],
                                               prep=(si, d) if (jj == 0 and hl == 0) else None))
            NG = 3
            for g0_ in range(0, len(stream), NG):
                INS = stream[g0_:g0_ + NG]
                for sl_, I in enumerate(INS):
                    I['i'] = sl_; I['XB'] = PB[2 * sl_]; I['xn'] = 'pb%d' % (2 * sl_); I['QB'] = PB[2 * sl_ + 1]; I['qn'] = 'pb%d' % (2 * sl_ + 1)
                    if I['prep'] is not None:
                        emit_prep(*I['prep'])
                for I in INS:
                    i, B, hs, cs, bn_, d = I['i'], I['B'], I['hs'], I['cs'], I['bn'], I['d']
                    XB_, xn, QB_, qn = I['XB'], I['xn'], I['QB'], I['qn']
                    for q_, (l_, r_) in enumerate([('bT', 'aT'), ('bT', 'rT'), ('kT', 'aT'), ('kT', 'rT')]):
                        S.op('pe', lambda e, XB_=XB_, B=B, hs=hs, cs=cs, q_=q_, l_=l_, r_=r_: e.matmul(XB_[:, q_ * 128:(q_ + 1) * 128], lhsT=B[l_][hs, cs], rhs=B[r_][hs, cs], start=True, stop=True),
                             r=[bn_ + l_, bn_ + r_], w=[xn])
                    S.op('pe', lambda e, QB_=QB_, B=B, hs=hs, cs=cs: e.matmul(QB_[:, 0:128], lhsT=B['aT'][hs, cs], rhs=B['bT'][hs, cs], start=True, stop=True),
                         r=[bn_ + 'aT', bn_ + 'bT'], w=[qn])
                for I in INS:
                    i, d, XB_, xn, QB_, qn = I['i'], I['d'], I['XB'], I['xn'], I['QB'], I['qn']
                    S.op('dve', lambda e, i=i, d=d, XB_=XB_: e.tensor_tensor(out=Gt[i][:], in0=XB_[:, :], in1=mG[:, d, :], op=ALU.mult), r=[xn, 'mG'], w=['G%d' % i])
                    S.op('dve', lambda e, i=i, d=d, XB_=XB_: e.tensor_tensor(out=NT0[i][:], in0=XB_[:, 0:128], in1=mN[1 - d], op=ALU.mult), r=[xn, 'cst'], w=['N0_%d' % i])
                    S.op('dve', lambda e, i=i, d=d, QB_=QB_: e.tensor_tensor(out=N0[i][:], in0=QB_[:, 0:128], in1=mN[d], op=ALU.mult), r=[qn, 'cst'], w=['N0_%d' % i])
                for I in INS:
                    i, B, hs, cs, bn_, pb_, c, XB_, xn = I['i'], I['B'], I['hs'], I['cs'], I['bn'], I['pb'], I['c'], I['XB'], I['xn']
                    S.op('pe', lambda e, XB_=XB_, B=B, hs=hs, cs=cs, pb_=pb_: e.matmul(XB_[:, 0:64], lhsT=B['aT'][hs, cs], rhs=identb[hs, pb_:pb_ + 64], start=True, stop=False),
                         r=[bn_ + 'aT', 'cstb', 'G%d' % i, 'N0_%d' % i], w=[xn])
                    S.op('pe', lambda e, XB_=XB_, i=i, pb_=pb_, c=c: e.matmul(XB_[:, 64:128], lhsT=Gt[i][:, 256:384], rhs=Vm[:, c, pb_:pb_ + 64], start=False, stop=False, skip_group_check=True),
                         r=['G%d' % i, 'Vm'], w=[xn])
                Ncur = [N0[i][:] for i in range(NG)]; NTcur = [NT0[i][:] for i in range(NG)]
                Nres = [['N0_%d' % i] for i in range(NG)]
                for p in range(7):
                    for I in INS:
                        i, XB_, xn, QB_, qn = I['i'], I['XB'], I['xn'], I['QB'], I['qn']
                        xbn = 'Xb%d_%d' % (i, p % 2)
                        lowp = (p >= LOWP_FROM)
                        xb_ap = Xb[i][p % 2][:].bitcast(BF16)[:, 0:128] if lowp else Xb[i][p % 2][:]
                        S.op('act', lambda e, XB_=XB_, xb_ap=xb_ap: e.activation(out=xb_ap, in_=XB_[:, 0:128], func=AF.Copy), r=[xn], w=[xbn])
                        S.op('pe', lambda e, XB_=XB_, xb_ap=xb_ap, nt=NTcur[i], p=p: e.matmul(XB_[:, 0:128], lhsT=nt, rhs=xb_ap, start=False, stop=(p == 6), skip_group_check=True),
                             r=[xbn] + Nres[i], w=[xn])
                        if p < 6:
                            S.op('pe', lambda e, QB_=QB_, nt=NTcur[i], nn=Ncur[i]: e.matmul(QB_[:, 128:256], lhsT=nt, rhs=nn, start=True, stop=True), r=Nres[i], w=[qn])
                            S.op('pe', lambda e, QB_=QB_, nt=NTcur[i], nn=Ncur[i]: e.matmul(QB_[:, 256:384], lhsT=nn, rhs=nt, start=True, stop=True), r=Nres[i], w=[qn])
                            for _dm in range(int(os.environ.get('DUMMY', '0'))):
                                S.op('pe', lambda e, QB_=QB_: e.matmul(QB_[:, 384:512], lhsT=identb, rhs=identb, start=True, stop=True), r=['cstb'], w=[qn])
                            npn = 'NP%d_%d' % (i, p % 2)
                            npt_ap = NP[i][p % 2][:].bitcast(BF16)[:, 0:256] if p >= LOWP_FROM - 1 else NP[i][p % 2][:]
                            S.op('dve', lambda e, QB_=QB_, npt_ap=npt_ap: e.tensor_copy(out=npt_ap, in_=QB_[:, 128:384]), r=[qn], w=[npn])
                            Ncur[i] = npt_ap[:, 0:128]; NTcur[i] = npt_ap[:, 128:256]; Nres[i] = [npn]
                for I in INS:
                    i, XB_, xn = I['i'], I['XB'], I['xn']
                    S.op('act', lambda e, i=i, XB_=XB_: e.activation(out=Xf[i][:], in_=XB_[:, 0:128], func=AF.Copy), r=[xn], w=['Xf%d' % i])
                for I in INS:
                    i, B, hs, cs, bn_, pb_, c, d, jc, hl, xbk, xn = I['i'], I['B'], I['hs'], I['cs'], I['bn'], I['pb'], I['c'], I['d'], I['jc'], I['hl'], I['XB'], I['xn']
                    S.op('pe', lambda e, B=B, hs=hs, pb_=pb_, jc=jc, i=i, xbk=xbk: e.matmul(xbk[hs, 128:192], lhsT=B['Bhm'][:, jc, pb_:pb_ + 64], rhs=Xf[i][:, 64:128], start=True, stop=False),
                         r=[bn_ + 'Bhm', 'Xf%d' % i], w=[xn])
                    S.op('pe', lambda e, B=B, hs=hs, pb_=pb_, jc=jc, c=c, xbk=xbk: e.matmul(xbk[hs, 128:192], lhsT=B['Khm'][:, jc, pb_:pb_ + 64], rhs=Vm[:, c, pb_:pb_ + 64], start=False, stop=True),
                         r=[bn_ + 'Khm', 'Vm'], w=[xn])
                    S.op('pe', lambda e, B=B, hs=hs, pb_=pb_, jc=jc, i=i, xbk=xbk: e.matmul(xbk[hs, 192:256], lhsT=Xf[i][:, 0:64], rhs=B['Bhm'][:, jc, pb_:pb_ + 64], start=True, stop=True),
                         r=[bn_ + 'Bhm', 'Xf%d' % i], w=[xn])
                    S.op('pe', lambda e, hs=hs, i=i, xbk=xbk: e.matmul(xbk[hs, 256:384], lhsT=Xf[i][:, 0:64], rhs=Gt[i][:, 128:256], start=True, stop=True),
                         r=['G%d' % i, 'Xf%d' % i], w=[xn])
                    S.op('dve', lambda e, hs=hs, pb_=pb_, d=d, c=c, i=i, xbk=xbk: e.scalar_tensor_tensor(out=MTs[i][hs, :], in0=identf[hs, pb_:pb_ + 64], scalar=PL[hs, d, c:c + 1], in1=xbk[hs, 192:256],
                                                                                                   op0=ALU.mult, op1=ALU.add), r=[xn, 'PL', 'cst'], w=['MTs%d' % i])
                    S.op('dve', lambda e, hs=hs, i=i, xbk=xbk: e.tensor_copy(out=Nns[i][hs, :], in_=xbk[hs, 128:192]), r=[xn], w=['Nns%d' % i])
                    S.op('dve', lambda e, B=B, hs=hs, cs=cs, i=i, xbk=xbk: e.tensor_tensor(out=RpT[i][hs, :], in0=xbk[hs, 256:384], in1=B['rT'][hs, cs], op=ALU.add), r=[xn, bn_ + 'rT'], w=['RpT%d' % i])
                for I in INS:
                    i, hs, pb_, c, d, hl, xbk, xn = I['i'], I['hs'], I['pb'], I['c'], I['d'], I['hl'], I['XB'], I['xn']
                    sp_ = par[d]
                    st0, st1 = Sst[d][sp_], Sst[d][1 - sp_]; sn0, sn1 = 'Sst%d_%d' % (d, sp_), 'Sst%d_%d' % (d, 1 - sp_)
                    S.op('pe', lambda e, hs=hs, i=i, st0=st0, xbk=xbk: e.matmul(xbk[hs, 384:448], lhsT=MTs[i][hs, :], rhs=st0[hs, :], start=True, stop=True),
                         r=['MTs%d' % i, sn0, 'Nns%d' % i, 'RpT%d' % i], w=[xn])
                    S.op('dve', lambda e, hs=hs, i=i, st1=st1, xbk=xbk: e.tensor_tensor(out=st1[hs, :], in0=xbk[hs, 384:448], in1=Nns[i][hs, :], op=ALU.add),
                         r=[xn, 'Nns%d' % i], w=[sn1])
                    if c >= 2:
                        lc = c - 2
                        S.op('pe', lambda e, hs=hs, d=d, i=i, xbk=xbk: e.matmul(xbk[:, 448:512], lhsT=RpT[i][hs, :], rhs=Sb[d][hs, :], start=True, stop=False),
                             r=['RpT%d' % i, 'Sb%d' % d], w=[xn])
                        S.op('pe', lambda e, i=i, xbk=xbk: e.matmul(xbk[:, 448:512], lhsT=Gt[i][:, 128:256], rhs=Xf[i][:, 64:128], start=False, stop=False),
                             r=['G%d' % i, 'Xf%d' % i], w=[xn])
                        S.op('pe', lambda e, i=i, pb_=pb_, c=c, xbk=xbk: e.matmul(xbk[:, 448:512], lhsT=Gt[i][:, 384:512], rhs=Vm[:, c, pb_:pb_ + 64], start=False, stop=True),
                             r=['G%d' % i, 'Vm'], w=[xn])
                        key = (lc, hl)
                        if key not in seen_y:
                            seen_y.add(key)
                            S.op('dve', lambda e, pb_=pb_, lc=lc, xbk=xbk: e.tensor_copy(out=ysum[:, lc, pb_:pb_ + 64], in_=xbk[:, 448:512]), r=[xn], w=['ysum'])
                        else:
                            S.op('dve', lambda e, pb_=pb_, lc=lc, xbk=xbk: e.tensor_tensor(out=ysum[:, lc, pb_:pb_ + 64], in0=xbk[:, 448:512], in1=ysum[:, lc, pb_:pb_ + 64], op=ALU.add),
                                 r=[xn, 'ysum'], w=['ysum'])
                    S.op('act', lambda e, d=d, hs=hs, st1=st1: e.activation(out=Sb[d][hs, :], in_=st1[hs, :], func=AF.Copy), r=[sn1], w=['Sb%d' % d])
                    if hl == 1:
                        par[d] = 1 - par[d]
            if stage == 'rwkv_y':
                return finish_early(ysum[:].rearrange("p a b -> p (a b)"), 128, 1024, 'ysum') if False else finish_early4(ysum)
            for s8 in range(8):
                finp = finA[s8 % 2]; FP_ = 'fin%d_' % (s8 % 2)
                ls = slice(s8 * 512, (s8 + 1) * 512); gsl = slice(T_CTX + s8 * 512, T_CTX + (s8 + 1) * 512)
                S.dma('sp', sgt[:, 0, :], sgd_s[0, :, gsl], r=['sgd_s'], w=['sgt'])
                S.dma('sp', sgt[0:32, 1, :], sgd_s[1, 0:32, gsl], r=['sgd_s'], w=['sgt'])
                S.op('dve', lambda e, ls=ls, gsl=gsl: e.tensor_tensor(out=finp['pr'][:], in0=rb[:, gsl], in1=ksum[:, ls], op=ALU.mult), r=['rb', 'ksum'], w=[FP_ + 'pr'])
                S.op('dve', lambda e: e.tensor_scalar(out=finp['pr'][:], in0=finp['pr'][:], scalar1=rkT[:, hp:hp + 1], scalar2=None, op0=ALU.mult), r=[FP_ + 'pr', 'rkT'], w=[FP_ + 'pr'])
                for j in range(4):
                    lc = s8 * 4 + j; c = lc + 2
                    fin = finA[j % 2]; FN_ = 'fin%d_' % (j % 2)
                    gbk = PB[5] if j % 2 == 0 else PB[4]; gbn = 'pb5' if j % 2 == 0 else 'pb4'
                    bbk = PB[6] if j % 2 == 0 else PB[3]; bbn = 'pb6' if j % 2 == 0 else 'pb3'
                    for hl in range(2):
                        S.op('pe', lambda e, hl=hl, j=j: e.matmul(bbk[:, hl:hl + 1], lhsT=finp['pr'][hl * 64:(hl + 1) * 64, j * 128:(j + 1) * 128],
                                                                  rhs=bonesb[hl * 64:(hl + 1) * 64, hl * 64:hl * 64 + 1], start=True, stop=True), r=[FP_ + 'pr', 'cstb'], w=[bbn])
                    S.op('act', lambda e: e.activation(out=fin['bon'][:], in_=bbk[:, 0:2], func=AF.Copy), r=[bbn], w=[FN_ + 'bon'])
                    S.op('pe', lambda e, c=c: e.matmul(gbk[:, 0:128], lhsT=sgt[:, 0, j * 128:(j + 1) * 128], rhs=gupb[:, 0, hp * 128:(hp + 1) * 128], start=True, stop=False),
                         r=['sgt', 'lwdst'], w=[gbn])
                    S.op('pe', lambda e, c=c: e.matmul(gbk[:, 0:128], lhsT=sgt[0:32, 1, j * 128:(j + 1) * 128], rhs=gupb[0:32, 1, hp * 128:(hp + 1) * 128], start=False, stop=True),
                         r=['sgt', 'lwdst'], w=[gbn])
                    for hl in range(2):
                        hc = slice(hl * 64, hl * 64 + 64)
                        ysl = ysum[:, lc, hc]
                        so = hl * 12
                        S.op('dve', lambda e, ysl=ysl, so=so: e.bn_stats(out=fin['st'][:, so:so + 6], in_=ysl), r=['ysum'], w=[FN_ + 'st%d' % hl])
                        S.op('dve', lambda e, so=so: e.bn_aggr(out=fin['st'][:, so + 6:so + 8], in_=fin['st'][:, so:so + 6]), r=[FN_ + 'st%d' % hl], w=[FN_ + 'nm%d' % hl])
                        S.op('dve', lambda e, so=so: e.tensor_scalar(out=fin['st'][:, so + 8:so + 9], in0=fin['st'][:, so + 7:so + 8], scalar1=64e-5, scalar2=None, op0=ALU.add),
                             r=[FN_ + 'nm%d' % hl], w=[FN_ + 'v%d' % hl])
                        S.op('act', lambda e, so=so: e.activation(out=fin['st'][:, so + 8:so + 9], in_=fin['st'][:, so + 8:so + 9], func=AF.Sqrt), r=[FN_ + 'v%d' % hl], w=[FN_ + 'v%d' % hl])
                        S.op('dve', lambda e, so=so: e.reciprocal(out=fin['st'][:, so + 9:so + 10], in_=fin['st'][:, so + 8:so + 9]), r=[FN_ + 'v%d' % hl], w=[FN_ + 'rs%d' % hl])
                        S.op('dve', lambda e, ysl=ysl, hc=hc, so=so: e.tensor_scalar(out=fin['yc'][:, hc], in0=ysl, scalar1=fin['st'][:, so + 6:so + 7], scalar2=fin['st'][:, so + 9:so + 10],
                                                                                  op0=ALU.subtract, op1=ALU.mult), r=['ysum', FN_ + 'nm%d' % hl, FN_ + 'rs%d' % hl], w=[FN_ + 'yc%d' % hl])
                        gch = slice(hl * 64, hl * 64 + 64)
                        S.op('pool', lambda e, hc=hc, gch=gch: e.tensor_tensor(out=fin['yc'][:, hc], in0=fin['yc'][:, hc], in1=lnxbc[:, 0, gch], op=ALU.mult), r=[FN_ + 'yc%d' % hl, 'lnxbc'], w=[FN_ + 'yc%d' % hl])
                        S.op('pool', lambda e, hc=hc, gch=gch: e.tensor_tensor(out=fin['yc'][:, hc], in0=fin['yc'][:, hc], in1=lnxbc[:, 1, gch], op=ALU.add),
                             r=[FN_ + 'yc%d' % hl, 'lnxbc'], w=[FN_ + 'yc%d' % hl])
                        S.op('dve', lambda e, hl=hl, hc=hc, c=c: e.scalar_tensor_tensor(out=fin['yc'][:, hc], in0=Vm[:, c, hc], scalar=fin['bon'][:, hl:hl + 1], in1=fin['yc'][:, hc],
                                                                                      op0=ALU.mult, op1=ALU.add), r=['Vm', FN_ + 'bon', FN_ + 'yc%d' % hl], w=[FN_ + 'yc%d' % hl])
                    S.op('dve', lambda e: e.tensor_tensor(out=fin['yo'][:], in0=gbk[:, 0:128], in1=fin['yc'][:], op=ALU.mult), r=[gbn, FN_ + 'yc0', FN_ + 'yc1'], w=[FN_ + 'yo'])
                    S.op('pe', lambda e, j=j: e.transpose(out=PT[:, j * 128:(j + 1) * 128], in_=fin['yo'][:], identity=identb), r=[FN_ + 'yo', 'cstb'], w=['pbT'])
                S.op('act', lambda e, s8=s8: e.activation(out=yaT[s8 % 2][:], in_=PT[:, 0:512], func=AF.Copy), r=['pbT'], w=['yaT%d' % (s8 % 2)])
                S.dma('sp', ya_s[hp, :, ls], yaT[s8 % 2][:], r=['yaT%d' % (s8 % 2)], w=['ya_s'])
        S.barrier(); S.emit()
        rw.close(); open_stacks.pop()

        if stage == 'rwkv':
            with ExitStack() as ps:
                dt_ = T(ps, 'dbg_t', [128, T_LAT], BF16); df_ = T(ps, 'dbg_f', [128, T_LAT], F32)
                toks = []
                dbgv = dbg_d.rearrange("(a b) d -> a (b d)", b=4)
                for hp in range(nhp):
                    S.dma('sp', dt_[:], ya_s[hp, :, :], r=['ya_s'], w=['dbg_t'])
                    S.op('dve', lambda e: e.tensor_copy(out=df_[:], in_=dt_[:]), r=['dbg_t'], w=['dbg_f'])
                    toks.append(S.dma('sp', dbgv[hp * 128:(hp + 1) * 128, :], df_[:], r=['dbg_f'], w=['dbg']))
                S.op('pool', lambda e: e.memset(df_[:, 0:8], 0.0), r=['dbg'], w=['dbg_f'])
                toks.append(S.dma('sp', out_d[0:128, 0:8], df_[:, 0:8], r=['dbg_f'], w=['out']))
                S.wait_all('sp', toks)
                S.emit()
            return nc

        lr = ExitStack(); open_stacks.append(lr)
        zx = T(lr, 'zx', [128, T_ALL], F32); xc = T(lr, 'xc', [128, T_ALL], F32); xcb = T(lr, 'xcb', [128, T_ALL], BF16)
        glu = T(lr, 'glu', [128, T_LAT], BF16)
        aa = [T(lr, 'aa0', [128, T_ALL], F32)] * 2
        bx = [T(lr, 'bx0', [128, T_ALL], F32)] * 2
        hh = [T(lr, 'hh%d' % i, [128, T_ALL], F32) for i in range(2)]
        ybt = T(lr, 'ybt', [128, T_LAT], BF16)
        gws = T(lr, 'gws', [128, 4, 64], F32); gwb = T(lr, 'gwb', [128, 4, 64], BF16)
        ltA = [[T(lr, 'lt%d_%d' % (i, r_), [128, 512], F32) for i in range(4)] for r_ in range(3)]
        ltc = {'i': 0}
        for cb in range(8 if nhp == 8 else 1):
            def cons_lx(ps_ap, bn, g0, n, is_ctx):
                S.op('act', lambda e: e.activation(out=zx[:, g0:g0 + n], in_=ps_ap, func=AF.Copy), r=[bn], w=['zx'])

            def cons_lg(ps_ap, bn, g0, n, is_ctx):
                if not is_ctx:
                    S.op('act', lambda e: e.activation(out=glu[:, g0 - T_CTX:g0 - T_CTX + n], in_=ps_ap, func=AF.Gelu_apprx_tanh), r=[bn], w=['glu'])
            project_group([(OFF_LX + cb * 128, 128, cons_lx), (OFF_LG + cb * 128, 128, cons_lg)])
            S.dma('sp', gws[:], gw_d[:, cb, :, :], w=['gws'])
            S.op('pool', lambda e: e.tensor_copy(out=gwb[:], in_=gws[:]), r=['gws'], w=['gwb'])
            S.op('act', lambda e: e.activation(out=xc[:], in_=zx[:], func=AF.Identity, scale=cwT[:, cb, 2:3], bias=cbT[:, cb:cb + 1]), r=['zx', 'cwT', 'cbT'], w=['xc'])
            L0 = T_CTX
            for (o_sl, i_sl, wj) in [(slice(L0 + 128, T_ALL), slice(L0, T_ALL - 128), 0), (slice(L0 + 64, T_ALL), slice(L0, T_ALL - 64), 1),
                                     (slice(L0, T_ALL - 64), slice(L0 + 64, T_ALL), 3),
                                     (slice(2, 256), slice(0, 254), 0), (slice(1, 256), slice(0, 255), 1), (slice(0, 255), slice(1, 256), 3)]:
                S.op('dve', lambda e, o_sl=o_sl, i_sl=i_sl, wj=wj: e.scalar_tensor_tensor(out=xc[:, o_sl], in0=zx[:, i_sl], scalar=cwT[:, cb, wj:wj + 1], in1=xc[:, o_sl],
                                                                                      op0=ALU.mult, op1=ALU.add), r=['zx', 'xc', 'cwT'], w=['xc'])
            S.op('pool', lambda e: e.tensor_copy(out=xcb[:], in_=xc[:]), r=['xc'], w=['xcb'])
            for d in range(2):
                for ti, (g0, n, is_ctx) in enumerate(TT):
                    sl = slice(g0, g0 + n)
                    rot = ltc['i'] % 3; bset = ltc['i'] % 2; ltc['i'] += 1
                    lt = ltA[rot]; L_ = lambda k_: 'lt%d_%d' % (k_, rot)
                    for g_ in range(2):
                        dg = d * 2 + g_
                        bank = PB[g_ + 2 * bset]; bnn = 'pb%d' % (g_ + 2 * bset)
                        for nl in range(2):
                            hs = slice(nl * 64, nl * 64 + 64)
                            S.op('pe', lambda e, hs=hs, dg=dg, bank=bank, sl=sl, n=n: e.matmul(bank[hs, 0:n], lhsT=gwb[hs, dg, :], rhs=xcb[hs, sl], start=True, stop=True),
                                 r=['gwb', 'xcb'], w=[bnn])
                        S.op('act', lambda e, bank=bank, g_=g_, dg=dg, n=n: e.activation(out=lt[g_][:, 0:n], in_=bank[:, 0:n], func=AF.Sigmoid, bias=gbT[:, cb, dg:dg + 1]),
                             r=[bnn, 'gbT'], w=[L_(g_)])
                    if is_ctx:
                        a_out = aa[d][:, sl]; b_out = bx[d][:, sl]; v3 = lambda ap_: ap_
                    else:
                        r0 = (g0 - T_CTX) // 64
                        a_out = aa[d][:, T_CTX:].rearrange("p (c r) -> p r c", r=64)[:, r0:r0 + 8, :]
                        b_out = bx[d][:, T_CTX:].rearrange("p (c r) -> p r c", r=64)[:, r0:r0 + 8, :]
                        v3 = lambda ap_: ap_.rearrange("p (r c) -> p r c", c=64)
                    S.op('act', lambda e, n=n, a_out=a_out, v3=v3: e.activation(out=a_out, in_=v3(lt[0][:, 0:n]), func=AF.Exp, scale=nsp[:, cb, d:d + 1]), r=[L_(0), 'nsp'], w=['aa0'])
                    S.op('act', lambda e, n=n: e.activation(out=lt[2][:, 0:n], in_=lt[0][:, 0:n], func=AF.Exp, scale=nsp2[:, cb, d:d + 1]), r=[L_(0), 'nsp2'], w=[L_(2)])
                    S.op('act', lambda e, n=n: e.activation(out=lt[3][:, 0:n], in_=lt[2][:, 0:n], func=AF.Sqrt, scale=-1.0, bias=1.0), r=[L_(2)], w=[L_(3)])
                    S.op('pool', lambda e, n=n: e.tensor_tensor(out=lt[3][:, 0:n], in0=lt[3][:, 0:n], in1=lt[1][:, 0:n], op=ALU.mult), r=[L_(3), L_(1)], w=[L_(3)])
                    S.op('dve', lambda e, sl=sl, n=n, b_out=b_out, v3=v3: e.tensor_tensor(out=b_out, in0=v3(lt[3][:, 0:n]), in1=v3(xc[:, sl]), op=ALU.mult), r=[L_(3), 'xc'], w=['bx0'])
                A, Bx, Hh = aa[d], bx[d], hh[d]; an, bn2, hn = 'aa0', 'bx0', 'hh%d' % d
                if d == 0:
                    S.op('dve', lambda e: e.tensor_tensor_scan(out=Hh[:, 0:256], data0=A[:, 0:256], data1=Bx[:, 0:256], initial=0.0, op0=ALU.mult, op1=ALU.add), r=[an, bn2], w=[hn])
                    S.op('dve', lambda e: e.tensor_tensor_scan(out=Hh[:, L0:], data0=A[:, L0:], data1=Bx[:, L0:], initial=Hh[:, 255:256], op0=ALU.mult, op1=ALU.add), r=[an, bn2, hn], w=[hn])
                else:
                    S.op('dve', lambda e: e.tensor_tensor_scan(out=Hh[:, 255::-1], data0=A[:, 255::-1], data1=Bx[:, 255::-1], initial=0.0, op0=ALU.mult, op1=ALU.add), r=[an, bn2], w=[hn])
                    S.op('dve', lambda e: e.tensor_tensor_scan(out=Hh[:, T_ALL - 1:L0 - 1:-1], data0=A[:, T_ALL - 1:L0 - 1:-1], data1=Bx[:, T_ALL - 1:L0 - 1:-1], initial=Hh[:, 0:1], op0=ALU.mult, op1=ALU.add),
                         r=[an, bn2, hn], w=[hn])
            S.op('pool', lambda e: e.tensor_tensor(out=hh[0][:, L0:], in0=hh[0][:, L0:], in1=hh[1][:, L0:], op=ALU.add), r=['hh0', 'hh1'], w=['hh0'])
            S.op('dve', lambda e: e.tensor_tensor(out=ybt[:].rearrange("p (r c) -> p r c", c=64), in0=hh[0][:, L0:].rearrange("p (c r) -> p r c", r=64), in1=glu[:].rearrange("p (r c) -> p r c", c=64), op=ALU.mult), r=['hh0', 'glu'], w=['ybt'])
            S.dma('sp', yb_s[cb, :, :], ybt[:], r=['ybt'], w=['yb_s'])
        if stage == 'lru':
            S.dma('sp', ybt[:], yb_s[0, :, :], r=['yb_s'], w=['ybt'])
            S.op('dve', lambda e: e.tensor_copy(out=hh[0][:, 0:T_LAT], in_=ybt[:]), r=['ybt'], w=['hh0'])
            return finish_early(hh[0][:, 0:T_LAT], 128, 1024, 'hh0') if False else finish_rows(hh[0])
        S.barrier(); S.emit()
        lr.close(); open_stacks.pop()

        gates = T(es, 'gates', [128, 32, NEXP], F32)
        mg = ExitStack(); open_stacks.append(mg)
        Wm = {nm: T(mg, 'W_' + nm, [128, 8, D], BF16) for nm in ['ga', 'gb', 'a', 'b', 'o']}
        wstg = T(mg, 'wstg', [128, 8, 256], F32)
        srcs = {'ga': winv[:, :, OFF_GA:OFF_GA + D], 'gb': winv[:, :, OFF_GB:OFF_GB + D],
                'a': wa_d.rearrange("(k p) c -> p k c", p=128), 'b': wb_d.rearrange("(k p) c -> p k c", p=128), 'o': wo_d.rearrange("(k p) c -> p k c", p=128)}
        for nm in ['ga', 'gb', 'a', 'b', 'o']:
            for hf in range(4):
                S.dma('sp', wstg[:], srcs[nm][:, :, hf * 256:(hf + 1) * 256], w=['wstg'])
                S.op('pool', lambda e, nm=nm, hf=hf: e.tensor_copy(out=Wm[nm][:, :, hf * 256:(hf + 1) * 256], in_=wstg[:]), r=['wstg'], w=['W_' + nm])
        rws = T(mg, 'rws', [128, 8, NEXP], F32); rwb = T(mg, 'rwb', [128, 8, NEXP], BF16)
        S.dma('sp', rws[:], rw_d.rearrange("(k p) c -> p k c", p=128), w=['rws'])
        S.op('pool', lambda e: e.tensor_copy(out=rwb[:], in_=rws[:]), r=['rws'], w=['rwb'])
        rbb = T(mg, 'rbb', [128, NEXP], F32); S.dma('sp', rbb[:], rb_d[:, :], w=['rbb'])
        gg = T(mg, 'gg', [128, 2, D], F32); S.dma('sp', gg[:], gt_s[:, :, :], r=['gt_s'], w=['gg'])
        yat = T(mg, 'yat', [128, 8, 512], BF16); ybt2 = T(mg, 'ybt2', [128, 8, 512], BF16)
        mixT = T(mg, 'mixT', [128, 8, 512], BF16); ust = T(mg, 'ust', [128, 8, 512], BF16)
        sgt = [T(mg, 'sgt%d' % i, [128, 512], F32) for i in range(2)]
        mt_ = [T(mg, 'mt%d' % i, [128, 512], F32) for i in range(2)]
        xres = T(mg, 'xres', [128, D], F32); h1t = T(mg, 'h1t', [128, D], F32); tmpy = h1t
        nst = {'ss': T(mg, 'm_ss', [128, 4], F32), 'xs': T(mg, 'm_xs', [128, D], BF16), 'junk': T(mg, 'm_junk', [128, D], BF16)}
        rt = {nm: T(mg, 'rt_' + nm, shp, F32) for nm, shp in [('sc', [128, 64]), ('bi', [128, 64]), ('m8', [128, 8, 8]), ('gs', [128, 8]), ('g8', [128, 8]),
                                                            ('gm', [128, 8]), ('mk', [128, 64]), ('t8', [128, 8]), ('gu', [128, 64]), ('dn', [128, 2]), ('ss', [128, 4])]}
        yav = ya_s.rearrange("h p t -> p h t"); ybv = yb_s.rearrange("h p t -> p h t")
        NT6 = 8 if nhp == 8 else 1
        for tt in range(NT6):
            g0 = T_CTX + tt * 512; l0 = tt * 512
            xt = xnt[tt % 2]; xtn = 'xnt%d' % (tt % 2)
            S.dma('sp', xt[:, 0:4, :], xnv(tt + 1)[:, 0:4, :], r=['xn_s'], w=[xtn + 'a'])
            S.dma('sp', xt[:, 4:8, :], xnv(tt + 1)[:, 4:8, :], r=['xn_s'], w=[xtn + 'b'])
            S.dma('sp', yat[:], yav[:, :, l0:l0 + 512], r=['ya_s'], w=['yat'])
            S.dma('sp', ybt2[:], ybv[:, :, l0:l0 + 512], r=['yb_s'], w=['ybt2'])
            for dc in range(8):
                dsl = slice(dc * 128, (dc + 1) * 128)
                for bi_, (wn, rhs_t, rn) in enumerate([('ga', xt, xtn + 'a'), ('a', yat, 'yat'), ('gb', xt, xtn + 'a'), ('b', ybt2, 'ybt2')]):
                    for k in range(8):
                        S.op('pe', lambda e, bi_=bi_, wn=wn, rhs_t=rhs_t, k=k: e.matmul(PB[bi_][:, :], lhsT=Wm[wn][:, k, dsl], rhs=rhs_t[:, k, :], start=(k == 0), stop=(k == 7)),
                             r=['W_' + wn, rn] + ([xtn + 'b'] if rhs_t is xt else []), w=['pb%d' % bi_])
                S.op('act', lambda e: e.activation(out=sgt[0][:], in_=PB[0][:, :], func=AF.Sigmoid), r=['pb0'], w=['sgt0'])
                S.op('act', lambda e: e.activation(out=sgt[1][:], in_=PB[2][:, :], func=AF.Sigmoid), r=['pb2'], w=['sgt1'])
                S.op('dve', lambda e: e.tensor_tensor(out=mt_[0][:], in0=PB[1][:, :], in1=sgt[0][:], op=ALU.mult), r=['pb1', 'sgt0'], w=['mt0'])
                S.op('dve', lambda e: e.tensor_tensor(out=mt_[1][:], in0=PB[3][:, :], in1=sgt[1][:], op=ALU.mult), r=['pb3', 'sgt1'], w=['mt1'])
                S.op('pool', lambda e, dc=dc: e.tensor_tensor(out=mixT[:, dc, :], in0=mt_[0][:], in1=mt_[1][:], op=ALU.add), r=['mt0', 'mt1'], w=['mixT'])
            for j in range(4):
                tsl = slice(j * 128, (j + 1) * 128); st_i = tt * 4 + j
                row0 = l0 + j * 128
                S.dma('sp', xres[:], x_d[row0:row0 + 128, :], w=['xres'])
                for hf in range(2):
                    for k in range(8):
                        S.op('pe', lambda e, hf=hf, k=k: e.matmul(PB[4 + hf][:, :], lhsT=mixT[:, k, tsl], rhs=Wm['o'][:, k, hf * 512:(hf + 1) * 512], start=(k == 0), stop=(k == 7)),
                             r=['mixT', 'W_o'], w=['pb%d' % (4 + hf)])
                ss = rt['ss']
                for hf in range(2):
                    S.op('act', lambda e, hf=hf: e.activation(out=nst['junk'][:, hf * 512:(hf + 1) * 512], in_=PB[4 + hf][:, :], func=AF.Square, accum_out=ss[:, hf:hf + 1]),
                         r=['pb%d' % (4 + hf)], w=['m_junk', 'rt_ss%d' % hf])
                S.op('dve', lambda e: e.tensor_tensor(out=ss[:, 2:3], in0=ss[:, 0:1], in1=ss[:, 1:2], op=ALU.add), r=['rt_ss0', 'rt_ss1'], w=['rt_ss2'])
                S.op('dve', lambda e: e.tensor_scalar(out=ss[:, 2:3], in0=ss[:, 2:3], scalar1=1.0 / D, scalar2=1e-6, op0=ALU.mult, op1=ALU.add), r=['rt_ss2'], w=['rt_ss2'])
                S.op('act', lambda e: e.activation(out=ss[:, 2:3], in_=ss[:, 2:3], func=AF.Sqrt), r=['rt_ss2'], w=['rt_ss2'])
                S.op('dve', lambda e: e.reciprocal(out=ss[:, 3:4], in_=ss[:, 2:3]), r=['rt_ss2'], w=['rt_ss3'])
                for hf in range(2):
                    hsl = slice(hf * 512, (hf + 1) * 512)
                    S.op('dve', lambda e, hf=hf, hsl=hsl: e.scalar_tensor_tensor(out=tmpy[:, hsl], in0=PB[4 + hf][:, :], scalar=ss[:, 3:4], in1=gg[:, 0, hsl], op0=ALU.mult, op1=ALU.mult),
                         r=['pb%d' % (4 + hf), 'rt_ss3', 'gg'], w=['h1t'])
                S.op('pool', lambda e: e.tensor_tensor(out=h1t[:], in0=tmpy[:], in1=xres[:], op=ALU.add), r=['h1t', 'xres'], w=['h1t'])
                S.dma('sp', h1_s[row0:row0 + 128, :], h1t[:], r=['h1t'], w=['h1_s'])
                norm_to_featT(nst, h1t[:], 'h1t', ust, 'ust', j * 128, lambda k: gs2[:, k:k + 1], lambda k: sh2[:, k:k + 1], 'm_')
                for k in range(8):
                    S.op('pe', lambda e, k=k: e.matmul(PB[6][:, 0:NEXP], lhsT=ust[:, k, tsl], rhs=rwb[:, k, :], start=(k == 0), stop=(k == 7)), r=['ust', 'rwb'], w=['pb6'])
                S.op('act', lambda e: e.activation(out=rt['sc'][:], in_=PB[6][:, 0:NEXP], func=AF.Sigmoid), r=['pb6'], w=['rt_sc'])
                S.op('dve', lambda e: e.tensor_tensor(out=rt['bi'][:], in0=rt['sc'][:], in1=rbb[:], op=ALU.add), r=['rt_sc', 'rbb'], w=['rt_bi'])
                for gI in range(8):
                    S.op('dve', lambda e, gI=gI: e.max(out=rt['m8'][:, gI, :], in_=rt['bi'][:, gI * 8:(gI + 1) * 8]), r=['rt_bi'], w=['rt_m8'])
                S.op('dve', lambda e: e.tensor_tensor(out=rt['gs'][:], in0=rt['m8'][:, :, 0], in1=rt['m8'][:, :, 1], op=ALU.add), r=['rt_m8'], w=['rt_gs'])
                S.op('dve', lambda e: e.max(out=rt['g8'][:], in_=rt['gs'][:]), r=['rt_gs'], w=['rt_g8'])
                S.op('dve', lambda e: e.tensor_scalar(out=rt['gm'][:], in0=rt['gs'][:], scalar1=rt['g8'][:, 3:4], scalar2=None, op0=ALU.is_ge), r=['rt_gs', 'rt_g8'], w=['rt_gm'])
                for gI in range(8):
                    S.op('dve', lambda e, gI=gI: e.tensor_scalar(out=rt['mk'][:, gI * 8:(gI + 1) * 8], in0=rt['bi'][:, gI * 8:(gI + 1) * 8], scalar1=10.0, scalar2=rt['gm'][:, gI:gI + 1],
                                                                op0=ALU.add, op1=ALU.mult), r=['rt_bi', 'rt_gm'], w=['rt_mk'])
                S.op('dve', lambda e: e.max(out=rt['t8'][:], in_=rt['mk'][:]), r=['rt_mk'], w=['rt_t8'])
                S.op('dve', lambda e: e.scalar_tensor_tensor(out=rt['gu'][:], in0=rt['mk'][:], scalar=rt['t8'][:, 5:6], in1=rt['sc'][:], op0=ALU.is_ge, op1=ALU.mult),
                     r=['rt_mk', 'rt_t8', 'rt_sc'], w=['rt_gu'])
                S.op('dve', lambda e: e.tensor_reduce(out=rt['dn'][:, 0:1], in_=rt['gu'][:], axis=AX.X, op=ALU.add), r=['rt_gu'], w=['rt_dn'])
                S.op('dve', lambda e: e.reciprocal(out=rt['dn'][:, 1:2], in_=rt['dn'][:, 0:1]), r=['rt_dn'], w=['rt_dn1'])
                S.op('dve', lambda e, st_i=st_i: e.tensor_scalar(out=gates[:, st_i, :], in0=rt['gu'][:], scalar1=rt['dn'][:, 1:2], scalar2=2.5, op0=ALU.mult, op1=ALU.mult),
                     r=['rt_gu', 'rt_dn1'], w=['gates'])
            S.dma('sp', uT_s[:, :, l0:l0 + 512], ust[:], r=['ust'], w=['uT_s'])
        S.barrier(); S.emit()
        mg.close(); open_stacks.pop()
        if stage == 'merge':
            return finish_rows2(h1_s, gates)

        me = ExitStack(); open_stacks.append(me)
        HT = T_LAT // 2
        uTh = T(me, 'uTh', [128, 8, HT], BF16)
        acc = T(me, 'acc', [128, 16, D], F32)
        gus = T(me, 'gus', [128, 4, 512], F32); dns = T(me, 'dns', [128, 2, 512], F32)
        gub = [T(me, 'gub%d' % i, [128, 8, 512], BF16) for i in range(2)]
        dnb = [T(me, 'dnb%d' % i, [128, 2, D], BF16) for i in range(2)]
        sgm = [T(me, 'sgm%d' % i, [128, 512], F32) for i in range(2)]
        hT = [T(me, 'hT%d' % i, [128, 2, 512], BF16) for i in range(2)]
        gg2 = T(me, 'gg2', [128, D], F32); S.dma('sp', gg2[:], gt_s[:, 1, :], r=['gt_s'], w=['gg2'])
        h1r = T(me, 'h1r', [128, D], F32); ot = T(me, 'ot', [128, D], F32)
        fss = T(me, 'fss', [128, 4], F32); fjk = gub[0][:, 0:2, :].rearrange("p a b -> p (a b)")
        out_toks = []
        NEX = NEXP if nhp == 8 else 2
        ei = 0
        pend = [None]

        def emit_down(e_, q, t4, hq, hqn, half):
            for j in range(4):
                st_l = t4 * 4 + j; st_g = half * 16 + st_l
                for hf in range(2):
                    bk = 4 + (j * 2 + hf) % 3; bkn = 'pb%d' % bk
                    for fc in range(2):
                        S.op('pe', lambda e, bk=bk, fc=fc, hf=hf, j=j, hq=hq, q=q: e.matmul(PB[bk][:, :], lhsT=hq[:, fc, j * 128:(j + 1) * 128], rhs=dnb[q][:, fc, hf * 512:(hf + 1) * 512],
                                                                                         start=(fc == 0), stop=(fc == 1)), r=[hqn, 'dnb%d' % q], w=[bkn])
                    asl = acc[:, st_l, hf * 512:(hf + 1) * 512]
                    if e_ < 0:
                        S.op('act', lambda e, bk=bk, asl=asl: e.activation(out=asl, in_=PB[bk][:, :], func=AF.Copy), r=[bkn], w=['acc%d' % st_l])
                    else:
                        S.op('dve', lambda e, bk=bk, asl=asl, st_g=st_g, e_=e_: e.scalar_tensor_tensor(out=asl, in0=PB[bk][:, :], scalar=gates[:, st_g, e_:e_ + 1], in1=asl, op0=ALU.mult, op1=ALU.add),
                             r=[bkn, 'gates', 'acc%d' % st_l], w=['acc%d' % st_l])

        for half in range(2 if nhp == 8 else 1):
            S.dma('sp', uTh[:], uT_s[:, :, half * HT:(half + 1) * HT], r=['uT_s'], w=['uTh'])
            for e_ in [-1] + list(range(NEX)):
                q = ei % 2; ei += 1
                gsrc = (sgu_d if e_ < 0 else egu_d[e_]).rearrange("(k p) c -> p k c", p=128)
                dsrc = (sdn_d if e_ < 0 else edn_d[e_]).rearrange("(k p) c -> p k c", p=128)
                for gh in range(2):
                    if 'w' in MOESKIP and e_ >= 1: break
                    S.dma('sp', gus[:], gsrc[:, gh * 4:(gh + 1) * 4, :], w=['gus'])
                    S.op('pool', lambda e, q=q, gh=gh: e.tensor_copy(out=gub[q][:, gh * 4:(gh + 1) * 4, :], in_=gus[:]), r=['gus'], w=['gub%d' % q])
                for dh in range(2):
                    if 'w' in MOESKIP and e_ >= 1: break
                    S.dma('sp', dns[:], dsrc[:, :, dh * 512:(dh + 1) * 512], w=['dns'])
                    S.op('pool', lambda e, q=q, dh=dh: e.tensor_copy(out=dnb[q][:, :, dh * 512:(dh + 1) * 512], in_=dns[:]), r=['dns'], w=['dnb%d' % q])
                for t4 in range(4):
                    tk = slice(t4 * 512, (t4 + 1) * 512)
                    for fc in range(4):
                        for k in range(8):
                            S.op('pe', lambda e, fc=fc, k=k, q=q: e.matmul(PB[fc][:, :], lhsT=gub[q][:, k, fc * 128:(fc + 1) * 128], rhs=uTh[:, k, tk], start=(k == 0), stop=(k == 7)),
                                 r=['gub%d' % q, 'uTh'], w=['pb%d' % fc])
                    hq = hT[t4 % 2]; hqn = 'hT%d' % (t4 % 2)
                    for fc in range(2):
                        S.op('act', lambda e, fc=fc: e.activation(out=sgm[fc][:], in_=PB[fc][:, :], func=AF.Silu), r=['pb%d' % fc], w=['sgm%d' % fc])
                        S.op('dve', lambda e, fc=fc, hq=hq: e.tensor_tensor(out=hq[:, fc, :], in0=PB[2 + fc][:, :], in1=sgm[fc][:], op=ALU.mult), r=['pb%d' % (2 + fc), 'sgm%d' % fc], w=[hqn])
                    if pend[0] is not None:
                        emit_down(*pend[0])
                    pend[0] = (e_, q, t4, hq, hqn, half)
            if pend[0] is not None:
                emit_down(*pend[0]); pend[0] = None
            for st_l in range(16):
                row0 = half * HT + st_l * 128
                S.dma('sp', h1r[:], h1_s[row0:row0 + 128, :], r=['h1_s'], w=['h1r'])
                S.op('act', lambda e, st_l=st_l: e.activation(out=fjk, in_=acc[:, st_l, :], func=AF.Square, accum_out=fss[:, 0:1]), r=['acc%d' % st_l], w=['gub0', 'fss0'])
                S.op('dve', lambda e: e.tensor_scalar(out=fss[:, 1:2], in0=fss[:, 0:1], scalar1=1.0 / D, scalar2=1e-6, op0=ALU.mult, op1=ALU.add), r=['fss0'], w=['fss1'])
                S.op('act', lambda e: e.activation(out=fss[:, 2:3], in_=fss[:, 1:2], func=AF.Sqrt), r=['fss1'], w=['fss2'])
                S.op('dve', lambda e: e.reciprocal(out=fss[:, 3:4], in_=fss[:, 2:3]), r=['fss2'], w=['fss3'])
                S.op('dve', lambda e, st_l=st_l: e.scalar_tensor_tensor(out=ot[:], in0=acc[:, st_l, :], scalar=fss[:, 3:4], in1=gg2[:], op0=ALU.mult, op1=ALU.mult),
                     r=['acc%d' % st_l, 'fss3', 'gg2'], w=['ot'])
                S.op('pool', lambda e: e.tensor_tensor(out=ot[:], in0=ot[:], in1=h1r[:], op=ALU.add), r=['ot', 'h1r'], w=['ot'])
                out_toks.append(S.dma('sp', out_d[row0:row0 + 128, :], ot[:], r=['ot'], w=['out']))
        S.wait_all('sp', out_toks)
        S.emit()
        me.close(); open_stacks.pop()
    return nc


def host_layout(inp, b):
    f = lambda a: np.ascontiguousarray(a, dtype=np.float32)
    pk = lambda v: f(np.asarray(v).reshape(-1, 128).T)
    bc = lambda v: f(np.broadcast_to(np.asarray(v)[None], (128,) + np.asarray(v).shape))
    m = {}
    m['x'] = f(inp['x'][b]); m['ctx'] = f(inp['ctx'][b])
    m['cvec'] = f(np.stack([pk(inp['c'][b]), pk(inp['c_ctx'])], axis=-1))
    m['w_mod'] = f(inp['w_mod'][0]); m['b_modT'] = pk(inp['b_mod'][0])
    bm = inp['b_mod'][0]
    m['b_mod_bc'] = bc(np.stack([bm[2048:3072], bm[5120:6144]]))
    ng = inp['norm_g'][0]
    m['norm_gT'] = f(np.stack([pk(ng[i]) for i in range(4)], axis=1))
    m['gpost_bc'] = bc(np.stack([ng[1], ng[3]]))
    m['w_in'] = f(inp['w_in'][0])
    mu = np.zeros((2, 28 * 128), np.float32); mu[:, :3488] = inp['shift_mu'][0]
    m['muT'] = f(np.stack([pk(mu[0]), pk(mu[1])], axis=-1))
    m['w0T'] = f(np.stack([pk(inp['rw_w0'][0][d]) for d in range(2)], axis=-1))
    m['a0T'] = f(np.stack([pk(inp['rw_a0'][0][d]) for d in range(2)], axis=-1))
    m['kkT'] = pk(inp['rw_k_k'][0]); m['kaT'] = pk(inp['rw_k_a'][0]); m['rkT'] = pk(inp['rw_r_k'][0].reshape(-1))
    m['lnx_bc'] = bc(inp['rw_lnx'][0])
    m['wupT'] = f(inp['rw_w_up'][0].reshape(128, D)); m['aupT'] = f(inp['rw_a_up'][0].reshape(128, D)); m['g_up'] = f(inp['rw_g_up'][0])
    m['cwT'] = f(np.stack([pk(inp['lru_conv_w'][0][j]) for j in range(4)], axis=-1)); m['cbT'] = pk(inp['lru_conv_b'][0])
    gb = inp['lru_gate_b'][0].reshape(4, D)
    m['gbT'] = f(np.stack([pk(gb[i]) for i in range(4)], axis=-1))
    m['llT'] = f(np.stack([pk(inp['lru_l'][0][d]) for d in range(2)], axis=-1))
    gw = inp['lru_gate_w'][0].reshape(4, 8, 2, 64, 64)
    m['gwT'] = f(np.transpose(gw, (2, 3, 1, 0, 4)).reshape(128, 8, 4, 64))
    m['w_branch_a'] = f(inp['w_branch_a'][0]); m['w_branch_b'] = f(inp['w_branch_b'][0]); m['w_out'] = f(inp['w_out'][0])
    m['router_w'] = f(inp['router_w'][0]); m['router_b_bc'] = bc(inp['router_b'][0])
    m['ex_w_gu'] = f(inp['ex_w_gu'][0]); m['ex_w_down'] = f(inp['ex_w_down'][0])
    m['sh_w_gu'] = f(inp['sh_w_gu'][0]); m['sh_w_down'] = f(inp['sh_w_down'][0])
    p = np.arange(128)[:, None]; c = np.arange(128)[None, :]
    cs = np.zeros((128, 7, 128), np.float32)
    cs[:, 0] = (p == c); cs[:, 1] = (p < c); cs[:, 2] = (p <= c); cs[:, 3] = (p > c); cs[:, 4] = (p >= c)
    cs[:, 5] = ((p // 64) == (c // 64)); cs[:, 6] = 1.0
    m['consts'] = cs
    return m


_NC = {}


def kernel(**inputs):
    inp = {k: np.asarray(v) for k, v in inputs.items()}
    if 'full' not in _NC:
        _NC['full'] = build('full')
    nc = _NC['full']
    in_maps = [host_layout(inp, b) for b in range(8)]
    res = run_bass_kernel_spmd(nc, in_maps, core_ids=list(range(8)))
    return np.stack([np.asarray(r['out'], dtype=np.float32) for r in res.results], axis=0)
```

```python
import numpy as np
import os
from contextlib import ExitStack
DBGSKIP = os.environ.get('DBGSKIP', '')
MOESKIP = os.environ.get('MOESKIP', '')
LOWP_FROM = int(os.environ.get('LOWP_FROM', '6'))
import concourse.bass as bass
import concourse.mybir as mybir
from concourse.bass_utils import run_bass_kernel_spmd

F32 = mybir.dt.float32
BF16 = mybir.dt.bfloat16
AF = mybir.ActivationFunctionType
ALU = mybir.AluOpType
AX = mybir.AxisListType

D = 1024
T_CTX = 256
T_LAT = 4096
T_ALL = T_CTX + T_LAT
NCH = T_ALL // 128
IN_COLS = 7584
OFF_WD, OFF_AD, OFF_GD, OFF_LX, OFF_LG, OFF_GA, OFF_GB = 3072, 3200, 3328, 3488, 4512, 5536, 6560
NEXP = 64
TT = [(0, 256, True)] + [(256 + i * 512, 512, False) for i in range(8)]


class _Rec:
    def __getattr__(self, name):
        def f(*a, **k):
            return (name, a, k)
        return f


_REC = _Rec()


class Sched:
    ENG = ['pe', 'act', 'dve', 'pool', 'sp']

    def __init__(self, nc, es, ndma=16):
        self.nc = nc
        self.ops = {e: [] for e in self.ENG}
        self.cnt = {e: 0 for e in self.ENG}
        self.sem = {e: es.enter_context(nc.semaphore('s_' + e)) for e in self.ENG}
        self.dsem = {e: [es.enter_context(nc.semaphore('d_%s%d' % (e, i))) for i in range(ndma)]
                     for e in ('sp', 'act')}
        self.dcnt = {e: 0 for e in self.dsem}
        self.dval = {e: [0] * ndma for e in self.dsem}
        self.lastw = {}
        self.readers = {}
        self.waited = {e: {} for e in self.ENG}
        self.ndma = ndma

    def _deps(self, eng, r, w, is_dma):
        deps = []
        for x in r:
            lw = self.lastw.get(x)
            if lw is not None:
                deps.append(lw)
        for x in w:
            lw = self.lastw.get(x)
            if lw is not None and (is_dma or lw[0] != eng or lw[3]):
                deps.append(lw)
            for rd in self.readers.get(x, ()):
                if is_dma or rd[0] != eng or rd[3]:
                    deps.append(rd)
        out = []
        wd = self.waited[eng]
        for (pe_, sem, val, pdma) in deps:
            if pe_ == 'pe' and eng == 'pe' and not pdma and not is_dma:
                continue
            k = id(sem)
            if wd.get(k, 0) >= val:
                continue
            wd[k] = val
            out.append((sem, val))
        return out

    def _commit(self, tok, r, w):
        for x in r:
            self.readers.setdefault(x, []).append(tok)
        for x in w:
            self.lastw[x] = tok
            self.readers[x] = []

    def op(self, eng, fn, r=(), w=()):
        waits = self._deps(eng, r, w, False)
        self.cnt[eng] += 1
        tok = (eng, self.sem[eng], self.cnt[eng], False)
        self.ops[eng].append((fn(_REC), waits, self.sem[eng], 1))
        self._commit(tok, r, w)

    def dma(self, q, out, in_, r=(), w=(), **kw):
        waits = self._deps(q, r, w, True)
        i = self.dcnt[q]
        self.dcnt[q] += 1
        slot = i % self.ndma
        sem = self.dsem[q][slot]
        prev = self.dval[q][slot]
        if prev > 0 and self.waited[q].get(id(sem), 0) < prev:
            self.waited[q][id(sem)] = prev
            waits.append((sem, prev))
        self.dval[q][slot] = prev + 16
        tok = (q, sem, prev + 16, True)
        self.ops[q].append((('dma_start', (), dict(out=out, in_=in_, **kw)), waits, sem, 16))
        self._commit(tok, r, w)
        return tok

    def wait_all(self, eng, toks):
        self.ops[eng].append((None, [(t[1], t[2]) for t in toks], None, 0))

    def barrier(self):
        allw = [(self.sem[e], self.cnt[e]) for e in self.ENG if self.cnt[e] > 0]
        for q in self.dsem:
            for i in range(self.ndma):
                if self.dval[q][i] > 0:
                    allw.append((self.dsem[q][i], self.dval[q][i]))
        for e in self.ENG:
            waits = []
            for (sem, val) in allw:
                if self.waited[e].get(id(sem), 0) < val:
                    self.waited[e][id(sem)] = val
                    waits.append((sem, val))
            self.ops[e].append((None, waits, None, 0))

    def emit(self):
        with self.nc.Block() as block:
            def run(e, name):
                for call, waits, sem, inc in self.ops[name]:
                    for (s, v) in waits:
                        e.wait_ge(s, v)
                    if call is not None:
                        getattr(e, call[0])(*call[1], **call[2]).then_inc(sem, inc)
                self.ops[name] = []

            @block.tensor
            def _(e):
                run(e, 'pe')

            @block.scalar
            def _(e):
                run(e, 'act')

            @block.vector
            def _(e):
                run(e, 'dve')

            @block.gpsimd
            def _(e):
                run(e, 'pool')

            @block.sync
            def _(e):
                run(e, 'sp')


def build(stage='full', nhp=8):
    nc = bass.Bass("TRN2", target_bir_lowering=False)
    di = lambda name, shape, dt=F32: nc.dram_tensor(name, list(shape), dt, kind="ExternalInput").ap()
    x_d = di('x', [T_LAT, D]); ctx_d = di('ctx', [T_CTX, D])
    cvec_d = di('cvec', [128, 8, 2]); wmod_d = di('w_mod', [D, 6 * D]); bmod_d = di('b_modT', [128, 48])
    bmodbc_d = di('b_mod_bc', [128, 2, D])
    ngT_d = di('norm_gT', [128, 4, 8]); gpost_d = di('gpost_bc', [128, 2, D])
    win_d = di('w_in', [D, IN_COLS]); mu_d = di('muT', [128, 28, 2])
    w0_d = di('w0T', [128, 8, 2]); a0_d = di('a0T', [128, 8, 2]); kk_d = di('kkT', [128, 8]); ka_d = di('kaT', [128, 8])
    rk_d = di('rkT', [128, 8]); lnx_d = di('lnx_bc', [128, 2, D])
    wup_d = di('wupT', [128, D]); aup_d = di('aupT', [128, D]); gup_d = di('g_up', [160, D])
    cw_d = di('cwT', [128, 8, 4]); cb_d = di('cbT', [128, 8]); gb_d = di('gbT', [128, 8, 4]); ll_d = di('llT', [128, 8, 2])
    gw_d = di('gwT', [128, 8, 4, 64])
    wa_d = di('w_branch_a', [D, D]); wb_d = di('w_branch_b', [D, D]); wo_d = di('w_out', [D, D])
    rw_d = di('router_w', [D, NEXP]); rb_d = di('router_b_bc', [128, NEXP])
    egu_d = di('ex_w_gu', [NEXP, D, 512]); edn_d = di('ex_w_down', [NEXP, 256, D])
    sgu_d = di('sh_w_gu', [D, 512]); sdn_d = di('sh_w_down', [256, D])
    cst_d = di('consts', [128, 7, 128])
    out_d = nc.dram_tensor('out', [T_LAT, D], F32, kind="ExternalOutput").ap()
    ya_s = nc.dram_tensor('ya_s', [8, 128, T_LAT], BF16, kind="Internal").ap()
    yb_s = nc.dram_tensor('yb_s', [8, 128, T_LAT], BF16, kind="Internal").ap()
    h1_s = nc.dram_tensor('h1_s', [T_LAT, D], F32, kind="Internal").ap()
    uT_s = nc.dram_tensor('uT_s', [128, 8, T_LAT], BF16, kind="Internal").ap()
    xn_s = nc.dram_tensor('xn_s', [9, 128, 8 * 512], BF16, kind="Internal").ap()
    xnv = lambda ti: xn_s[ti].rearrange("p (k t) -> p k t", t=512)
    sgd_s = nc.dram_tensor('sgd_s', [2, 128, T_ALL], BF16, kind="Internal").ap()
    gt_s = nc.dram_tensor('gt_s', [128, 2, D], F32, kind="Internal").ap()
    dbg_d = None
    if stage != 'full':
        dbg_d = nc.dram_tensor('dbg', [T_LAT, D], F32, kind="ExternalOutput").ap()

    with ExitStack() as es:
        S = Sched(nc, es)

        def T(st, name, shape, dt):
            return st.enter_context(nc.sbuf_tensor('t_' + name, list(shape), dt))

        open_stacks = []

        def finish_early(src_ap, rows, cols, rname):
            ps = ExitStack(); open_stacks.append(ps)
            if True:
                df_ = T(ps, 'dbg_f', [128, 8], F32)
                S.op('pool', lambda e: e.memset(df_[:], 0.0), w=['dbg_f'])
                toks = [S.dma('sp', dbg_d[0:rows, 0:cols], src_ap, r=[rname], w=['dbg']),
                        S.dma('sp', out_d[0:128, 0:8], df_[:], r=['dbg_f'], w=['out'])]
                S.wait_all('sp', toks)
                S.emit()
            for st_ in reversed(open_stacks):
                st_.close()
            return nc

        def finish_early4(ys):
            ps = ExitStack(); open_stacks.append(ps)
            df_ = T(ps, 'dbg_f', [128, 8], F32)
            S.op('pool', lambda e: e.memset(df_[:], 0.0), w=['dbg_f'])
            toks = [S.dma('sp', dbg_d[0:128, :], ys[:, 0:8, :].rearrange("p a b -> p (a b)"), r=['ysum'], w=['dbg']),
                    S.dma('sp', dbg_d[128:256, :], ys[:, 8:16, :].rearrange("p a b -> p (a b)"), r=['ysum'], w=['dbg']),
                    S.dma('sp', dbg_d[256:384, :], ys[:, 16:24, :].rearrange("p a b -> p (a b)"), r=['ysum'], w=['dbg']),
                    S.dma('sp', dbg_d[384:512, :], ys[:, 24:32, :].rearrange("p a b -> p (a b)"), r=['ysum'], w=['dbg']),
                    S.dma('sp', out_d[0:128, 0:8], df_[:], r=['dbg_f'], w=['out'])]
            S.wait_all('sp', toks)
            S.emit()
            for st_ in reversed(open_stacks):
                st_.close()
            return nc

        def finish_rows(tile_):
            ps = ExitStack(); open_stacks.append(ps)
            df_ = T(ps, 'dbg_f', [128, 8], F32)
            S.op('pool', lambda e: e.memset(df_[:], 0.0), w=['dbg_f'])
            toks = [S.dma('sp', dbg_d.rearrange("(a b) d -> a (b d)", b=4)[0:128, :], tile_[:, 0:T_LAT], r=['hh0'], w=['dbg']),
                    S.dma('sp', out_d[0:128, 0:8], df_[:], r=['dbg_f'], w=['out'])]
            S.wait_all('sp', toks); S.emit()
            for st_ in reversed(open_stacks):
                st_.close()
            return nc

        def finish_rows2(h1s, gts):
            ps = ExitStack(); open_stacks.append(ps)
            df_ = T(ps, 'dbg_f', [128, D], F32)
            S.dma('sp', df_[:], h1s[0:128, :], r=['h1_s'], w=['dbg_f'])
            toks = [S.dma('sp', dbg_d[0:128, :], df_[:], r=['dbg_f'], w=['dbg']),
                    S.dma('sp', dbg_d[128:256, 0:256], gts[:, 0:4, :].rearrange("p a b -> p (a b)"), r=['gates'], w=['dbg']),
                    S.dma('sp', out_d[0:128, 0:8], df_[:, 0:8], r=['dbg_f'], w=['out'])]
            S.wait_all('sp', toks); S.emit()
            for st_ in reversed(open_stacks):
                st_.close()
            return nc

        PB = [es.enter_context(nc.psum_tensor('pb%d' % i, [128, 512], F32)) for i in range(7)]
        PT = es.enter_context(nc.psum_tensor('pbT', [128, 1024], BF16))

        cst = T(es, 'cst', [128, 7, 128], F32)
        cstb = T(es, 'cstb', [128, 7, 128], BF16)
        S.dma('sp', cst[:], cst_d[:, :, :], w=['cst'])
        S.op('dve', lambda e: e.tensor_copy(out=cstb[:], in_=cst[:]), r=['cst'], w=['cstb'])
        identb = cstb[:, 0, :]; identf = cst[:, 0, :]
        MASK = {'SU': cst[:, 1, :], 'IU': cst[:, 2, :], 'SL': cst[:, 3, :], 'IL': cst[:, 4, :]}
        bonesb = cstb[:, 5, :]; onesf = cst[:, 6, :]
        mG = T(es, 'mG', [128, 2, 512], BF16)
        for d_, (s_, i_) in enumerate([('SU', 'IU'), ('SL', 'IL')]):
            for q_, nm in enumerate([s_, i_, s_, i_]):
                S.op('pool', lambda e, d_=d_, q_=q_, nm=nm: e.tensor_copy(out=mG[:, d_, q_ * 128:(q_ + 1) * 128], in_=MASK[nm]),
                     r=['cst'], w=['mG'])
        mN = [MASK['SL'], MASK['SU']]

        def small(name, src, shape):
            t = T(es, name, shape, F32)
            S.dma('sp', t[:], src, w=[name])
            return t
        cvec = small('cvec', cvec_d[:, :, :], [128, 8, 2])
        bmodT = small('bmodT', bmod_d[:, :], [128, 48])
        ngT = small('ngT', ngT_d[:, :, :], [128, 4, 8])
        muT = small('muT', mu_d[:, :, :], [128, 28, 2])
        w0T = small('w0T', w0_d[:, :, :], [128, 8, 2]); a0T = small('a0T', a0_d[:, :, :], [128, 8, 2])
        kkT = small('kkT', kk_d[:, :], [128, 8]); kaT = small('kaT', ka_d[:, :], [128, 8]); rkT = small('rkT', rk_d[:, :], [128, 8])
        cwT = small('cwT', cw_d[:, :, :], [128, 8, 4]); cbT = small('cbT', cb_d[:, :], [128, 8])
        gbT = small('gbT', gb_d[:, :, :], [128, 8, 4]); llT = small('llT', ll_d[:, :, :], [128, 8, 2])
        SM = ['cvec', 'bmodT', 'ngT', 'muT', 'w0T', 'a0T', 'kkT', 'kaT', 'rkT', 'cwT', 'cbT', 'gbT', 'llT']

        cmu = T(es, 'cmu', [128, 28], F32)
        S.op('dve', lambda e: e.tensor_tensor(out=cmu[:], in0=muT[:, :, 0], in1=muT[:, :, 1], op=ALU.add), r=['muT'], w=['cmu'])
        S.op('dve', lambda e: e.tensor_scalar(out=cmu[:], in0=cmu[:], scalar1=-1.0, scalar2=1.0, op0=ALU.mult, op1=ALU.add), r=['cmu'], w=['cmu'])
        nw0 = T(es, 'nw0', [128, 8, 2], F32)
        S.op('dve', lambda e: e.tensor_scalar(out=nw0[:], in0=w0T[:], scalar1=-1.0, scalar2=None, op0=ALU.mult), r=['w0T'], w=['nw0'])
        oka = T(es, 'oka', [128, 8], F32)
        S.op('dve', lambda e: e.tensor_scalar(out=oka[:], in0=kaT[:], scalar1=-1.0, scalar2=1.0, op0=ALU.mult, op1=ALU.add), r=['kaT'], w=['oka'])
        nsp = T(es, 'nsp', [128, 8, 2], F32)
        S.op('act', lambda e: e.activation(out=nsp[:], in_=llT[:], func=AF.Softplus, scale=-1.0), r=['llT'], w=['nsp'])
        S.op('dve', lambda e: e.tensor_scalar(out=nsp[:], in0=nsp[:], scalar1=-8.0, scalar2=None, op0=ALU.mult), r=['nsp'], w=['nsp'])
        nsp2 = T(es, 'nsp2', [128, 8, 2], F32)
        S.op('dve', lambda e: e.tensor_scalar(out=nsp2[:], in0=nsp[:], scalar1=2.0, scalar2=None, op0=ALU.mult), r=['nsp'], w=['nsp2'])

        scT = T(es, 'scT', [128, 8, 2], F32)
        S.op('act', lambda e: e.activation(out=scT[:], in_=cvec[:], func=AF.Silu), r=['cvec'], w=['scT'])
        modT = T(es, 'modT', [128, 48, 2], F32)
        with ExitStack() as ps:
            gt_bc = T(ps, 'gt_bc', [128, 2, D], F32)
            scbc = T(ps, 'scbc', [128, 8, 128], F32)
            for k in range(8):
                S.op('act', lambda e, k=k: e.activation(out=scbc[:, k, :], in_=onesf, func=AF.Identity, scale=scT[:, k, 0:1]),
                     r=['cst', 'scT'], w=['scbc'])
            wms = [T(ps, 'wms%d' % i, [128, 8, 512], F32) for i in range(2)]
            wmv = wmod_d.rearrange("(k p) c -> p k c", p=128)
            for g in range(12):
                wm = wms[g % 2]; wn = 'wms%d' % (g % 2)
                S.dma('sp', wm[:], wmv[:, :, g * 512:(g + 1) * 512], w=[wn])
                bank = PB[g % 2]; bn = 'pb%d' % (g % 2)
                for sub in range(4):
                    j = g * 4 + sub
                    for k in range(8):
                        S.op('pe', lambda e, wm=wm, sub=sub, k=k, bank=bank: e.matmul(
                            bank[:, sub * 2:sub * 2 + 2], lhsT=wm[:, k, sub * 128:(sub + 1) * 128], rhs=scT[:, k, :],
                            start=(k == 0), stop=(k == 7)), r=[wn, 'scT'], w=[bn])
                    S.op('dve', lambda e, j=j, sub=sub, bank=bank: e.tensor_scalar(
                        out=modT[:, j, :], in0=bank[:, sub * 2:sub * 2 + 2], scalar1=bmodT[:, j:j + 1], scalar2=None, op0=ALU.add),
                        r=[bn, 'bmodT'], w=['modT'])
                if g // 2 in (2, 5):
                    which = 0 if g < 6 else 1
                    half = g % 2
                    bk = PB[2 + half]; bkn = 'pb%d' % (2 + half)
                    for k in range(8):
                        S.op('pe', lambda e, wm=wm, k=k, bk=bk: e.matmul(bk[:, :], lhsT=scbc[:, k, :], rhs=wm[:, k, :],
                                                                         start=(k == 0), stop=(k == 7)), r=[wn, 'scbc'], w=[bkn])
                    S.op('act', lambda e, which=which, half=half, bk=bk: e.activation(
                        out=gt_bc[:, which, half * 512:(half + 1) * 512], in_=bk[:, :], func=AF.Copy), r=[bkn], w=['gt_bc'])
            bmbc = T(ps, 'bmbc', [128, 2, D], F32); gpbc = T(ps, 'gpbc', [128, 2, D], F32)
            S.dma('sp', bmbc[:], bmodbc_d[:, :, :], w=['bmbc']); S.dma('sp', gpbc[:], gpost_d[:, :, :], w=['gpbc'])
            S.op('dve', lambda e: e.tensor_tensor(out=gt_bc[:], in0=gt_bc[:], in1=bmbc[:], op=ALU.add), r=['gt_bc', 'bmbc'], w=['gt_bc'])
            S.op('dve', lambda e: e.tensor_tensor(out=gt_bc[:], in0=gt_bc[:], in1=gpbc[:], op=ALU.mult), r=['gt_bc', 'gpbc'], w=['gt_bc'])
            S.dma('sp', gt_s[:, :, :], gt_bc[:], r=['gt_bc'], w=['gt_s'])
            S.barrier(); S.emit()
        gs1 = T(es, 'gs1', [128, 8, 2], F32); sh1 = T(es, 'sh1', [128, 8, 2], F32)
        gs2 = T(es, 'gs2', [128, 8], F32); sh2 = T(es, 'sh2', [128, 8], F32)
        for ci in range(2):
            S.op('dve', lambda e, ci=ci: e.scalar_tensor_tensor(out=gs1[:, :, ci], in0=modT[:, 8:16, ci], scalar=1.0, in1=ngT[:, 0, :],
                                                                 op0=ALU.add, op1=ALU.mult), r=['modT', 'ngT'], w=['gs1'])
            S.op('dve', lambda e, ci=ci: e.tensor_copy(out=sh1[:, :, ci], in_=modT[:, 0:8, ci]), r=['modT'], w=['sh1'])
        S.op('dve', lambda e: e.scalar_tensor_tensor(out=gs2[:], in0=modT[:, 32:40, 0], scalar=1.0, in1=ngT[:, 2, :],
                                                     op0=ALU.add, op1=ALU.mult), r=['modT', 'ngT'], w=['gs2'])
        S.op('dve', lambda e: e.tensor_copy(out=sh2[:], in_=modT[:, 24:32, 0]), r=['modT'], w=['sh2'])

        if stage == 'p1':
            return finish_early(modT[:].rearrange("p a b -> p (a b)"), 128, 96, 'modT')
        def norm_to_featT(st, xt, xtn, dstT, dstn, col0, gs_ap, sh_ap, tag):
            ss = st['ss']; xs = st['xs']
            S.op('act', lambda e: e.activation(out=st['junk'][:], in_=xt, func=AF.Square, accum_out=ss[:, 0:1]),
                 r=[xtn], w=[tag + 'junk', tag + 'ss'])
            S.op('dve', lambda e: e.tensor_scalar(out=ss[:, 1:2], in0=ss[:, 0:1], scalar1=1.0 / D, scalar2=1e-6, op0=ALU.mult, op1=ALU.add),
                 r=[tag + 'ss'], w=[tag + 'ss1'])
            S.op('act', lambda e: e.activation(out=ss[:, 2:3], in_=ss[:, 1:2], func=AF.Sqrt), r=[tag + 'ss1'], w=[tag + 'ss2'])
            S.op('dve', lambda e: e.reciprocal(out=ss[:, 3:4], in_=ss[:, 2:3]), r=[tag + 'ss2'], w=[tag + 'ss3'])
            S.op('dve', lambda e: e.tensor_scalar(out=xs[:], in0=xt, scalar1=ss[:, 3:4], scalar2=None, op0=ALU.mult),
                 r=[xtn, tag + 'ss3'], w=[tag + 'xs'])
            for k in range(8):
                S.op('pe', lambda e, k=k: e.transpose(out=PT[:, k * 128:(k + 1) * 128], in_=xs[:, k * 128:(k + 1) * 128], identity=identb),
                     r=[tag + 'xs', 'cstb'], w=['pbT'])
            for k in range(8):
                S.op('act', lambda e, k=k: e.activation(out=dstT[:, k, col0:col0 + 128], in_=PT[:, k * 128:(k + 1) * 128], func=AF.Identity,
                                                        scale=gs_ap(k), bias=sh_ap(k)), r=['pbT', 'gs1', 'sh1', 'gs2', 'sh2'], w=[dstn])

        xnt = [T(es, 'xnt%d' % i, [128, 8, 512], BF16) for i in range(2)]
        with ExitStack() as ps:
            xin = [T(ps, 'xin%d' % i, [128, D], F32) for i in range(2)]
            st = {'ss': T(ps, 'n_ss', [128, 4], F32), 'xs': T(ps, 'n_xs', [128, D], BF16), 'junk': T(ps, 'n_junk', [128, D], BF16)}
            for tti, (g0, n, is_ctx) in enumerate(TT):
                stg = xnt[tti % 2]; sn = 'xnt%d' % (tti % 2) + 'a'
                for j in range(n // 128):
                    ti = g0 // 128 + j
                    xt = xin[ti % 2]; xn_ = 'xin%d' % (ti % 2)
                    src = ctx_d[ti * 128:(ti + 1) * 128, :] if ti < 2 else x_d[(ti - 2) * 128:(ti - 1) * 128, :]
                    S.dma('sp', xt[:], src, w=[xn_])
                    ci = 1 if ti < 2 else 0
                    norm_to_featT(st, xt[:], xn_, stg, sn, j * 128,
                                  lambda k, ci=ci: gs1[:, k, ci:ci + 1], lambda k, ci=ci: sh1[:, k, ci:ci + 1], 'n_')
                S.dma('sp', xnv(tti)[:, :, 0:n], stg[:, :, 0:n], r=[sn], w=['xn_s'])
            S.barrier(); S.emit()

        if stage == 'p2':
            xf_ = T(es, 'xf_dbg', [128, 512], F32)
            S.dma('sp', xnt[0][:, :, 0:512], xnv(1)[:, :, 0:512], r=['xn_s'], w=['xnt0a', 'xnt0b'])
            S.op('dve', lambda e: e.tensor_copy(out=xf_[:], in_=xnt[0][:, 3, :]), r=['xnt0'], w=['xf_dbg'])
            return finish_early(xf_[:], 128, 512, 'xf_dbg')
        winv = win_d.rearrange("(k p) c -> p k c", p=128)
        wst = [T(es, 'wst%d' % i, [128, 8, 128], F32) for i in range(2)]
        wbf = T(es, 'wbf', [128, 8, 512], BF16)
        pj = {'i': 0, 'bank': 0, 'x': 0}

        def project_group(chunks, banks=(4, 5, 6)):
            offs = []
            o = 0
            for (col0, m, _) in chunks:
                i = pj['i']; pj['i'] += 1
                ws = wst[i % 2]
                S.dma('sp', ws[:, :, 0:m], winv[:, :, col0:col0 + m], w=['wst%d' % (i % 2)])
                S.op('pool', lambda e, ws=ws, o=o, m=m: e.tensor_copy(out=wbf[:, :, o:o + m], in_=ws[:, :, 0:m]), r=['wst%d' % (i % 2)], w=['wbf'])
                offs.append(o); o += m
            for ti, (g0, n, is_ctx) in enumerate(TT):
                xi = pj['x'] % 2; pj['x'] += 1
                xt = xnt[xi]; xtn = 'xnt%d' % xi
                S.dma('sp', xt[:, 0:4, 0:n], xnv(ti)[:, 0:4, 0:n], r=['xn_s'], w=[xtn + 'a'])
                S.dma('sp', xt[:, 4:8, 0:n], xnv(ti)[:, 4:8, 0:n], r=['xn_s'], w=[xtn + 'b'])
                for (col0, m, consumer), o in zip(chunks, offs):
                    bi = banks[pj['bank'] % len(banks)]; pj['bank'] += 1
                    bank = PB[bi]; bn = 'pb%d' % bi
                    for k in range(8):
                        S.op('pe', lambda e, k=k, bank=bank, n=n, m=m, o=o, xt=xt: e.matmul(bank[0:m, 0:n], lhsT=wbf[:, k, o:o + m], rhs=xt[:, k, 0:n],
                                                                                          start=(k == 0), stop=(k == 7)),
                             r=['wbf', xtn + ('a' if k < 4 else 'b')], w=[bn])
                    consumer(bank[0:m, 0:n], bn, g0, n, is_ctx)

        sh_t1 = [T(es, 'sh_t1_%d' % i, [128, 512], F32) for i in range(2)]
        shc = {'i': 0}

        def shift_consume(m, ci, final):
            def cons(ps_ap, bn, g0, n, is_ctx):
                i = shc['i']; shc['i'] += 1
                t1 = sh_t1[i % 2]; tn = 'sh_t1_%d' % (i % 2)
                rw = n if is_ctx else 64
                S.op('act', lambda e: e.activation(out=t1[0:m, 0:n], in_=ps_ap, func=AF.Identity, scale=cmu[0:m, ci:ci + 1]),
                     r=[bn, 'cmu'], w=[tn])
                zv = ps_ap.rearrange("p (r c) -> p r c", c=rw)
                tv = t1[0:m, 0:n].rearrange("p (r c) -> p r c", c=rw)
                S.op('dve', lambda e: e.scalar_tensor_tensor(out=tv[:, :, 1:rw], in0=zv[:, :, 0:rw - 1], scalar=muT[0:m, ci, 0:1], in1=tv[:, :, 1:rw],
                                                             op0=ALU.mult, op1=ALU.add), r=[bn, tn, 'muT'], w=[tn])
                S.op('dve', lambda e: e.scalar_tensor_tensor(out=tv[:, :, 0:rw - 1], in0=zv[:, :, 1:rw], scalar=muT[0:m, ci, 1:2], in1=tv[:, :, 0:rw - 1],
                                                             op0=ALU.mult, op1=ALU.add), r=[bn, tn, 'muT'], w=[tn])
                final(t1[0:m, 0:n], tn, g0, n)
            return cons

        rw = ExitStack(); open_stacks.append(rw)
        twd = T(rw, 'twd', [128, T_ALL], BF16); adT = T(rw, 'adT', [128, T_ALL], BF16)
        sgst = [T(rw, 'sgst%d' % i, [128, 512], BF16) for i in range(2)]
        sgt = T(rw, 'sgt', [128, 2, 512], BF16)
        sgc = {'i': 0}

        def sg_final(ch, m):
            def f(t1, tn, g0, n):
                i = sgc['i']; sgc['i'] += 1
                st_ = sgst[i % 2]; sn_ = 'sgst%d' % (i % 2)
                S.op('act', lambda e: e.activation(out=st_[0:m, 0:n], in_=t1, func=AF.Sigmoid), r=[tn], w=[sn_])
                S.dma('sp', sgd_s[ch, 0:m, g0:g0 + n], st_[0:m, 0:n], r=[sn_], w=['sgd_s'])
            return f
        project_group([
            (OFF_WD, 128, shift_consume(128, 24, lambda t1, tn, g0, n: S.op(
                'act', lambda e: e.activation(out=twd[:, g0:g0 + n], in_=t1, func=AF.Tanh), r=[tn], w=['twd']))),
            (OFF_AD, 128, shift_consume(128, 25, lambda t1, tn, g0, n: S.op(
                'pool', lambda e: e.tensor_copy(out=adT[:, g0:g0 + n], in_=t1), r=[tn], w=['adT']))),
            (OFF_GD, 128, shift_consume(128, 26, sg_final(0, 128))),
            (OFF_GD + 128, 32, shift_consume(32, 27, sg_final(1, 32)))])
        if stage == 'p3':
            xf_ = T(rw, 'xf_dbg', [128, 512], F32)
            S.op('dve', lambda e: e.tensor_copy(out=xf_[:], in_=twd[:, 256:768]), r=['twd'], w=['xf_dbg'])
            return finish_early(xf_[:], 128, 512, 'xf_dbg')
        ysum = T(rw, 'ysum', [128, 32, 128], F32)
        lw_st = ysum[:, 0:8, :].rearrange("p a b -> p (a b)"); wupb = T(rw, 'wupb', [128, D], BF16); aupb = T(rw, 'aupb', [128, D], BF16)
        gupb = T(rw, 'gupb', [128, 2, D], BF16)
        for src_, dst_, np_ in [(wup_d[:, :], wupb[:], 128), (aup_d[:, :], aupb[:], 128), (gup_d[0:128, :], gupb[:, 0, :], 128), (gup_d[128:160, :], gupb[0:32, 1, :], 32)]:
            S.dma('sp', lw_st[0:np_, :], src_, w=['ysum'])
            S.op('pool', lambda e, dst_=dst_, np_=np_: e.tensor_copy(out=dst_, in_=lw_st[0:np_, :]), r=['ysum'], w=['lwdst'])
        lnxbc = T(rw, 'lnxbc', [128, 2, 128], F32)

        rb = T(rw, 'rb', [128, T_ALL], BF16); kb = T(rw, 'kb', [128, T_ALL], BF16); vb = T(rw, 'vb', [128, T_ALL], BF16)
        kkb = T(rw, 'kkb', [128, T_ALL], BF16); ksum = T(rw, 'ksum', [128, T_LAT], BF16)
        Vm = T(rw, 'Vm', [128, NCH, 128], BF16)
        yaT = [T(rw, 'yaT%d' % i, [128, 512], BF16) for i in range(2)]
        PL = T(rw, 'PL', [128, 2, NCH], F32)
        seg_sh = {}
        for nm in ['nlw', 'cn', 'en']:
            seg_sh[nm] = T(rw, 'sgs_%s' % nm, [128, 512], F32)
        for nm in ['Ee', 'Ec', 'Ei', 'Et', 'af', 'beta', 'kd', 'tt']:
            seg_sh[nm] = T(rw, 'sgs_%s' % nm, [128, 512], F32 if nm == 'tt' else BF16)

        def seg_bufs(i):
            b = dict(seg_sh)
            for nm in ['aT', 'rT', 'bT', 'kT', 'BhT', 'KhT']:
                b[nm] = T(rw, 'sg%d_%s' % (i, nm), [128, 512], BF16)
            b['Bhm'] = T(rw, 'sg%d_Bhm' % i, [128, 4, 128], BF16)
            b['Khm'] = T(rw, 'sg%d_Khm' % i, [128, 4, 128], BF16)
            b['n'] = 'sg%d' % i
            return b
        SG = [seg_bufs(0), seg_bufs(1)]
        Gt = [T(rw, 'G%d' % i, [128, 512], BF16) for i in range(4)]
        N0 = [T(rw, 'N0_%d' % i, [128, 128], F32) for i in range(4)]
        NT0 = [T(rw, 'NT0_%d' % i, [128, 128], F32) for i in range(4)]
        NP = [[T(rw, 'NP%d_%d' % (i, j), [128, 256], F32) for j in range(2)] for i in range(4)]
        Xb = [[T(rw, 'Xb%d_%d' % (i, j), [128, 128], F32) for j in range(2)] for i in range(4)]
        Xf = [T(rw, 'Xf%d' % i, [128, 128], BF16) for i in range(4)]
        MTs = [T(rw, 'MTs%d' % i, [128, 64], F32) for i in range(3)]; Nns = [T(rw, 'Nns%d' % i, [128, 64], F32) for i in range(3)]; RpT = [T(rw, 'RpT%d' % i, [128, 128], BF16) for i in range(3)]
        Sst = [[T(rw, 'Sst%d_%d' % (d_, i), [128, 64], F32) for i in range(2)] for d_ in range(2)]
        Sb = [T(rw, 'Sb%d' % i, [128, 64], BF16) for i in range(2)]
        kk_t = [seg_sh['tt'], seg_sh['cn']]
        kk_q = [seg_sh['Ee'], seg_sh['Ec']]
        kk_r = [seg_sh['nlw'], seg_sh['en']]
        KKN = [('sgs_tt', 'sgs_Ee', 'sgs_nlw'), ('sgs_cn', 'sgs_Ec', 'sgs_en')]
        finA = [{nm: T(rw, 'fin%d_' % r_ + nm, shp, dt) for nm, shp, dt in [
            ('st', [128, 24], F32), ('yc', [128, 128], F32), ('sq', [128, 128], BF16), ('bon', [128, 2], F32),
('yo', [128, 128], BF16), ('pr', [128, 512], BF16)]} for r_ in range(2)]

        for hp in range(nhp):
            project_group([
                (hp * 128, 128, shift_consume(128, hp, lambda t1, tn, g0, n: S.op(
                    'pool', lambda e: e.tensor_copy(out=rb[:, g0:g0 + n], in_=t1), r=[tn], w=['rb']))),
                (D + hp * 128, 128, shift_consume(128, 8 + hp, lambda t1, tn, g0, n: S.op(
                    'pool', lambda e: e.tensor_copy(out=kb[:, g0:g0 + n], in_=t1), r=[tn], w=['kb']))),
                (2 * D + hp * 128, 128, shift_consume(128, 16 + hp, lambda t1, tn, g0, n: S.op(
                    'pool', lambda e: e.tensor_copy(out=vb[:, g0:g0 + n], in_=t1), r=[tn], w=['vb'])))])
            S.dma('sp', lnxbc[:, 0, :], lnx_d[:, 0, hp * 128:(hp + 1) * 128], w=['lnxbc'])
            S.dma('sp', lnxbc[:, 1, :], lnx_d[:, 1, hp * 128:(hp + 1) * 128], w=['lnxbc'])
            for ti, (g0, n, is_ctx) in enumerate(TT):
                q = ti % 2
                kt, kq, kr = kk_t[q], kk_q[q], kk_r[q]
                ktn, kqn, krn = KKN[q]
                kbank = PB[6] if q == 0 else PB[3]; kbn = 'pb6' if q == 0 else 'pb3'
                S.op('act', lambda e, kq=kq, g0=g0, n=n: e.activation(out=kq[:, 0:n], in_=kb[:, g0:g0 + n], func=AF.Square, scale=kkT[:, hp:hp + 1]), r=['kb', 'kkT'], w=[kqn])
                S.op('pe', lambda e, kq=kq, n=n, kbank=kbank: e.matmul(kbank[:, 0:n], lhsT=bonesb, rhs=kq[:, 0:n], start=True, stop=True), r=[kqn, 'cstb'], w=[kbn])
                S.op('act', lambda e, kr=kr, n=n, kbank=kbank: e.activation(out=kr[:, 0:n], in_=kbank[:, 0:n], func=AF.Sqrt), r=[kbn], w=[krn])
                S.op('dve', lambda e, kr=kr, n=n: e.tensor_scalar(out=kr[:, 0:n], in0=kr[:, 0:n], scalar1=1e-12, scalar2=None, op0=ALU.max), r=[krn], w=[krn])
                S.op('dve', lambda e, kr=kr, n=n: e.reciprocal(out=kr[:, 0:n], in_=kr[:, 0:n]), r=[krn], w=[krn])
                S.op('dve', lambda e, kr=kr, g0=g0, n=n: e.scalar_tensor_tensor(out=kkb[:, g0:g0 + n], in0=kb[:, g0:g0 + n], scalar=kkT[:, hp:hp + 1], in1=kr[:, 0:n], op0=ALU.mult, op1=ALU.mult),
                     r=['kb', 'kkT', krn], w=['kkb'])
            for c0 in range(0, NCH, 8):
                nchk = min(8, NCH - c0)
                for j in range(nchk):
                    c = c0 + j
                    S.op('pe', lambda e, c=c, j=j: e.transpose(out=PT[:, j * 128:(j + 1) * 128], in_=vb[:, c * 128:(c + 1) * 128], identity=identb),
                         r=['vb', 'cstb'], w=['pbT'])
                S.op('act', lambda e, c0=c0, nchk=nchk: e.activation(out=Vm[:, c0:c0 + nchk, :], in_=PT[:, 0:nchk * 128].rearrange("p (a b) -> p a b", b=128), func=AF.Copy),
                     r=['pbT'], w=['Vm'])

            if stage == 'p4':
                xf_ = ysum[:, 0:8, :].rearrange("p a b -> p (a b)")
                S.op('dve', lambda e: e.tensor_copy(out=xf_[:, 0:512], in_=kkb[:, 256:768]), r=['kkb'], w=['xf_dbg'])
                S.op('dve', lambda e: e.tensor_copy(out=xf_[:, 512:1024], in_=Vm[:, 2:6, :].rearrange("p a b -> p (a b)")), r=['Vm'], w=['xf_dbg'])
                return finish_early(xf_[:], 128, 1024, 'xf_dbg')
            segsD = [[[0, 1]] + [[2 + 4 * s_ + j for j in range(4)] for s_ in range(8)],
                     [[1, 0]] + [[2 + 4 * s_ + j for j in range(3, -1, -1)] for s_ in range(7, -1, -1)]]
            for d in range(2):
                S.op('pool', lambda e, d=d: e.memset(Sst[d][0][:], 0.0), w=['Sst%d_0' % d])
                S.op('pool', lambda e, d=d: e.memset(Sb[d][:], 0.0), w=['Sb%d' % d])
            par = [0, 0]
            seen_y = set(); seen_k = set()
            HS = [slice(0, 64), slice(64, 128)]

            def emit_prep(si, d):
                seg = segsD[d][si]
                B = SG[d]; bn_ = B['n']
                lo = min(seg) * 128; n = len(seg) * 128
                sl = slice(lo, lo + n)
                dsl = slice(d * 64, d * 64 + 64)
                S.op('pe', lambda e, sl=sl, n=n, dsl=dsl: e.matmul(PB[6][:, 0:n], lhsT=wupb[dsl, hp * 128:(hp + 1) * 128], rhs=twd[dsl, sl], start=True, stop=True),
                     r=['lwdst', 'twd'], w=['pb6'])
                S.op('act', lambda e, B=B, n=n, d=d: e.activation(out=B['tt'][:, 0:n], in_=PB[6][:, 0:n], func=AF.Softplus, scale=-1.0, bias=nw0[:, hp, d:d + 1]),
                     r=['pb6', 'nw0'], w=['sgs_tt'])
                S.op('pe', lambda e, sl=sl, n=n, dsl=dsl: e.matmul(PB[6][:, 0:n], lhsT=aupb[dsl, hp * 128:(hp + 1) * 128], rhs=adT[dsl, sl], start=True, stop=True),
                     r=['lwdst', 'adT'], w=['pb6'])
                S.op('act', lambda e, B=B, n=n: e.activation(out=B['nlw'][:, 0:n], in_=B['tt'][:, 0:n], func=AF.Exp, scale=-1.0, bias=-0.5),
                     r=['sgs_tt'], w=['sgs_nlw'])
                S.op('act', lambda e, B=B, n=n, d=d: e.activation(out=B['af'][:, 0:n], in_=PB[6][:, 0:n], func=AF.Sigmoid, bias=a0T[:, hp, d:d + 1]),
                     r=['pb6', 'a0T'], w=['sgs_af'])
                for c in seg:
                    o = c * 128 - lo
                    if d == 0:
                        S.op('dve', lambda e, B=B, o=o: e.tensor_tensor_scan(out=B['cn'][:, o:o + 128], data0=onesf, data1=B['nlw'][:, o:o + 128],
                                                                             initial=0.0, op0=ALU.mult, op1=ALU.add), r=['sgs_nlw', 'cst'], w=['sgs_cn'])
                        tot = B['cn'][:, o + 127:o + 128]
                    else:
                        S.op('dve', lambda e, B=B, o=o: e.tensor_tensor_scan(out=B['cn'][:, o + 127:(o - 1 if o > 0 else None):-1], data0=onesf,
                                                                             data1=B['nlw'][:, o + 127:(o - 1 if o > 0 else None):-1],
                                                                             initial=0.0, op0=ALU.mult, op1=ALU.add), r=['sgs_nlw', 'cst'], w=['sgs_cn'])
                        tot = B['cn'][:, o:o + 1]
                    S.op('dve', lambda e, tot=tot, c=c, d=d: e.tensor_scalar(out=PL[:, d, c:c + 1], in0=tot, scalar1=-1.0, scalar2=None, op0=ALU.mult),
                         r=['sgs_cn'], w=['PLn'])
                    S.op('act', lambda e, B=B, o=o, c=c, d=d: e.activation(out=B['Et'][:, o:o + 128], in_=B['cn'][:, o:o + 128], func=AF.Exp, bias=PL[:, d, c:c + 1]),
                         r=['sgs_cn', 'PLn'], w=['sgs_Et'])
                    S.op('act', lambda e, c=c, d=d: e.activation(out=PL[:, d, c:c + 1], in_=PL[:, d, c:c + 1], func=AF.Exp), r=['PLn'], w=['PLn', 'PL'])
                S.op('pool', lambda e, B=B, n=n: e.tensor_tensor(out=B['en'][:, 0:n], in0=B['cn'][:, 0:n], in1=B['nlw'][:, 0:n], op=ALU.subtract),
                     r=['sgs_cn', 'sgs_nlw'], w=['sgs_en'])
                S.op('act', lambda e, B=B, n=n: e.activation(out=B['Ee'][:, 0:n], in_=B['en'][:, 0:n], func=AF.Exp, scale=-1.0), r=['sgs_en'], w=['sgs_Ee'])
                S.op('act', lambda e, B=B, n=n: e.activation(out=B['Ec'][:, 0:n], in_=B['cn'][:, 0:n], func=AF.Exp, scale=-1.0), r=['sgs_cn'], w=['sgs_Ec'])
                S.op('act', lambda e, B=B, n=n: e.activation(out=B['Ei'][:, 0:n], in_=B['cn'][:, 0:n], func=AF.Exp), r=['sgs_cn'], w=['sgs_Ei'])
                S.op('dve', lambda e, B=B, sl=sl, n=n: e.scalar_tensor_tensor(out=B['aT'][:, 0:n], in0=kkb[:, sl], scalar=-1.0, in1=B['Ee'][:, 0:n], op0=ALU.mult, op1=ALU.mult),
                     r=['kkb', 'sgs_Ee'], w=[bn_ + 'aT'])
                S.op('pool', lambda e, B=B, sl=sl, n=n: e.tensor_tensor(out=B['rT'][:, 0:n], in0=rb[:, sl], in1=B['Ec'][:, 0:n], op=ALU.mult),
                     r=['rb', 'sgs_Ec'], w=[bn_ + 'rT'])
                S.op('pool', lambda e, B=B, sl=sl, n=n: e.tensor_tensor(out=B['beta'][:, 0:n], in0=kkb[:, sl], in1=B['af'][:, 0:n], op=ALU.mult),
                     r=['kkb', 'sgs_af'], w=['sgs_beta'])
                S.op('dve', lambda e, B=B, n=n: e.tensor_tensor(out=B['bT'][:, 0:n], in0=B['beta'][:, 0:n], in1=B['Ei'][:, 0:n], op=ALU.mult),
                     r=['sgs_beta', 'sgs_Ei'], w=[bn_ + 'bT'])
                S.op('pool', lambda e, B=B, n=n: e.tensor_tensor(out=B['BhT'][:, 0:n], in0=B['beta'][:, 0:n], in1=B['Et'][:, 0:n], op=ALU.mult),
                     r=['sgs_beta', 'sgs_Et'], w=[bn_ + 'BhT'])
                S.op('dve', lambda e, B=B, n=n: e.tensor_scalar(out=B['tt'][:, 0:n], in0=B['af'][:, 0:n], scalar1=kaT[:, hp:hp + 1], scalar2=oka[:, hp:hp + 1], op0=ALU.mult, op1=ALU.add),
                     r=['sgs_af', 'kaT', 'oka'], w=['sgs_tt'])
                S.op('pool', lambda e, B=B, sl=sl, n=n: e.tensor_tensor(out=B['kd'][:, 0:n], in0=kb[:, sl], in1=B['tt'][:, 0:n], op=ALU.mult),
                     r=['kb', 'sgs_tt'], w=['sgs_kd'])
                S.op('dve', lambda e, B=B, n=n: e.tensor_tensor(out=B['kT'][:, 0:n], in0=B['kd'][:, 0:n], in1=B['Ei'][:, 0:n], op=ALU.mult),
                     r=['sgs_kd', 'sgs_Ei'], w=[bn_ + 'kT'])
                S.op('pool', lambda e, B=B, n=n: e.tensor_tensor(out=B['KhT'][:, 0:n], in0=B['kd'][:, 0:n], in1=B['Et'][:, 0:n], op=ALU.mult),
                     r=['sgs_kd', 'sgs_Et'], w=[bn_ + 'KhT'])
                if lo >= T_CTX:
                    ls = slice(lo - T_CTX, lo - T_CTX + n)
                    if lo not in seen_k:
                        seen_k.add(lo)
                        S.op('pool', lambda e, B=B, ls=ls, n=n: e.tensor_copy(out=ksum[:, ls], in_=B['kd'][:, 0:n]), r=['sgs_kd'], w=['ksum'])
                    else:
                        S.op('pool', lambda e, B=B, ls=ls, n=n: e.tensor_tensor(out=ksum[:, ls], in0=ksum[:, ls], in1=B['kd'][:, 0:n], op=ALU.add), r=['sgs_kd', 'ksum'], w=['ksum'])
                nck = len(seg)
                for j in range(nck):
                    S.op('pe', lambda e, B=B, j=j: e.transpose(out=PT[:, j * 128:(j + 1) * 128], in_=B['BhT'][:, j * 128:(j + 1) * 128], identity=identb),
                         r=[bn_ + 'BhT', 'cstb'], w=['pbT'])
                    S.op('pe', lambda e, B=B, j=j: e.transpose(out=PT[:, 512 + j * 128:512 + (j + 1) * 128], in_=B['KhT'][:, j * 128:(j + 1) * 128], identity=identb),
                         r=[bn_ + 'KhT', 'cstb'], w=['pbT'])
                S.op('act', lambda e, B=B, nck=nck: e.activation(out=B['Bhm'][:, 0:nck, :], in_=PT[:, 0:nck * 128].rearrange("p (a b) -> p a b", b=128), func=AF.Copy),
                     r=['pbT'], w=[bn_ + 'Bhm'])
                S.op('act', lambda e, B=B, nck=nck: e.activation(out=B['Khm'][:, 0:nck, :], in_=PT[:, 512:512 + nck * 128].rearrange("p (a b) -> p a b", b=128), func=AF.Copy),
                     r=['pbT'], w=[bn_ + 'Khm'])


            stream = []
            for si in range(9):
                for jj in range(len(segsD[0][si])):
                    for d in range(2):
                        seg = segsD[d][si]; c = seg[jj]; lo = min(seg) * 128
                        o = c * 128 - lo
                        for hl in range(2):
                            stream.append(dict(d=d, hl=hl, c=c, cs=slice(o, o + 128), jc=o // 128, hs=HS[hl], pb=hl * 64, B=SG[d], bn=SG[d]['n'],
                                               prep=(si, d) if (jj == 0 and hl == 0) else None))
            NG = 3
            for g0_ in range(0, len(stream), NG):
                INS = stream[g0_:g0_ + NG]
                for sl_, I in enumerate(INS):
                    I['i'] = sl_; I['XB'] = PB[2 * sl_]; I['xn'] = 'pb%d' % (2 * sl_); I['QB'] = PB[2 * sl_ + 1]; I['qn'] = 'pb%d' % (2 * sl_ + 1)
                    if I['prep'] is not None:
                        emit_prep(*I['prep'])
                for I in INS:
                    i, B, hs, cs, bn_, d = I['i'], I['B'], I['hs'], I['cs'], I['bn'], I['d']
                    XB_, xn, QB_, qn = I['XB'], I['xn'], I['QB'], I['qn']
                    for q_, (l_, r_) in enumerate([('bT', 'aT'), ('bT', 'rT'), ('kT', 'aT'), ('kT', 'rT')]):
                        S.op('pe', lambda e, XB_=XB_, B=B, hs=hs, cs=cs, q_=q_, l_=l_, r_=r_: e.matmul(XB_[:, q_ * 128:(q_ + 1) * 128], lhsT=B[l_][hs, cs], rhs=B[r_][hs, cs], start=True, stop=True),
                             r=[bn_ + l_, bn_ + r_], w=[xn])
                    S.op('pe', lambda e, QB_=QB_, B=B, hs=hs, cs=cs: e.matmul(QB_[:, 0:128], lhsT=B['aT'][hs, cs], rhs=B['bT'][hs, cs], start=True, stop=True),
                         r=[bn_ + 'aT', bn_ + 'bT'], w=[qn])
                for I in INS:
                    i, d, XB_, xn, QB_, qn = I['i'], I['d'], I['XB'], I['xn'], I['QB'], I['qn']
                    S.op('dve', lambda e, i=i, d=d, XB_=XB_: e.tensor_tensor(out=Gt[i][:], in0=XB_[:, :], in1=mG[:, d, :], op=ALU.mult), r=[xn, 'mG'], w=['G%d' % i])
                    S.op('dve', lambda e, i=i, d=d, XB_=XB_: e.tensor_tensor(out=NT0[i][:], in0=XB_[:, 0:128], in1=mN[1 - d], op=ALU.mult), r=[xn, 'cst'], w=['N0_%d' % i])
                    S.op('dve', lambda e, i=i, d=d, QB_=QB_: e.tensor_tensor(out=N0[i][:], in0=QB_[:, 0:128], in1=mN[d], op=ALU.mult), r=[qn, 'cst'], w=['N0_%d' % i])
                for I in INS:
                    i, B, hs, cs, bn_, pb_, c, XB_, xn = I['i'], I['B'], I['hs'], I['cs'], I['bn'], I['pb'], I['c'], I['XB'], I['xn']
                    S.op('pe', lambda e, XB_=XB_, B=B, hs=hs, cs=cs, pb_=pb_: e.matmul(XB_[:, 0:64], lhsT=B['aT'][hs, cs], rhs=identb[hs, pb_:pb_ + 64], start=True, stop=False),
                         r=[bn_ + 'aT', 'cstb', 'G%d' % i, 'N0_%d' % i], w=[xn])
                    S.op('pe', lambda e, XB_=XB_, i=i, pb_=pb_, c=c: e.matmul(XB_[:, 64:128], lhsT=Gt[i][:, 256:384], rhs=Vm[:, c, pb_:pb_ + 64], start=False, stop=False, skip_group_check=True),
                         r=['G%d' % i, 'Vm'], w=[xn])
                Ncur = [N0[i][:] for i in range(NG)]; NTcur = [NT0[i][:] for i in range(NG)]
                Nres = [['N0_%d' % i] for i in range(NG)]
                for p in range(7):
                    for I in INS:
                        i, XB_, xn, QB_, qn = I['i'], I['XB'], I['xn'], I['QB'], I['qn']
                        xbn = 'Xb%d_%d' % (i, p % 2)
                        lowp = (p >= LOWP_FROM)
                        xb_ap = Xb[i][p % 2][:].bitcast(BF16)[:, 0:128] if lowp else Xb[i][p % 2][:]
                        S.op('act', lambda e, XB_=XB_, xb_ap=xb_ap: e.activation(out=xb_ap, in_=XB_[:, 0:128], func=AF.Copy), r=[xn], w=[xbn])
                        S.op('pe', lambda e, XB_=XB_, xb_ap=xb_ap, nt=NTcur[i], p=p: e.matmul(XB_[:, 0:128], lhsT=nt, rhs=xb_ap, start=False, stop=(p == 6), skip_group_check=True),
                             r=[xbn] + Nres[i], w=[xn])
                        if p < 6:
                            S.op('pe', lambda e, QB_=QB_, nt=NTcur[i], nn=Ncur[i]: e.matmul(QB_[:, 128:256], lhsT=nt, rhs=nn, start=True, stop=True), r=Nres[i], w=[qn])
                            S.op('pe', lambda e, QB_=QB_, nt=NTcur[i], nn=Ncur[i]: e.matmul(QB_[:, 256:384], lhsT=nn, rhs=nt, start=True, stop=True), r=Nres[i], w=[qn])
                            for _dm in range(int(os.environ.get('DUMMY', '0'))):
                                S.op('pe', lambda e, QB_=QB_: e.matmul(QB_[:, 384:512], lhsT=identb, rhs=identb, start=True, stop=True), r=['cstb'], w=[qn])
                            npn = 'NP%d_%d' % (i, p % 2)
                            npt_ap = NP[i][p % 2][:].bitcast(BF16)[:, 0:256] if p >= LOWP_FROM - 1 else NP[i][p % 2][:]
                            S.op('dve', lambda e, QB_=QB_, npt_ap=npt_ap: e.tensor_copy(out=npt_ap, in_=QB_[:, 128:384]), r=[qn], w=[npn])
                            Ncur[i] = npt_ap[:, 0:128]; NTcur[i] = npt_ap[:, 128:256]; Nres[i] = [npn]
                for I in INS:
                    i, XB_, xn = I['i'], I['XB'], I['xn']
                    S.op('act', lambda e, i=i, XB_=XB_: e.activation(out=Xf[i][:], in_=XB_[:, 0:128], func=AF.Copy), r=[xn], w=['Xf%d' % i])
                for I in INS:
                    i, B, hs, cs, bn_, pb_, c, d, jc, hl, xbk, xn = I['i'], I['B'], I['hs'], I['cs'], I['bn'], I['pb'], I['c'], I['d'], I['jc'], I['hl'], I['XB'], I['xn']
                    S.op('pe', lambda e, B=B, hs=hs, pb_=pb_, jc=jc, i=i, xbk=xbk: e.matmul(xbk[hs, 128:192], lhsT=B['Bhm'][:, jc, pb_:pb_ + 64], rhs=Xf[i][:, 64:128], start=True, stop=False),
                         r=[bn_ + 'Bhm', 'Xf%d' % i], w=[xn])
                    S.op('pe', lambda e, B=B, hs=hs, pb_=pb_, jc=jc, c=c, xbk=xbk: e.matmul(xbk[hs, 128:192], lhsT=B['Khm'][:, jc, pb_:pb_ + 64], rhs=Vm[:, c, pb_:pb_ + 64], start=False, stop=True),
                         r=[bn_ + 'Khm', 'Vm'], w=[xn])
                    S.op('pe', lambda e, B=B, hs=hs, pb_=pb_, jc=jc, i=i, xbk=xbk: e.matmul(xbk[hs, 192:256], lhsT=Xf[i][:, 0:64], rhs=B['Bhm'][:, jc, pb_:pb_ + 64], start=True, stop=True),
                         r=[bn_ + 'Bhm', 'Xf%d' % i], w=[xn])
                    S.op('pe', lambda e, hs=hs, i=i, xbk=xbk: e.matmul(xbk[hs, 256:384], lhsT=Xf[i][:, 0:64], rhs=Gt[i][:, 128:256], start=True, stop=True),
                         r=['G%d' % i, 'Xf%d' % i], w=[xn])
                    S.op('dve', lambda e, hs=hs, pb_=pb_, d=d, c=c, i=i, xbk=xbk: e.scalar_tensor_tensor(out=MTs[i][hs, :], in0=identf[hs, pb_:pb_ + 64], scalar=PL[hs, d, c:c + 1], in1=xbk[hs, 192:256],
                                                                                                   op0=ALU.mult, op1=ALU.add), r=[xn, 'PL', 'cst'], w=['MTs%d' % i])
                    S.op('dve', lambda e, hs=hs, i=i, xbk=xbk: e.tensor_copy(out=Nns[i][hs, :], in_=xbk[hs, 128:192]), r=[xn], w=['Nns%d' % i])
                    S.op('dve', lambda e, B=B, hs=hs, cs=cs, i=i, xbk=xbk: e.tensor_tensor(out=RpT[i][hs, :], in0=xbk[hs, 256:384], in1=B['rT'][hs, cs], op=ALU.add), r=[xn, bn_ + 'rT'], w=['RpT%d' % i])
                for I in INS:
                    i, hs, pb_, c, d, hl, xbk, xn = I['i'], I['hs'], I['pb'], I['c'], I['d'], I['hl'], I['XB'], I['xn']
                    sp_ = par[d]
                    st0, st1 = Sst[d][sp_], Sst[d][1 - sp_]; sn0, sn1 = 'Sst%d_%d' % (d, sp_), 'Sst%d_%d' % (d, 1 - sp_)
                    S.op('pe', lambda e, hs=hs, i=i, st0=st0, xbk=xbk: e.matmul(xbk[hs, 384:448], lhsT=MTs[i][hs, :], rhs=st0[hs, :], start=True, stop=True),
                         r=['MTs%d' % i, sn0, 'Nns%d' % i, 'RpT%d' % i], w=[xn])
                    S.op('dve', lambda e, hs=hs, i=i, st1=st1, xbk=xbk: e.tensor_tensor(out=st1[hs, :], in0=xbk[hs, 384:448], in1=Nns[i][hs, :], op=ALU.add),
                         r=[xn, 'Nns%d' % i], w=[sn1])
                    if c >= 2:
                        lc = c - 2
                        S.op('pe', lambda e, hs=hs, d=d, i=i, xbk=xbk: e.matmul(xbk[:, 448:512], lhsT=RpT[i][hs, :], rhs=Sb[d][hs, :], start=True, stop=False),
                             r=['RpT%d' % i, 'Sb%d' % d], w=[xn])
                        S.op('pe', lambda e, i=i, xbk=xbk: e.matmul(xbk[:, 448:512], lhsT=Gt[i][:, 128:256], rhs=Xf[i][:, 64:128], start=False, stop=False),
                             r=['G%d' % i, 'Xf%d' % i], w=[xn])
                        S.op('pe', lambda e, i=i, pb_=pb_, c=c, xbk=xbk: e.matmul(xbk[:, 448:512], lhsT=Gt[i][:, 384:512], rhs=Vm[:, c, pb_:pb_ + 64], start=False, stop=True),
                             r=['G%d' % i, 'Vm'], w=[xn])
                        key = (lc, hl)
                        if key not in seen_y:
                            seen_y.add(key)
                            S.op('dve', lambda e, pb_=pb_, lc=lc, xbk=xbk: e.tensor_copy(out=ysum[:, lc, pb_:pb_ + 64], in_=xbk[:, 448:512]), r=[xn], w=['ysum'])
                        else:
                            S.op('dve', lambda e, pb_=pb_, lc=lc, xbk=xbk: e.tensor_tensor(out=ysum[:, lc, pb_:pb_ + 64], in0=xbk[:, 448:512], in1=ysum[:, lc, pb_:pb_ + 64], op=ALU.add),
                                 r=[xn, 'ysum'], w=['ysum'])
                    S.op('act', lambda e, d=d, hs=hs, st1=st1: e.activation(out=Sb[d][hs, :], in_=st1[hs, :], func=AF.Copy), r=[sn1], w=['Sb%d' % d])
                    if hl == 1:
                        par[d] = 1 - par[d]
            if stage == 'rwkv_y':
                return finish_early(ysum[:].rearrange("p a b -> p (a b)"), 128, 1024, 'ysum') if False else finish_early4(ysum)
            for s8 in range(8):
                finp = finA[s8 % 2]; FP_ = 'fin%d_' % (s8 % 2)
                ls = slice(s8 * 512, (s8 + 1) * 512); gsl = slice(T_CTX + s8 * 512, T_CTX + (s8 + 1) * 512)
                S.dma('sp', sgt[:, 0, :], sgd_s[0, :, gsl], r=['sgd_s'], w=['sgt'])
                S.dma('sp', sgt[0:32, 1, :], sgd_s[1, 0:32, gsl], r=['sgd_s'], w=['sgt'])
                S.op('dve', lambda e, ls=ls, gsl=gsl: e.tensor_tensor(out=finp['pr'][:], in0=rb[:, gsl], in1=ksum[:, ls], op=ALU.mult), r=['rb', 'ksum'], w=[FP_ + 'pr'])
                S.op('dve', lambda e: e.tensor_scalar(out=finp['pr'][:], in0=finp['pr'][:], scalar1=rkT[:, hp:hp + 1], scalar2=None, op0=ALU.mult), r=[FP_ + 'pr', 'rkT'], w=[FP_ + 'pr'])
                for j in range(4):
                    lc = s8 * 4 + j; c = lc + 2
                    fin = finA[j % 2]; FN_ = 'fin%d_' % (j % 2)
                    gbk = PB[5] if j % 2 == 0 else PB[4]; gbn = 'pb5' if j % 2 == 0 else 'pb4'
                    bbk = PB[6] if j % 2 == 0 else PB[3]; bbn = 'pb6' if j % 2 == 0 else 'pb3'
                    for hl in range(2):
                        S.op('pe', lambda e, hl=hl, j=j: e.matmul(bbk[:, hl:hl + 1], lhsT=finp['pr'][hl * 64:(hl + 1) * 64, j * 128:(j + 1) * 128],
                                                                  rhs=bonesb[hl * 64:(hl + 1) * 64, hl * 64:hl * 64 + 1], start=True, stop=True), r=[FP_ + 'pr', 'cstb'], w=[bbn])
                    S.op('act', lambda e: e.activation(out=fin['bon'][:], in_=bbk[:, 0:2], func=AF.Copy), r=[bbn], w=[FN_ + 'bon'])
                    S.op('pe', lambda e, c=c: e.matmul(gbk[:, 0:128], lhsT=sgt[:, 0, j * 128:(j + 1) * 128], rhs=gupb[:, 0, hp * 128:(hp + 1) * 128], start=True, stop=False),
                         r=['sgt', 'lwdst'], w=[gbn])
                    S.op('pe', lambda e, c=c: e.matmul(gbk[:, 0:128], lhsT=sgt[0:32, 1, j * 128:(j + 1) * 128], rhs=gupb[0:32, 1, hp * 128:(hp + 1) * 128], start=False, stop=True),
                         r=['sgt', 'lwdst'], w=[gbn])
                    for hl in range(2):
                        hc = slice(hl * 64, hl * 64 + 64)
                        ysl = ysum[:, lc, hc]
                        so = hl * 12
                        S.op('dve', lambda e, ysl=ysl, so=so: e.bn_stats(out=fin['st'][:, so:so + 6], in_=ysl), r=['ysum'], w=[FN_ + 'st%d' % hl])
                        S.op('dve', lambda e, so=so: e.bn_aggr(out=fin['st'][:, so + 6:so + 8], in_=fin['st'][:, so:so + 6]), r=[FN_ + 'st%d' % hl], w=[FN_ + 'nm%d' % hl])
                        S.op('dve', lambda e, so=so: e.tensor_scalar(out=fin['st'][:, so + 8:so + 9], in0=fin['st'][:, so + 7:so + 8], scalar1=64e-5, scalar2=None, op0=ALU.add),
                             r=[FN_ + 'nm%d' % hl], w=[FN_ + 'v%d' % hl])
                        S.op('act', lambda e, so=so: e.activation(out=fin['st'][:, so + 8:so + 9], in_=fin['st'][:, so + 8:so + 9], func=AF.Sqrt), r=[FN_ + 'v%d' % hl], w=[FN_ + 'v%d' % hl])
                        S.op('dve', lambda e, so=so: e.reciprocal(out=fin['st'][:, so + 9:so + 10], in_=fin['st'][:, so + 8:so + 9]), r=[FN_ + 'v%d' % hl], w=[FN_ + 'rs%d' % hl])
                        S.op('dve', lambda e, ysl=ysl, hc=hc, so=so: e.tensor_scalar(out=fin['yc'][:, hc], in0=ysl, scalar1=fin['st'][:, so + 6:so + 7], scalar2=fin['st'][:, so + 9:so + 10],
                                                                                  op0=ALU.subtract, op1=ALU.mult), r=['ysum', FN_ + 'nm%d' % hl, FN_ + 'rs%d' % hl], w=[FN_ + 'yc%d' % hl])
                        gch = slice(hl * 64, hl * 64 + 64)
                        S.op('pool', lambda e, hc=hc, gch=gch: e.tensor_tensor(out=fin['yc'][:, hc], in0=fin['yc'][:, hc], in1=lnxbc[:, 0, gch], op=ALU.mult), r=[FN_ + 'yc%d' % hl, 'lnxbc'], w=[FN_ + 'yc%d' % hl])
                        S.op('pool', lambda e, hc=hc, gch=gch: e.tensor_tensor(out=fin['yc'][:, hc], in0=fin['yc'][:, hc], in1=lnxbc[:, 1, gch], op=ALU.add),
                             r=[FN_ + 'yc%d' % hl, 'lnxbc'], w=[FN_ + 'yc%d' % hl])
                        S.op('dve', lambda e, hl=hl, hc=hc, c=c: e.scalar_tensor_tensor(out=fin['yc'][:, hc], in0=Vm[:, c, hc], scalar=fin['bon'][:, hl:hl + 1], in1=fin['yc'][:, hc],
                                                                                      op0=ALU.mult, op1=ALU.add), r=['Vm', FN_ + 'bon', FN_ + 'yc%d' % hl], w=[FN_ + 'yc%d' % hl])
                    S.op('dve', lambda e: e.tensor_tensor(out=fin['yo'][:], in0=gbk[:, 0:128], in1=fin['yc'][:], op=ALU.mult), r=[gbn, FN_ + 'yc0', FN_ + 'yc1'], w=[FN_ + 'yo'])
                    S.op('pe', lambda e, j=j: e.transpose(out=PT[:, j * 128:(j + 1) * 128], in_=fin['yo'][:], identity=identb), r=[FN_ + 'yo', 'cstb'], w=['pbT'])
                S.op('act', lambda e, s8=s8: e.activation(out=yaT[s8 % 2][:], in_=PT[:, 0:512], func=AF.Copy), r=['pbT'], w=['yaT%d' % (s8 % 2)])
                S.dma('sp', ya_s[hp, :, ls], yaT[s8 % 2][:], r=['yaT%d' % (s8 % 2)], w=['ya_s'])
        S.barrier(); S.emit()
        rw.close(); open_stacks.pop()

        if stage == 'rwkv':
            with ExitStack() as ps:
                dt_ = T(ps, 'dbg_t', [128, T_LAT], BF16); df_ = T(ps, 'dbg_f', [128, T_LAT], F32)
                toks = []
                dbgv = dbg_d.rearrange("(a b) d -> a (b d)", b=4)
                for hp in range(nhp):
                    S.dma('sp', dt_[:], ya_s[hp, :, :], r=['ya_s'], w=['dbg_t'])
                    S.op('dve', lambda e: e.tensor_copy(out=df_[:], in_=dt_[:]), r=['dbg_t'], w=['dbg_f'])
                    toks.append(S.dma('sp', dbgv[hp * 128:(hp + 1) * 128, :], df_[:], r=['dbg_f'], w=['dbg']))
                S.op('pool', lambda e: e.memset(df_[:, 0:8], 0.0), r=['dbg'], w=['dbg_f'])
                toks.append(S.dma('sp', out_d[0:128, 0:8], df_[:, 0:8], r=['dbg_f'], w=['out']))
                S.wait_all('sp', toks)
                S.emit()
            return nc

        lr = ExitStack(); open_stacks.append(lr)
        zx = T(lr, 'zx', [128, T_ALL], F32); xc = T(lr, 'xc', [128, T_ALL], F32); xcb = T(lr, 'xcb', [128, T_ALL], BF16)
        glu = T(lr, 'glu', [128, T_LAT], BF16)
        aa = [T(lr, 'aa0', [128, T_ALL], F32)] * 2
        bx = [T(lr, 'bx0', [128, T_ALL], F32)] * 2
        hh = [T(lr, 'hh%d' % i, [128, T_ALL], F32) for i in range(2)]
        ybt = T(lr, 'ybt', [128, T_LAT], BF16)
        gws = T(lr, 'gws', [128, 4, 64], F32); gwb = T(lr, 'gwb', [128, 4, 64], BF16)
        ltA = [[T(lr, 'lt%d_%d' % (i, r_), [128, 512], F32) for i in range(4)] for r_ in range(3)]
        ltc = {'i': 0}
        for cb in range(8 if nhp == 8 else 1):
            def cons_lx(ps_ap, bn, g0, n, is_ctx):
                S.op('act', lambda e: e.activation(out=zx[:, g0:g0 + n], in_=ps_ap, func=AF.Copy), r=[bn], w=['zx'])

            def cons_lg(ps_ap, bn, g0, n, is_ctx):
                if not is_ctx:
                    S.op('act', lambda e: e.activation(out=glu[:, g0 - T_CTX:g0 - T_CTX + n], in_=ps_ap, func=AF.Gelu_apprx_tanh), r=[bn], w=['glu'])
            project_group([(OFF_LX + cb * 128, 128, cons_lx), (OFF_LG + cb * 128, 128, cons_lg)])
            S.dma('sp', gws[:], gw_d[:, cb, :, :], w=['gws'])
            S.op('pool', lambda e: e.tensor_copy(out=gwb[:], in_=gws[:]), r=['gws'], w=['gwb'])
            S.op('act', lambda e: e.activation(out=xc[:], in_=zx[:], func=AF.Identity, scale=cwT[:, cb, 2:3], bias=cbT[:, cb:cb + 1]), r=['zx', 'cwT', 'cbT'], w=['xc'])
            L0 = T_CTX
            for (o_sl, i_sl, wj) in [(slice(L0 + 128, T_ALL), slice(L0, T_ALL - 128), 0), (slice(L0 + 64, T_ALL), slice(L0, T_ALL - 64), 1),
                                     (slice(L0, T_ALL - 64), slice(L0 + 64, T_ALL), 3),
                                     (slice(2, 256), slice(0, 254), 0), (slice(1, 256), slice(0, 255), 1), (slice(0, 255), slice(1, 256), 3)]:
                S.op('dve', lambda e, o_sl=o_sl, i_sl=i_sl, wj=wj: e.scalar_tensor_tensor(out=xc[:, o_sl], in0=zx[:, i_sl], scalar=cwT[:, cb, wj:wj + 1], in1=xc[:, o_sl],
                                                                                      op0=ALU.mult, op1=ALU.add), r=['zx', 'xc', 'cwT'], w=['xc'])
            S.op('pool', lambda e: e.tensor_copy(out=xcb[:], in_=xc[:]), r=['xc'], w=['xcb'])
            for d in range(2):
                for ti, (g0, n, is_ctx) in enumerate(TT):
                    sl = slice(g0, g0 + n)
                    rot = ltc['i'] % 3; bset = ltc['i'] % 2; ltc['i'] += 1
                    lt = ltA[rot]; L_ = lambda k_: 'lt%d_%d' % (k_, rot)
                    for g_ in range(2):
                        dg = d * 2 + g_
                        bank = PB[g_ + 2 * bset]; bnn = 'pb%d' % (g_ + 2 * bset)
                        for nl in range(2):
                            hs = slice(nl * 64, nl * 64 + 64)
                            S.op('pe', lambda e, hs=hs, dg=dg, bank=bank, sl=sl, n=n: e.matmul(bank[hs, 0:n], lhsT=gwb[hs, dg, :], rhs=xcb[hs, sl], start=True, stop=True),
                                 r=['gwb', 'xcb'], w=[bnn])
                        S.op('act', lambda e, bank=bank, g_=g_, dg=dg, n=n: e.activation(out=lt[g_][:, 0:n], in_=bank[:, 0:n], func=AF.Sigmoid, bias=gbT[:, cb, dg:dg + 1]),
                             r=[bnn, 'gbT'], w=[L_(g_)])
                    if is_ctx:
                        a_out = aa[d][:, sl]; b_out = bx[d][:, sl]; v3 = lambda ap_: ap_
                    else:
                        r0 = (g0 - T_CTX) // 64
                        a_out = aa[d][:, T_CTX:].rearrange("p (c r) -> p r c", r=64)[:, r0:r0 + 8, :]
                        b_out = bx[d][:, T_CTX:].rearrange("p (c r) -> p r c", r=64)[:, r0:r0 + 8, :]
                        v3 = lambda ap_: ap_.rearrange("p (r c) -> p r c", c=64)
                    S.op('act', lambda e, n=n, a_out=a_out, v3=v3: e.activation(out=a_out, in_=v3(lt[0][:, 0:n]), func=AF.Exp, scale=nsp[:, cb, d:d + 1]), r=[L_(0), 'nsp'], w=['aa0'])
                    S.op('act', lambda e, n=n: e.activation(out=lt[2][:, 0:n], in_=lt[0][:, 0:n], func=AF.Exp, scale=nsp2[:, cb, d:d + 1]), r=[L_(0), 'nsp2'], w=[L_(2)])
                    S.op('act', lambda e, n=n: e.activation(out=lt[3][:, 0:n], in_=lt[2][:, 0:n], func=AF.Sqrt, scale=-1.0, bias=1.0), r=[L_(2)], w=[L_(3)])
                    S.op('pool', lambda e, n=n: e.tensor_tensor(out=lt[3][:, 0:n], in0=lt[3][:, 0:n], in1=lt[1][:, 0:n], op=ALU.mult), r=[L_(3), L_(1)], w=[L_(3)])
                    S.op('dve', lambda e, sl=sl, n=n, b_out=b_out, v3=v3: e.tensor_tensor(out=b_out, in0=v3(lt[3][:, 0:n]), in1=v3(xc[:, sl]), op=ALU.mult), r=[L_(3), 'xc'], w=['bx0'])
                A, Bx, Hh = aa[d], bx[d], hh[d]; an, bn2, hn = 'aa0', 'bx0', 'hh%d' % d
                if d == 0:
                    S.op('dve', lambda e: e.tensor_tensor_scan(out=Hh[:, 0:256], data0=A[:, 0:256], data1=Bx[:, 0:256], initial=0.0, op0=ALU.mult, op1=ALU.add), r=[an, bn2], w=[hn])
                    S.op('dve', lambda e: e.tensor_tensor_scan(out=Hh[:, L0:], data0=A[:, L0:], data1=Bx[:, L0:], initial=Hh[:, 255:256], op0=ALU.mult, op1=ALU.add), r=[an, bn2, hn], w=[hn])
                else:
                    S.op('dve', lambda e: e.tensor_tensor_scan(out=Hh[:, 255::-1], data0=A[:, 255::-1], data1=Bx[:, 255::-1], initial=0.0, op0=ALU.mult, op1=ALU.add), r=[an, bn2], w=[hn])
                    S.op('dve', lambda e: e.tensor_tensor_scan(out=Hh[:, T_ALL - 1:L0 - 1:-1], data0=A[:, T_ALL - 1:L0 - 1:-1], data1=Bx[:, T_ALL - 1:L0 - 1:-1], initial=Hh[:, 0:1], op0=ALU.mult, op1=ALU.add),
                         r=[an, bn2, hn], w=[hn])
            S.op('pool', lambda e: e.tensor_tensor(out=hh[0][:, L0:], in0=hh[0][:, L0:], in1=hh[1][:, L0:], op=ALU.add), r=['hh0', 'hh1'], w=['hh0'])
            S.op('dve', lambda e: e.tensor_tensor(out=ybt[:].rearrange("p (r c) -> p r c", c=64), in0=hh[0][:, L0:].rearrange("p (c r) -> p r c", r=64), in1=glu[:].rearrange("p (r c) -> p r c", c=64), op=ALU.mult), r=['hh0', 'glu'], w=['ybt'])
            S.dma('sp', yb_s[cb, :, :], ybt[:], r=['ybt'], w=['yb_s'])
        if stage == 'lru':
            S.dma('sp', ybt[:], yb_s[0, :, :], r=['yb_s'], w=['ybt'])
            S.op('dve', lambda e: e.tensor_copy(out=hh[0][:, 0:T_LAT], in_=ybt[:]), r=['ybt'], w=['hh0'])
            return finish_early(hh[0][:, 0:T_LAT], 128, 1024, 'hh0') if False else finish_rows(hh[0])
        S.barrier(); S.emit()
        lr.close(); open_stacks.pop()

        gates = T(es, 'gates', [128, 32, NEXP], F32)
        mg = ExitStack(); open_stacks.append(mg)
        Wm = {nm: T(mg, 'W_' + nm, [128, 8, D], BF16) for nm in ['ga', 'gb', 'a', 'b', 'o']}
        mixT = T(mg, 'mixT', [128, 8, 512], BF16); ust = T(mg, 'ust', [128, 8, 512], BF16)
        wstg = ust[:].bitcast(F32)
        srcs = {'ga': winv[:, :, OFF_GA:OFF_GA + D], 'gb': winv[:, :, OFF_GB:OFF_GB + D],
                'a': wa_d.rearrange("(k p) c -> p k c", p=128), 'b': wb_d.rearrange("(k p) c -> p k c", p=128), 'o': wo_d.rearrange("(k p) c -> p k c", p=128)}
        for nm in ['ga', 'gb', 'a', 'b', 'o']:
            for hf in range(4):
                S.dma('sp', wstg, srcs[nm][:, :, hf * 256:(hf + 1) * 256], w=['ust'])
                S.op('pool', lambda e, nm=nm, hf=hf: e.tensor_copy(out=Wm[nm][:, :, hf * 256:(hf + 1) * 256], in_=wstg), r=['ust'], w=['W_' + nm])
        rws = T(mg, 'rws', [128, 8, NEXP], F32); rwb = T(mg, 'rwb', [128, 8, NEXP], BF16)
        S.dma('sp', rws[:], rw_d.rearrange("(k p) c -> p k c", p=128), w=['rws'])
        S.op('pool', lambda e: e.tensor_copy(out=rwb[:], in_=rws[:]), r=['rws'], w=['rwb'])
        rbb = T(mg, 'rbb', [128, NEXP], F32); S.dma('sp', rbb[:], rb_d[:, :], w=['rbb'])
        gg = T(mg, 'gg', [128, 2, D], F32); S.dma('sp', gg[:], gt_s[:, :, :], r=['gt_s'], w=['gg'])
        yat = T(mg, 'yat', [128, 8, 512], BF16); ybt2 = T(mg, 'ybt2', [128, 8, 512], BF16)
        sgt = [T(mg, 'sgt%d' % i, [128, 512], F32) for i in range(2)]
        mt_ = [T(mg, 'mt%d' % i, [128, 512], F32) for i in range(2)]
        xres = T(mg, 'xres', [128, D], F32); h1t = T(mg, 'h1t', [128, D], F32); tmpy = h1t
        nst = {'ss': T(mg, 'm_ss', [128, 4], F32), 'xs': T(mg, 'm_xs', [128, D], BF16), 'junk': T(mg, 'm_junk', [128, D], BF16)}
        rt = {nm: T(mg, 'rt_' + nm, shp, F32) for nm, shp in [('sc', [128, 64]), ('bi', [128, 64]), ('m8', [128, 8, 8]), ('gs', [128, 8]), ('g8', [128, 8]),
                                                            ('gm', [128, 8]), ('mk', [128, 64]), ('t8', [128, 8]), ('gu', [128, 64]), ('dn', [128, 2]), ('ss', [128, 4])]}
        yav = ya_s.rearrange("h p t -> p h t"); ybv = yb_s.rearrange("h p t -> p h t")
        NT6 = 8 if nhp == 8 else 1
        mixA = [mixT, T(mg, 'mixT2', [128, 8, 512], BF16)]

        def emit_sub(tt, j):
            l0 = tt * 512
            mx = mixA[tt % 2]; mxn = 'mixT%d' % (tt % 2)
            tsl = slice(j * 128, (j + 1) * 128); st_i = tt * 4 + j
            row0 = l0 + j * 128
            S.dma('sp', xres[:], x_d[row0:row0 + 128, :], w=['xres'])
            for hf in range(2):
                for k in range(8):
                    S.op('pe', lambda e, hf=hf, k=k: e.matmul(PB[4 + hf][:, :], lhsT=mx[:, k, tsl], rhs=Wm['o'][:, k, hf * 512:(hf + 1) * 512], start=(k == 0), stop=(k == 7)),
                         r=[mxn, 'W_o'], w=['pb%d' % (4 + hf)])
            ss = rt['ss']
            for hf in range(2):
                S.op('act', lambda e, hf=hf: e.activation(out=nst['junk'][:, hf * 512:(hf + 1) * 512], in_=PB[4 + hf][:, :], func=AF.Square, accum_out=ss[:, hf:hf + 1]),
                     r=['pb%d' % (4 + hf)], w=['m_junk', 'rt_ss%d' % hf])
            S.op('dve', lambda e: e.tensor_tensor(out=ss[:, 2:3], in0=ss[:, 0:1], in1=ss[:, 1:2], op=ALU.add), r=['rt_ss0', 'rt_ss1'], w=['rt_ss2'])
            S.op('dve', lambda e: e.tensor_scalar(out=ss[:, 2:3], in0=ss[:, 2:3], scalar1=1.0 / D, scalar2=1e-6, op0=ALU.mult, op1=ALU.add), r=['rt_ss2'], w=['rt_ss2'])
            S.op('act', lambda e: e.activation(out=ss[:, 2:3], in_=ss[:, 2:3], func=AF.Sqrt), r=['rt_ss2'], w=['rt_ss2'])
            S.op('dve', lambda e: e.reciprocal(out=ss[:, 3:4], in_=ss[:, 2:3]), r=['rt_ss2'], w=['rt_ss3'])
            for hf in range(2):
                hsl = slice(hf * 512, (hf + 1) * 512)
                S.op('dve', lambda e, hf=hf, hsl=hsl: e.scalar_tensor_tensor(out=tmpy[:, hsl], in0=PB[4 + hf][:, :], scalar=ss[:, 3:4], in1=gg[:, 0, hsl], op0=ALU.mult, op1=ALU.mult),
                     r=['pb%d' % (4 + hf), 'rt_ss3', 'gg'], w=['h1t'])
            S.op('pool', lambda e: e.tensor_tensor(out=h1t[:], in0=tmpy[:], in1=xres[:], op=ALU.add), r=['h1t', 'xres'], w=['h1t'])
            S.dma('sp', h1_s[row0:row0 + 128, :], h1t[:], r=['h1t'], w=['h1_s'])
            norm_to_featT(nst, h1t[:], 'h1t', ust, 'ust', j * 128, lambda k: gs2[:, k:k + 1], lambda k: sh2[:, k:k + 1], 'm_')
            for k in range(8):
                S.op('pe', lambda e, k=k: e.matmul(PB[6][:, 0:NEXP], lhsT=ust[:, k, tsl], rhs=rwb[:, k, :], start=(k == 0), stop=(k == 7)), r=['ust', 'rwb'], w=['pb6'])
            S.op('act', lambda e: e.activation(out=rt['sc'][:], in_=PB[6][:, 0:NEXP], func=AF.Sigmoid), r=['pb6'], w=['rt_sc'])
            S.op('dve', lambda e: e.tensor_tensor(out=rt['bi'][:], in0=rt['sc'][:], in1=rbb[:], op=ALU.add), r=['rt_sc', 'rbb'], w=['rt_bi'])
            for gI in range(8):
                S.op('dve', lambda e, gI=gI: e.max(out=rt['m8'][:, gI, :], in_=rt['bi'][:, gI * 8:(gI + 1) * 8]), r=['rt_bi'], w=['rt_m8'])
            S.op('dve', lambda e: e.tensor_tensor(out=rt['gs'][:], in0=rt['m8'][:, :, 0], in1=rt['m8'][:, :, 1], op=ALU.add), r=['rt_m8'], w=['rt_gs'])
            S.op('dve', lambda e: e.max(out=rt['g8'][:], in_=rt['gs'][:]), r=['rt_gs'], w=['rt_g8'])
            S.op('dve', lambda e: e.tensor_scalar(out=rt['gm'][:], in0=rt['gs'][:], scalar1=rt['g8'][:, 3:4], scalar2=None, op0=ALU.is_ge), r=['rt_gs', 'rt_g8'], w=['rt_gm'])
            for gI in range(8):
                S.op('dve', lambda e, gI=gI: e.tensor_scalar(out=rt['mk'][:, gI * 8:(gI + 1) * 8], in0=rt['bi'][:, gI * 8:(gI + 1) * 8], scalar1=10.0, scalar2=rt['gm'][:, gI:gI + 1],
                                                            op0=ALU.add, op1=ALU.mult), r=['rt_bi', 'rt_gm'], w=['rt_mk'])
            S.op('dve', lambda e: e.max(out=rt['t8'][:], in_=rt['mk'][:]), r=['rt_mk'], w=['rt_t8'])
            S.op('dve', lambda e: e.scalar_tensor_tensor(out=rt['gu'][:], in0=rt['mk'][:], scalar=rt['t8'][:, 5:6], in1=rt['sc'][:], op0=ALU.is_ge, op1=ALU.mult),
                 r=['rt_mk', 'rt_t8', 'rt_sc'], w=['rt_gu'])
            S.op('dve', lambda e: e.tensor_reduce(out=rt['dn'][:, 0:1], in_=rt['gu'][:], axis=AX.X, op=ALU.add), r=['rt_gu'], w=['rt_dn'])
            S.op('dve', lambda e: e.reciprocal(out=rt['dn'][:, 1:2], in_=rt['dn'][:, 0:1]), r=['rt_dn'], w=['rt_dn1'])
            S.op('dve', lambda e, st_i=st_i: e.tensor_scalar(out=gates[:, st_i, :], in0=rt['gu'][:], scalar1=rt['dn'][:, 1:2], scalar2=2.5, op0=ALU.mult, op1=ALU.mult),
                 r=['rt_gu', 'rt_dn1'], w=['gates'])
            if j == 3:
                S.dma('sp', uT_s[:, :, l0:l0 + 512], ust[:], r=['ust'], w=['uT_s'])

        for tt in range(NT6):
            g0 = T_CTX + tt * 512; l0 = tt * 512
            xt = xnt[tt % 2]; xtn = 'xnt%d' % (tt % 2)
            S.dma('sp', xt[:, 0:4, :], xnv(tt + 1)[:, 0:4, :], r=['xn_s'], w=[xtn + 'a'])
            S.dma('sp', xt[:, 4:8, :], xnv(tt + 1)[:, 4:8, :], r=['xn_s'], w=[xtn + 'b'])
            S.dma('sp', yat[:], yav[:, :, l0:l0 + 512], r=['ya_s'], w=['yat'])
            S.dma('sp', ybt2[:], ybv[:, :, l0:l0 + 512], r=['yb_s'], w=['ybt2'])
            for dc in range(8):
                dsl = slice(dc * 128, (dc + 1) * 128)
                for bi_, (wn, rhs_t, rn) in enumerate([('ga', xt, xtn + 'a'), ('a', yat, 'yat'), ('gb', xt, xtn + 'a'), ('b', ybt2, 'ybt2')]):
                    for k in range(8):
                        S.op('pe', lambda e, bi_=bi_, wn=wn, rhs_t=rhs_t, k=k: e.matmul(PB[bi_][:, :], lhsT=Wm[wn][:, k, dsl], rhs=rhs_t[:, k, :], start=(k == 0), stop=(k == 7)),
                             r=['W_' + wn, rn] + ([xtn + 'b'] if rhs_t is xt else []), w=['pb%d' % bi_])
                S.op('act', lambda e: e.activation(out=sgt[0][:], in_=PB[0][:, :], func=AF.Sigmoid), r=['pb0'], w=['sgt0'])
                S.op('act', lambda e: e.activation(out=sgt[1][:], in_=PB[2][:, :], func=AF.Sigmoid), r=['pb2'], w=['sgt1'])
                S.op('dve', lambda e: e.tensor_tensor(out=mt_[0][:], in0=PB[1][:, :], in1=sgt[0][:], op=ALU.mult), r=['pb1', 'sgt0'], w=['mt0'])
                S.op('dve', lambda e: e.tensor_tensor(out=mt_[1][:], in0=PB[3][:, :], in1=sgt[1][:], op=ALU.mult), r=['pb3', 'sgt1'], w=['mt1'])
                S.op('pool', lambda e, dc=dc: e.tensor_tensor(out=mixA[tt % 2][:, dc, :], in0=mt_[0][:], in1=mt_[1][:], op=ALU.add), r=['mt0', 'mt1'], w=['mixT%d' % (tt % 2)])
                if tt > 0 and dc % 2 == 1:
                    emit_sub(tt - 1, dc // 2)
        for j_ in range(4):
            emit_sub(NT6 - 1, j_)
        S.barrier(); S.emit()
        mg.close(); open_stacks.pop()
        if stage == 'merge':
            return finish_rows2(h1_s, gates)

        me = ExitStack(); open_stacks.append(me)
        HT = T_LAT // 2
        uTh = T(me, 'uTh', [128, 8, HT], BF16)
        acc = T(me, 'acc', [128, 16, D], F32)
        gus = T(me, 'gus', [128, 4, 512], F32); dns = T(me, 'dns', [128, 2, 512], F32)
        gub = [T(me, 'gub%d' % i, [128, 8, 512], BF16) for i in range(2)]
        dnb = [T(me, 'dnb%d' % i, [128, 2, D], BF16) for i in range(2)]
        sgm = [T(me, 'sgm%d' % i, [128, 512], F32) for i in range(2)]
        hT = [T(me, 'hT%d' % i, [128, 2, 512], BF16) for i in range(2)]
        gg2 = T(me, 'gg2', [128, D], F32); S.dma('sp', gg2[:], gt_s[:, 1, :], r=['gt_s'], w=['gg2'])
        h1r = T(me, 'h1r', [128, D], F32); ot = T(me, 'ot', [128, D], F32)
        fss = T(me, 'fss', [128, 4], F32); fjk = gub[0][:, 0:2, :].rearrange("p a b -> p (a b)")
        out_toks = []
        NEX = NEXP if nhp == 8 else 2
        ei = 0
        pend = [None]

        def emit_down(e_, q, t4, hq, hqn, half):
            for j in range(4):
                st_l = t4 * 4 + j; st_g = half * 16 + st_l
                for hf in range(2):
                    bk = 4 + (j * 2 + hf) % 3; bkn = 'pb%d' % bk
                    for fc in range(2):
                        S.op('pe', lambda e, bk=bk, fc=fc, hf=hf, j=j, hq=hq, q=q: e.matmul(PB[bk][:, :], lhsT=hq[:, fc, j * 128:(j + 1) * 128], rhs=dnb[q][:, fc, hf * 512:(hf + 1) * 512],
                                                                                         start=(fc == 0), stop=(fc == 1)), r=[hqn, 'dnb%d' % q], w=[bkn])
                    asl = acc[:, st_l, hf * 512:(hf + 1) * 512]
                    if e_ < 0:
                        S.op('act', lambda e, bk=bk, asl=asl: e.activation(out=asl, in_=PB[bk][:, :], func=AF.Copy), r=[bkn], w=['acc%d' % st_l])
                    else:
                        S.op('dve', lambda e, bk=bk, asl=asl, st_g=st_g, e_=e_: e.scalar_tensor_tensor(out=asl, in0=PB[bk][:, :], scalar=gates[:, st_g, e_:e_ + 1], in1=asl, op0=ALU.mult, op1=ALU.add),
                             r=[bkn, 'gates', 'acc%d' % st_l], w=['acc%d' % st_l])

        for half in range(2 if nhp == 8 else 1):
            S.dma('sp', uTh[:], uT_s[:, :, half * HT:(half + 1) * HT], r=['uT_s'], w=['uTh'])
            for e_ in [-1] + list(range(NEX)):
                q = ei % 2; ei += 1
                gsrc = (sgu_d if e_ < 0 else egu_d[e_]).rearrange("(k p) c -> p k c", p=128)
                dsrc = (sdn_d if e_ < 0 else edn_d[e_]).rearrange("(k p) c -> p k c", p=128)
                for gh in range(2):
                    if 'w' in MOESKIP and e_ >= 1: break
                    S.dma('sp', gus[:], gsrc[:, gh * 4:(gh + 1) * 4, :], w=['gus'])
                    S.op('pool', lambda e, q=q, gh=gh: e.tensor_copy(out=gub[q][:, gh * 4:(gh + 1) * 4, :], in_=gus[:]), r=['gus'], w=['gub%d' % q])
                for dh in range(2):
                    if 'w' in MOESKIP and e_ >= 1: break
                    S.dma('sp', dns[:], dsrc[:, :, dh * 512:(dh + 1) * 512], w=['dns'])
                    S.op('pool', lambda e, q=q, dh=dh: e.tensor_copy(out=dnb[q][:, :, dh * 512:(dh + 1) * 512], in_=dns[:]), r=['dns'], w=['dnb%d' % q])
                for t4 in range(4):
                    tk = slice(t4 * 512, (t4 + 1) * 512)
                    for fc in range(4):
                        for k in range(8):
                            S.op('pe', lambda e, fc=fc, k=k, q=q: e.matmul(PB[fc][:, :], lhsT=gub[q][:, k, fc * 128:(fc + 1) * 128], rhs=uTh[:, k, tk], start=(k == 0), stop=(k == 7)),
                                 r=['gub%d' % q, 'uTh'], w=['pb%d' % fc])
                    hq = hT[t4 % 2]; hqn = 'hT%d' % (t4 % 2)
                    for fc in range(2):
                        S.op('act', lambda e, fc=fc: e.activation(out=sgm[fc][:], in_=PB[fc][:, :], func=AF.Silu), r=['pb%d' % fc], w=['sgm%d' % fc])
                        S.op('dve', lambda e, fc=fc, hq=hq: e.tensor_tensor(out=hq[:, fc, :], in0=PB[2 + fc][:, :], in1=sgm[fc][:], op=ALU.mult), r=['pb%d' % (2 + fc), 'sgm%d' % fc], w=[hqn])
                    if pend[0] is not None:
                        emit_down(*pend[0])
                    pend[0] = (e_, q, t4, hq, hqn, half)
            if pend[0] is not None:
                emit_down(*pend[0]); pend[0] = None
            for st_l in range(16):
                row0 = half * HT + st_l * 128
                S.dma('sp', h1r[:], h1_s[row0:row0 + 128, :], r=['h1_s'], w=['h1r'])
                S.op('act', lambda e, st_l=st_l: e.activation(out=fjk, in_=acc[:, st_l, :], func=AF.Square, accum_out=fss[:, 0:1]), r=['acc%d' % st_l], w=['gub0', 'fss0'])
                S.op('dve', lambda e: e.tensor_scalar(out=fss[:, 1:2], in0=fss[:, 0:1], scalar1=1.0 / D, scalar2=1e-6, op0=ALU.mult, op1=ALU.add), r=['fss0'], w=['fss1'])
                S.op('act', lambda e: e.activation(out=fss[:, 2:3], in_=fss[:, 1:2], func=AF.Sqrt), r=['fss1'], w=['fss2'])
                S.op('dve', lambda e: e.reciprocal(out=fss[:, 3:4], in_=fss[:, 2:3]), r=['fss2'], w=['fss3'])
                S.op('dve', lambda e, st_l=st_l: e.scalar_tensor_tensor(out=ot[:], in0=acc[:, st_l, :], scalar=fss[:, 3:4], in1=gg2[:], op0=ALU.mult, op1=ALU.mult),
                     r=['acc%d' % st_l, 'fss3', 'gg2'], w=['ot'])
                S.op('pool', lambda e: e.tensor_tensor(out=ot[:], in0=ot[:], in1=h1r[:], op=ALU.add), r=['ot', 'h1r'], w=['ot'])
                out_toks.append(S.dma('sp', out_d[row0:row0 + 128, :], ot[:], r=['ot'], w=['out']))
        S.wait_all('sp', out_toks)
        S.emit()
        me.close(); open_stacks.pop()
    return nc


def host_layout(inp, b):
    f = lambda a: np.ascontiguousarray(a, dtype=np.float32)
    pk = lambda v: f(np.asarray(v).reshape(-1, 128).T)
    bc = lambda v: f(np.broadcast_to(np.asarray(v)[None], (128,) + np.asarray(v).shape))
    m = {}
    m['x'] = f(inp['x'][b]); m['ctx'] = f(inp['ctx'][b])
    m['cvec'] = f(np.stack([pk(inp['c'][b]), pk(inp['c_ctx'])], axis=-1))
    m['w_mod'] = f(inp['w_mod'][0]); m['b_modT'] = pk(inp['b_mod'][0])
    bm = inp['b_mod'][0]
    m['b_mod_bc'] = bc(np.stack([bm[2048:3072], bm[5120:6144]]))
    ng = inp['norm_g'][0]
    m['norm_gT'] = f(np.stack([pk(ng[i]) for i in range(4)], axis=1))
    m['gpost_bc'] = bc(np.stack([ng[1], ng[3]]))
    m['w_in'] = f(inp['w_in'][0])
    mu = np.zeros((2, 28 * 128), np.float32); mu[:, :3488] = inp['shift_mu'][0]
    m['muT'] = f(np.stack([pk(mu[0]), pk(mu[1])], axis=-1))
    m['w0T'] = f(np.stack([pk(inp['rw_w0'][0][d]) for d in range(2)], axis=-1))
    m['a0T'] = f(np.stack([pk(inp['rw_a0'][0][d]) for d in range(2)], axis=-1))
    m['kkT'] = pk(inp['rw_k_k'][0]); m['kaT'] = pk(inp['rw_k_a'][0]); m['rkT'] = pk(inp['rw_r_k'][0].reshape(-1))
    m['lnx_bc'] = bc(inp['rw_lnx'][0])
    m['wupT'] = f(inp['rw_w_up'][0].reshape(128, D)); m['aupT'] = f(inp['rw_a_up'][0].reshape(128, D)); m['g_up'] = f(inp['rw_g_up'][0])
    m['cwT'] = f(np.stack([pk(inp['lru_conv_w'][0][j]) for j in range(4)], axis=-1)); m['cbT'] = pk(inp['lru_conv_b'][0])
    gb = inp['lru_gate_b'][0].reshape(4, D)
    m['gbT'] = f(np.stack([pk(gb[i]) for i in range(4)], axis=-1))
    m['llT'] = f(np.stack([pk(inp['lru_l'][0][d]) for d in range(2)], axis=-1))
    gw = inp['lru_gate_w'][0].reshape(4, 8, 2, 64, 64)
    m['gwT'] = f(np.transpose(gw, (2, 3, 1, 0, 4)).reshape(128, 8, 4, 64))
    m['w_branch_a'] = f(inp['w_branch_a'][0]); m['w_branch_b'] = f(inp['w_branch_b'][0]); m['w_out'] = f(inp['w_out'][0])
    m['router_w'] = f(inp['router_w'][0]); m['router_b_bc'] = bc(inp['router_b'][0])
    m['ex_w_gu'] = f(inp['ex_w_gu'][0]); m['ex_w_down'] = f(inp['ex_w_down'][0])
    m['sh_w_gu'] = f(inp['sh_w_gu'][0]); m['sh_w_down'] = f(inp['sh_w_down'][0])
    p = np.arange(128)[:, None]; c = np.arange(128)[None, :]
    cs = np.zeros((128, 7, 128), np.float32)
    cs[:, 0] = (p == c); cs[:, 1] = (p < c); cs[:, 2] = (p <= c); cs[:, 3] = (p > c); cs[:, 4] = (p >= c)
    cs[:, 5] = ((p // 64) == (c // 64)); cs[:, 6] = 1.0
    m['consts'] = cs
    return m


_NC = {}


def kernel(**inputs):
    inp = {k: np.asarray(v) for k, v in inputs.items()}
    if 'full' not in _NC:
        _NC['full'] = build('full')
    nc = _NC['full']
    in_maps = [host_layout(inp, b) for b in range(8)]
    res = run_bass_kernel_spmd(nc, in_maps, core_ids=list(range(8)))
    return np.stack([np.asarray(r['out'], dtype=np.float32) for r in res.results], axis=0)
```
